# Optimizing a Trainium2 kernel written in Bass

```python
import math
import jax, jax.numpy as jnp
from jax import lax
import numpy as np

D_MODEL = 1024
BATCH = 8
SEQ = 8192
DEPTH = 4

MIX_WIDTH = D_MODEL
GROUP_WIDTH = MIX_WIDTH // 4
DA_HEADS = 4
DA_QK_DIM = 32
DA_V_DIM = GROUP_WIDTH // DA_HEADS
Q_BLOCK = 128
GLA_HEADS = 4
GLA_K_DIM = 32
GLA_V_DIM = GROUP_WIDTH // GLA_HEADS
GLA_GATE_RANK = 16
GLA_GATE_NORMALIZER = 16.0
GLA_CHUNK = 64
HG_HEADS = 4
HG_K_DIM = 64
HG_V_DIM = GROUP_WIDTH // HG_HEADS
HG_CHUNK = 64
RW_HEADS = 4
RW_HEAD = GROUP_WIDTH // RW_HEADS
RW_DECAY_RANK = 32
RW_AAA_RANK = 32
RW_GATE_RANK = 64
RW_DECAY_SCALE = 0.606531
RW_LN_EPS = 64e-5
D_FF = 4 * D_MODEL
RMS_EPS = 1e-6
MASK_VALUE = -1e30
TINY = 1e-30

A_IN = 3 * GROUP_WIDTH
B_IN = 2 * GLA_HEADS * GLA_K_DIM + GROUP_WIDTH + GLA_GATE_RANK + GROUP_WIDTH
C_IN = HG_HEADS * HG_K_DIM * 2 + GROUP_WIDTH * 2
D_IN = 3 * GROUP_WIDTH + RW_DECAY_RANK + RW_AAA_RANK + RW_GATE_RANK
P_IN = A_IN + B_IN + C_IN + D_IN

kernel_name = "hybrid_parallel_head_group_decoder"


def split_cols(p, sizes):
    idx = [int(s) for s in np.cumsum(sizes)[:-1]]
    return jnp.split(p, idx, axis=-1)


def rms_norm(x, g, eps=RMS_EPS):
    xf = x.astype(jnp.float32)
    y = xf * lax.rsqrt(jnp.mean(xf * xf, axis=-1, keepdims=True) + eps)
    return (y * g.astype(jnp.float32)).astype(x.dtype)


def alibi_slopes(n):
    return 2.0 ** (-8.0 * jnp.arange(1, n + 1, dtype=jnp.float32) / n)


def diff_attention(p, q_norm_g, k_norm_g, lq1, lk1, lq2, lk2, out_norm_g, lam_init):
    B, S, _ = p.shape
    q, k, v = split_cols(p, (GROUP_WIDTH, GROUP_WIDTH, GROUP_WIDTH))
    q = rms_norm(q.reshape(B, S, DA_HEADS, 2, DA_QK_DIM), q_norm_g) * (DA_QK_DIM ** -0.5)
    k = rms_norm(k.reshape(B, S, DA_HEADS, 2, DA_QK_DIM), k_norm_g)
    v = v.reshape(B, S, DA_HEADS, DA_V_DIM).transpose(0, 2, 1, 3)
    lam = (jnp.exp(jnp.sum(lq1 * lk1)) - jnp.exp(jnp.sum(lq2 * lk2)) + lam_init).astype(jnp.float32)
    n_blk = S // Q_BLOCK
    qb = q.reshape(B, n_blk, Q_BLOCK, DA_HEADS, 2, DA_QK_DIM).transpose(1, 0, 3, 4, 2, 5)
    kt = k.transpose(0, 2, 3, 1, 4)
    slopes = alibi_slopes(DA_HEADS)
    k_pos = jnp.arange(S)

    def attend_block(args):
        blk, q_blk = args
        q_pos = blk * Q_BLOCK + jnp.arange(Q_BLOCK)
        dist = (q_pos[:, None] - k_pos[None, :]).astype(jnp.float32)
        bias = jnp.where(dist[None] >= 0, -slopes[:, None, None] * dist[None], MASK_VALUE)
        s = jnp.einsum('bhcqd,bhckd->bhcqk', q_blk, kt).astype(jnp.float32) + bias[None, :, None]
        prob = jax.nn.softmax(s, axis=-1)
        weights = prob[:, :, 0] - lam * prob[:, :, 1]
        return jnp.einsum('bhqk,bhkv->bhqv', weights.astype(v.dtype), v)

    o = lax.map(attend_block, (jnp.arange(n_blk), qb))
    o = o.transpose(1, 0, 3, 2, 4).reshape(B, S, DA_HEADS, DA_V_DIM)
    o = rms_norm(o, out_norm_g) * (1.0 - lam_init)
    return o.reshape(B, S, GROUP_WIDTH)


def chunk_gla(q, k, v, log_f, chunk):
    B, H, T, K = q.shape
    V = v.shape[-1]
    n = T // chunk

    def to_chunks(z):
        return z.astype(jnp.float32).reshape(B, H, n, chunk, z.shape[-1]).transpose(2, 0, 1, 3, 4)

    qc, kc, vc = to_chunks(q), to_chunks(k), to_chunks(v)
    bc = jnp.cumsum(to_chunks(log_f), axis=-2)
    causal = jnp.tril(jnp.ones((chunk, chunk), dtype=bool))[:, :, None]

    def step(state, inp):
        q_i, k_i, v_i, b_i = inp
        o_inter = jnp.einsum('bhck,bhkv->bhcv', q_i * jnp.exp(b_i), state)
        rel = b_i[:, :, :, None, :] - b_i[:, :, None, :, :]
        decay = jnp.exp(jnp.where(causal, rel, MASK_VALUE))
        scores = jnp.einsum('bhik,bhijk->bhij', q_i, decay * k_i[:, :, None, :, :])
        o_intra = jnp.einsum('bhij,bhjv->bhiv', scores, v_i)
        b_last = b_i[:, :, -1:, :]
        state = jnp.exp(b_last[:, :, 0, :, None]) * state + jnp.einsum(
            'bhck,bhcv->bhkv', k_i * jnp.exp(b_last - b_i), v_i)
        return state, o_inter + o_intra

    state0 = jnp.zeros((B, H, K, V), jnp.float32)
    _, o = lax.scan(step, state0, (qc, kc, vc, bc))
    return o.transpose(1, 2, 0, 3, 4).reshape(B, H, T, V)


def gla_mixer(p, gate_up, gate_b, out_norm_g):
    B, S, _ = p.shape
    qk = GLA_HEADS * GLA_K_DIM
    q, k, v, gd, og = split_cols(p, (qk, qk, GROUP_WIDTH, GLA_GATE_RANK, GROUP_WIDTH))
    heads = lambda z, d: z.reshape(B, S, -1, d).transpose(0, 2, 1, 3)
    log_a = jax.nn.log_sigmoid((gd @ gate_up + gate_b).astype(jnp.float32)) / GLA_GATE_NORMALIZER
    o = chunk_gla(heads(q, GLA_K_DIM) * (GLA_K_DIM ** -0.5), heads(k, GLA_K_DIM),
                  heads(v, GLA_V_DIM), heads(log_a, GLA_K_DIM), GLA_CHUNK)
    o = o.transpose(0, 2, 1, 3).astype(p.dtype)
    return rms_norm(o, out_norm_g).reshape(B, S, GROUP_WIDTH) * jax.nn.silu(og)


def hgrn2_mixer(p, lower_bound, out_norm_g):
    B, S, _ = p.shape
    fk = HG_HEADS * HG_K_DIM
    q, f, i, og = split_cols(p, (fk, fk, GROUP_WIDTH, GROUP_WIDTH))
    lb = lower_bound.astype(jnp.float32)
    z = f.astype(jnp.float32)
    forget = lb + (1.0 - lb) * jax.nn.sigmoid(z)
    log_f = jnp.log(jnp.maximum(forget, TINY))
    k = (1.0 - lb) * jax.nn.sigmoid(-z)
    heads = lambda t, d: t.reshape(B, S, -1, d).transpose(0, 2, 1, 3)
    o = chunk_gla(heads(jax.nn.silu(q), HG_K_DIM), heads(k, HG_K_DIM),
                  heads(i, HG_V_DIM), heads(log_f, HG_K_DIM), HG_CHUNK)
    o = o.transpose(0, 2, 1, 3).astype(p.dtype)
    return rms_norm(o, out_norm_g).reshape(B, S, GROUP_WIDTH) * jax.nn.silu(og)


def rwkv7_recurrence(r, w, k, v, kk, a):
    B, S, H, N = r.shape
    tm = lambda z: z.astype(jnp.float32).transpose(1, 0, 2, 3)

    def step(state, inp):
        r_t, w_t, k_t, v_t, kk_t, a_t = inp
        sa = jnp.einsum('bhvk,bhk->bhv', state, -kk_t)
        state = (state * w_t[:, :, None, :] + sa[..., None] * (kk_t * a_t)[:, :, None, :]
                 + v_t[..., None] * k_t[:, :, None, :])
        return state, jnp.einsum('bhvk,bhk->bhv', state, r_t)

    state0 = jnp.zeros((B, H, N, N), jnp.float32)
    _, y = lax.scan(step, state0, (tm(r), tm(w), tm(k), tm(v), tm(kk), tm(a)))
    return y.transpose(1, 0, 2, 3)


def rwkv7_mixer(p, shift_mu, w0, w_up, a0, a_up, g_up, k_k, k_a, r_k, ln_g, ln_b):
    B, S, _ = p.shape
    prev = jnp.pad(p, ((0, 0), (1, 0), (0, 0)))[:, :-1]
    p = p + shift_mu * (prev - p)
    r, k, v, wd, ad, gd = split_cols(p, (GROUP_WIDTH, GROUP_WIDTH, GROUP_WIDTH,
                                         RW_DECAY_RANK, RW_AAA_RANK, RW_GATE_RANK))
    log_w = -RW_DECAY_SCALE * jax.nn.sigmoid(w0 + jnp.tanh(wd) @ w_up)
    a = jax.nn.sigmoid(a0 + ad @ a_up)
    g = jax.nn.sigmoid(gd) @ g_up
    heads = lambda z: z.reshape(B, S, RW_HEADS, RW_HEAD)
    kk = heads(k * k_k).astype(jnp.float32)
    kk = kk * lax.rsqrt(jnp.maximum(jnp.sum(kk * kk, axis=-1, keepdims=True), 1e-24))
    k = k * (1.0 + (a - 1.0) * k_a)
    y = rwkv7_recurrence(heads(r), jnp.exp(heads(log_w).astype(jnp.float32)), heads(k), heads(v), kk, heads(a))
    mu = jnp.mean(y, axis=-1, keepdims=True)
    var = jnp.mean(jnp.square(y - mu), axis=-1, keepdims=True)
    yn = ((y - mu) * lax.rsqrt(var + RW_LN_EPS)).reshape(B, S, GROUP_WIDTH)
    yn = yn * ln_g.astype(jnp.float32) + ln_b.astype(jnp.float32)
    bonus = jnp.sum(heads(r) * heads(k) * r_k, axis=-1, keepdims=True) * heads(v)
    out = (yn + bonus.reshape(B, S, GROUP_WIDTH).astype(jnp.float32)) * g.astype(jnp.float32)
    return out.astype(p.dtype)


def setup_inputs(seed: int = 0) -> dict:
    key = jax.random.key(seed)
    ks = list(jax.random.split(key, 32))
    nrm = lambda shape, scale: scale * jax.random.normal(ks.pop(), shape, jnp.float32)
    gain = lambda shape: 1.0 + nrm(shape, 0.02)
    L = DEPTH
    return {
        "x": nrm((BATCH, SEQ, D_MODEL), 1.0),
        "norm_mix_g": gain((L, D_MODEL)),
        "w_in": nrm((L, D_MODEL, P_IN), D_MODEL ** -0.5),
        "da_q_norm_g": gain((L, DA_QK_DIM)),
        "da_k_norm_g": gain((L, DA_QK_DIM)),
        "da_lambda_q1": nrm((L, DA_QK_DIM), 0.1),
        "da_lambda_k1": nrm((L, DA_QK_DIM), 0.1),
        "da_lambda_q2": nrm((L, DA_QK_DIM), 0.1),
        "da_lambda_k2": nrm((L, DA_QK_DIM), 0.1),
        "da_out_norm_g": gain((L, DA_V_DIM)),
        "gla_gate_up": nrm((L, GLA_GATE_RANK, GLA_HEADS * GLA_K_DIM), GLA_GATE_RANK ** -0.5),
        "gla_gate_b": nrm((L, GLA_HEADS * GLA_K_DIM), 0.1),
        "gla_out_norm_g": gain((L, GLA_V_DIM)),
        "hgrn_lb_logits": nrm((L, HG_HEADS * HG_K_DIM), 0.5),
        "hgrn_out_norm_g": gain((L, HG_V_DIM)),
        "rw_shift_mu": jax.random.uniform(ks.pop(), (L, D_IN), jnp.float32, 0.0, 1.0),
        "rw_w0": -0.5 + nrm((L, GROUP_WIDTH), 0.5),
        "rw_w_up": nrm((L, RW_DECAY_RANK, GROUP_WIDTH), 0.5 * RW_DECAY_RANK ** -0.5),
        "rw_a0": nrm((L, GROUP_WIDTH), 0.1),
        "rw_a_up": nrm((L, RW_AAA_RANK, GROUP_WIDTH), 0.5 * RW_AAA_RANK ** -0.5),
        "rw_g_up": nrm((L, RW_GATE_RANK, GROUP_WIDTH), RW_GATE_RANK ** -0.5),
        "rw_k_k": 0.85 + nrm((L, GROUP_WIDTH), 0.05),
        "rw_k_a": 1.0 + nrm((L, GROUP_WIDTH), 0.05),
        "rw_r_k": nrm((L, RW_HEADS, RW_HEAD), 0.1),
        "rw_ln_g": gain((L, GROUP_WIDTH)),
        "rw_ln_b": nrm((L, GROUP_WIDTH), 0.02),
        "w_out": nrm((L, MIX_WIDTH, D_MODEL), MIX_WIDTH ** -0.5),
        "norm_mlp_g": gain((L, D_MODEL)),
        "w_mlp_up": nrm((L, D_MODEL, D_FF), D_MODEL ** -0.5),
        "w_mlp_down": nrm((L, D_FF, D_MODEL), D_FF ** -0.5),
    }


def reference(x, norm_mix_g, w_in, da_q_norm_g, da_k_norm_g, da_lambda_q1, da_lambda_k1,
              da_lambda_q2, da_lambda_k2, da_out_norm_g, gla_gate_up, gla_gate_b, gla_out_norm_g,
              hgrn_lb_logits, hgrn_out_norm_g, rw_shift_mu, rw_w0, rw_w_up, rw_a0, rw_a_up, rw_g_up,
              rw_k_k, rw_k_a, rw_r_k, rw_ln_g, rw_ln_b, w_out, norm_mlp_g, w_mlp_up, w_mlp_down):
    probs = jax.nn.softmax(hgrn_lb_logits.astype(jnp.float32), axis=0)
    lower_bounds = jnp.cumsum(probs, axis=0) - probs[0]
    for l in range(DEPTH):
        lam_init = 0.8 - 0.6 * math.exp(-0.3 * l)
        h = rms_norm(x, norm_mix_g[l])
        proj = h @ w_in[l]
        pa, pb, pc, pd = split_cols(proj, (A_IN, B_IN, C_IN, D_IN))
        o_a = diff_attention(pa, da_q_norm_g[l], da_k_norm_g[l], da_lambda_q1[l], da_lambda_k1[l],
                             da_lambda_q2[l], da_lambda_k2[l], da_out_norm_g[l], lam_init)
        o_b = gla_mixer(pb, gla_gate_up[l], gla_gate_b[l], gla_out_norm_g[l])
        o_c = hgrn2_mixer(pc, lower_bounds[l], hgrn_out_norm_g[l])
        o_d = rwkv7_mixer(pd, rw_shift_mu[l], rw_w0[l], rw_w_up[l], rw_a0[l], rw_a_up[l], rw_g_up[l],
                          rw_k_k[l], rw_k_a[l], rw_r_k[l], rw_ln_g[l], rw_ln_b[l])
        mixed = jnp.concatenate([o_a, o_b.astype(x.dtype), o_c.astype(x.dtype), o_d], axis=-1)
        x = x + mixed @ w_out[l]
        h = rms_norm(x, norm_mlp_g[l])
        x = x + jnp.square(jax.nn.relu(h @ w_mlp_up[l])) @ w_mlp_down[l]
    return x
```

```python
import numpy as np
from contextlib import ExitStack
import concourse.bass as bass
import concourse.mybir as mybir

F32 = mybir.dt.float32
BF16 = mybir.dt.bfloat16
I32 = mybir.dt.int32
ALU = mybir.AluOpType
AF = mybir.ActivationFunctionType
AX = mybir.AxisListType

ENGS = ("pe", "act", "dve", "pool", "sp")
N_DMA_SEMS = 24


import types


def freeze(fn):
    if fn is None or fn.__closure__ is None:
        return fn
    cells = []
    for c in fn.__closure__:
        try:
            cells.append(types.CellType(c.cell_contents))
        except ValueError:
            cells.append(c)
    return types.FunctionType(fn.__code__, fn.__globals__, fn.__name__, fn.__defaults__, tuple(cells))


class Prog:
    def __init__(self, nc):
        self.nc = nc
        self.es = ExitStack()
        self.ops = {e: [] for e in ENGS}
        self.cnt = {e: 0 for e in ENGS}
        self.st = {}
        self.dma_tot = [0] * N_DMA_SEMS
        self.dma_rr = 0
        self.waited = {e: {} for e in ENGS}
        self.dma_events = []
        self.n_t = 0

    def sb(self, shape, dt, name=None):
        self.n_t += 1
        return self.es.enter_context(self.nc.sbuf_tensor(name or f"t{self.n_t}", list(shape), dt))

    def ps(self, shape, dt, name=None):
        self.n_t += 1
        return self.es.enter_context(self.nc.psum_tensor(name or f"p{self.n_t}", list(shape), dt))

    @staticmethod
    def K(k):
        if isinstance(k, (str, int)):
            return k
        if isinstance(k, tuple):
            return tuple(Prog.K(x) for x in k)
        return k.name

    def _deps(self, eng, reads, writes, is_dma):
        reads = [self.K(k) for k in reads]
        writes = [self.K(k) for k in writes]
        deps = []
        for k in reads:
            s = self.st.get(k)
            if s and s[0] is not None:
                deps.append((s[0], "raw"))
        for k in writes:
            s = self.st.get(k)
            if s:
                if s[0] is not None:
                    deps.append((s[0], "waw"))
                for r in s[1]:
                    deps.append((r, "war"))
        waits = []
        for ev, kind in deps:
            if ev[0] == "eng":
                _, e2, n = ev
                if e2 == eng and not is_dma:
                    if kind != "raw" or eng in ("pe", "sp"):
                        continue
                semkey = ("eng", e2)
                val = n
            else:
                _, si, tot = ev
                semkey = ("dma", si)
                val = tot
            if self.waited[eng].get(semkey, 0) >= val:
                continue
            self.waited[eng][semkey] = val
            waits.append((semkey, val))
        return waits

    def _commit(self, ev, reads, writes):
        reads = [self.K(k) for k in reads]
        writes = [self.K(k) for k in writes]
        for k in reads:
            s = self.st.setdefault(k, [None, []])
            s[1].append(ev)
        for k in writes:
            self.st[k] = [ev, []]

    def op(self, eng, fn, reads=(), writes=()):
        waits = self._deps(eng, reads, writes, False)
        self.cnt[eng] += 1
        ev = ("eng", eng, self.cnt[eng])
        self.ops[eng].append(dict(fn=freeze(fn), waits=waits, ev=ev))
        self._commit(ev, reads, writes)
        return ev

    def dma(self, eng, fn, reads=(), writes=()):
        si = self.dma_rr
        self.dma_rr = (self.dma_rr + 1) % N_DMA_SEMS
        waits = self._deps(eng, reads, writes, True)
        semkey = ("dma", si)
        prev = self.dma_tot[si]
        if prev > 0 and self.waited[eng].get(semkey, 0) < prev:
            self.waited[eng][semkey] = prev
            waits.append((semkey, prev))
        self.dma_tot[si] += 16
        ev = ("dma", si, self.dma_tot[si])
        self.ops[eng].append(dict(fn=freeze(fn), waits=waits, ev=ev))
        self._commit(ev, reads, writes)
        self.dma_events.append(ev)
        return ev

    def wait_events(self, eng, events):
        waits = []
        for ev in events:
            if ev[0] == "eng":
                semkey, val = ("eng", ev[1]), ev[2]
            else:
                semkey, val = ("dma", ev[1]), ev[2]
            if self.waited[eng].get(semkey, 0) >= val:
                continue
            self.waited[eng][semkey] = val
            waits.append((semkey, val))
        self.ops[eng].append(dict(fn=None, waits=waits, ev=None))

    def finalize(self):
        nc = self.nc
        fin = [("dma", si, self.dma_tot[si]) for si in range(N_DMA_SEMS) if self.dma_tot[si] > 0]
        self.wait_events("sp", fin)
        needed = {e: set() for e in ENGS}
        for e in ENGS:
            for o in self.ops[e]:
                for (semkey, val) in o["waits"]:
                    if semkey[0] == "eng":
                        needed[semkey[1]].add(val)
        rank = {}
        for e in ENGS:
            for i, v in enumerate(sorted(needed[e])):
                rank[(e, v)] = i + 1
        sems = {}
        for e in ENGS:
            sems[("eng", e)] = self.es.enter_context(nc.semaphore(f"s_{e}"))
        for si in range(N_DMA_SEMS):
            sems[("dma", si)] = self.es.enter_context(nc.semaphore(f"s_dma{si}"))
        handles = dict(pe="tensor", act="scalar", dve="vector", pool="gpsimd", sp="sync")
        self.n_inst = 0

        def emit_engine(e, eng):
            for o in self.ops[e]:
                for (semkey, val) in o["waits"]:
                    if semkey[0] == "eng":
                        val = rank[(semkey[1], val)]
                    eng.wait_ge(sems[semkey], val)
                    self.n_inst += 1
                if o["fn"] is None:
                    continue
                ins = o["fn"](eng)
                self.n_inst += 1
                ev = o["ev"]
                if ev[0] == "dma":
                    ins.then_inc(sems[("dma", ev[1])], 16)
                elif (e, ev[2]) in rank:
                    ins.then_inc(sems[("eng", e)], 1)

        with nc.Block() as block:
            @block.tensor
            def _(eng):
                emit_engine("pe", eng)

            @block.scalar
            def _(eng):
                emit_engine("act", eng)

            @block.vector
            def _(eng):
                emit_engine("dve", eng)

            @block.gpsimd
            def _(eng):
                emit_engine("pool", eng)

            @block.sync
            def _(eng):
                emit_engine("sp", eng)
        self.es.close()


import math
import numpy as np
import concourse.bass as bass
import concourse.mybir as mybir
from concourse.bass_utils import run_bass_kernel_spmd

D = 1024
PIN = 3472
DFF = 4096
TT = 256
NDIAG = TT // 128
RMS_EPS = 1e-6
SLOPES = [2.0 ** (-8.0 * (i + 1) / 4) for i in range(4)]
ND = 66

C_ID = 0
C_BD32 = 128
C_ST64 = 256
C_IN64 = 384
C_LO64 = 512
C_RS32 = 640
C_RS64 = 1152
C_CAUS = 1664
C_TM4 = 2176
C_CM = 2688
C_HM32 = 3712
C_HM64 = 3716
C_HM64B = 3718
C_TM2 = 3846
C_B0 = 4102
C_BH = C_B0 + 5
NCONST = C_BH + 3 * ND


def make_consts():
    c = np.zeros((128, NCONST), np.float32)
    p = np.arange(128)
    c[:, C_ID:C_ID + 128] = np.eye(128)
    same32 = (p[:, None] // 32) == (p[None, :] // 32)
    same64 = (p[:, None] // 64) == (p[None, :] // 64)
    c[:, C_BD32:C_BD32 + 128] = same32 & (p[:, None] <= p[None, :])
    c[:, C_ST64:C_ST64 + 128] = same64 & (p[:, None] < p[None, :])
    c[:, C_IN64:C_IN64 + 128] = same64 & (p[:, None] <= p[None, :])
    c[:, C_LO64:C_LO64 + 128] = same64 & (p[:, None] > p[None, :])
    t = np.arange(512)
    c[:, C_RS32:C_RS32 + 512] = (t % 32 != 0)[None, :]
    c[:, C_RS64:C_RS64 + 512] = (t % 64 != 0)[None, :]
    c[:, C_CAUS:C_CAUS + 512] = t[None, :] >= p[:, None]
    tok = np.arange(128)
    for cc in range(4):
        c[:, C_TM4 + 128 * cc:C_TM4 + 128 * (cc + 1)] = (tok // 32 == cc)[None, :]
    col = np.arange(256)
    for h in range(4):
        c[:, C_CM + 256 * h:C_CM + 256 * (h + 1)] = (col // 64 == h)[None, :]
        c[:, C_HM32 + h] = (p // 32 == h)
    for h in range(2):
        c[:, C_HM64 + h] = (p // 64 == h)
    c[:, C_HM64B:C_HM64B + 128] = (p[:, None] // 64) == (tok[None, :] // 64)
    for j in range(2):
        c[:, C_TM2 + 128 * j:C_TM2 + 128 * (j + 1)] = (tok // 64 == j)[None, :]
    for d in range(5):
        c[:, C_B0 + d] = SLOPES[0] * (p - 127 - 128 * d)
    for h in range(1, 4):
        for di in range(ND):
            delta = di - (NDIAG - 1)
            c[:, C_BH + (h - 1) * ND + di] = SLOPES[h] * (p - (TT - 1) - 128 * delta)
    return c


V_GMIX = 0
V_GMLP = 8
V_GQ = 16
V_GK = 17
V_GA = 18
V_GLAB = 19
V_MU = 20
V_W0 = 29
V_A0 = 31
V_KK = 33
V_KA = 35
V_RK = 37
V_LNG = 39
V_LNB = 41
NV = 43
R_LAM = 0
R_GB = 128
R_GC = 384
NR = 640
SW_GU = 0
SW_WU = 128
SW_AU = 384
SW_GUP = 640
NSW = 896


def pack_layer_params(inp, l):
    g = lambda n: np.asarray(inp[n][l], np.float32)
    v = np.zeros((128, NV), np.float32)
    v[:, V_GMIX:V_GMIX + 8] = g("norm_mix_g").reshape(8, 128).T
    v[:, V_GMLP:V_GMLP + 8] = g("norm_mlp_g").reshape(8, 128).T
    v[:, V_GQ] = np.tile(g("da_q_norm_g"), 4)
    v[:, V_GK] = np.tile(g("da_k_norm_g"), 4)
    v[:, V_GA] = np.tile(g("da_out_norm_g"), 2)
    v[:, V_GLAB] = g("gla_gate_b")
    mu = g("rw_shift_mu")
    for i in range(6):
        v[:, V_MU + i] = mu[128 * i:128 * (i + 1)]
    v[:32, V_MU + 6] = mu[768:800]
    v[:32, V_MU + 7] = mu[800:832]
    v[:64, V_MU + 8] = mu[832:896]
    for name, col in (("rw_w0", V_W0), ("rw_a0", V_A0), ("rw_k_k", V_KK), ("rw_k_a", V_KA),
                      ("rw_ln_g", V_LNG), ("rw_ln_b", V_LNB)):
        v[:, col:col + 2] = g(name).reshape(2, 128).T
    v[:, V_RK:V_RK + 2] = g("rw_r_k").reshape(2, 128).T
    r = np.zeros((128, NR), np.float32)
    lam = np.concatenate([g("da_lambda_q1"), g("da_lambda_k1"), g("da_lambda_q2"), g("da_lambda_k2")])
    r[:, R_LAM:R_LAM + 128] = lam[None, :]
    r[:, R_GB:R_GB + 256] = np.tile(g("gla_out_norm_g"), 4)[None, :]
    r[:, R_GC:R_GC + 256] = np.tile(g("hgrn_out_norm_g"), 4)[None, :]
    sw = np.zeros((64, NSW), np.float32)
    sw[:16, SW_GU:SW_GU + 128] = g("gla_gate_up")
    sw[:32, SW_WU:SW_WU + 256] = g("rw_w_up")
    sw[:32, SW_AU:SW_AU + 256] = g("rw_a_up")
    sw[:64, SW_GUP:SW_GUP + 256] = g("rw_g_up")
    return v, r, sw


class Ring:
    def __init__(self, items):
        self.items = items
        self.i = 0

    def get(self):
        x = self.items[self.i]
        self.i = (self.i + 1) % len(self.items)
        return x


class View:
    def __init__(self, ap, name):
        self.ap = ap
        self.name = name

    def __getitem__(self, k):
        return self.ap[k]


def build_program(S, n_layers, L_total, layer_ids, debug=False, mixers="ABCD", do_mlp=True):
    NT = S // TT
    nc = bass.Bass("TRN2", target_bir_lowering=False)
    dt_in = lambda name, shape: nc.dram_tensor(name, list(shape), F32, kind="ExternalInput").ap()
    xT_in = dt_in("xT", [D, S])
    w_in_d = dt_in("w_in", [n_layers, D, PIN])
    w_out_d = dt_in("w_out", [n_layers, D, D])
    w_up_d = dt_in("w_up", [n_layers, D, DFF])
    w_dn_d = dt_in("w_dn", [n_layers, DFF, D])
    vecs_d = dt_in("vecs", [n_layers, 128, NV])
    rows_d = dt_in("rows", [n_layers, 128, NR])
    sw_d = dt_in("smallw", [n_layers, 64, NSW])
    lbl_d = dt_in("lbl", [128, 2 * L_total])
    consts_d = dt_in("consts", [128, NCONST])
    yT = nc.dram_tensor("yT", [D, S], F32, kind="ExternalOutput").ap()
    xa = nc.dram_tensor("xa", [D, S], F32, kind="Internal").ap()
    xb = nc.dram_tensor("xb", [D, S], F32, kind="Internal").ap()
    dbg = nc.dram_tensor("dbg", [D, S], F32, kind="ExternalOutput").ap() if debug else None

    P = Prog(nc)
    cstb = P.sb([128, C_B0], BF16, "cstb")
    for cb in range(0, C_B0, 1024):
        ce = min(C_B0, cb + 1024)
        P.dma("pool", lambda e, cb=cb, ce=ce: e.dma_start(out=cstb[:, cb:ce], in_=consts_d[:, cb:ce]), writes=[cstb])
    biasc = P.sb([128, NCONST - C_B0], F32, "biasc")
    P.dma("sp", lambda e: e.dma_start(out=biasc[:], in_=consts_d[:, C_B0:NCONST]), writes=[biasc])
    ident = cstb[:, C_ID:C_ID + 128]
    caus = cstb[:, C_CAUS:C_CAUS + 512]
    ones_bf = P.sb([128, 128], BF16, "ones_bf")
    P.op("pool", lambda e: e.memset(ones_bf[:], 1.0), writes=[ones_bf])
    bo32 = P.sb([128, 128], BF16, "bo32")
    bo64 = P.sb([128, 128], BF16, "bo64")
    P.op("pool", lambda e: e.memset(bo32[:], 0.0), writes=[bo32])
    P.op("pool", lambda e: e.memset(bo64[:], 0.0), writes=[bo64])
    for b in range(4):
        P.op("pool", lambda e, b=b: e.memset(bo32[32 * b:32 * b + 32, 32 * b:32 * b + 32], 1.0), writes=[bo32])
    for b in range(2):
        P.op("pool", lambda e, b=b: e.memset(bo64[64 * b:64 * b + 64, 64 * b:64 * b + 64], 1.0), writes=[bo64])
    Esel = P.sb([128, 64], F32, "Esel")
    P.op("pool", lambda e: e.memset(Esel[:], 0.0), writes=[Esel])
    P.op("pool", lambda e: e.memset(Esel[64:65, :], 1.0), writes=[Esel])

    NKT = S // 128
    R1_EL = 65536
    R1 = P.sb([128, R1_EL], BF16, "R1")
    winA = R1[:, 0:6144].rearrange("p (k n) -> p k n", k=8)
    woA = R1[:, 6144:10240].rearrange("p (k n) -> p k n", k=4)
    o = 10240
    KT = R1[:, o:o + 2 * S].rearrange("p (c s) -> p c s", c=2); o += 2 * S
    Vc = R1[:, o:o + NKT * 260].rearrange("p (t h v) -> p t h v", h=4, v=65); o += NKT * 260
    assert o <= R1_EL
    winB = R1[:, 0:8 * PIN].rearrange("p (k n) -> p k n", k=8)
    woB = R1[:, 8 * PIN:8 * PIN + 6 * D].rearrange("p (k n) -> p k n", k=6)
    carve_o = [8 * PIN + 6 * D]

    def carve(shape, dt, name):
        n = int(np.prod(shape[1:]))
        nb = n if dt == BF16 else 2 * n
        a = carve_o[0]
        carve_o[0] += nb + (nb % 2)
        assert carve_o[0] <= R1_EL, (name, carve_o[0])
        ap = R1[:, a:a + nb]
        if dt != BF16:
            ap = ap.bitcast(dt)
        if len(shape) == 3:
            ap = ap.rearrange("p (a b) -> p a b", a=shape[1])
        return View(ap[0:shape[0]], name)
    wup = R1[:, 0:32768].rearrange("p (k n) -> p k n", k=8)
    wdn = R1[:, 32768:65536].rearrange("p (k n) -> p k n", k=32)

    NF = 16
    R2 = P.sb([128, 6144], F32, "R2")
    F = [View(R2[:, TT * i:TT * (i + 1)], f"F{i}") for i in range(NF)]
    R2b = R2[:, 4096:6144].bitcast(BF16)
    pT = Ring([View(R2b[:, TT * i:TT * (i + 1)], f"pT{i}") for i in range(4)])
    qn = R2b[:, 1024:1536].rearrange("p (c n) -> p c n", c=2)
    mixA = R2b[:, 1536:2560].rearrange("p (c n) -> p c n", c=4)
    mixB = R2b[:, 2560:4096].rearrange("p (c n) -> p c n", c=6)
    uT = R2[:, 0:4096].bitcast(BF16).rearrange("p (k n) -> p k n", k=32)
    FM = [View(R2[:, 4096 + TT * i:4096 + TT * (i + 1)], f"FM{i}") for i in range(8)]

    banks = [P.ps([128, 512], F32, f"bank{i}") for i in range(8)]
    pring = Ring(banks[0:4])
    acc = banks[4:8]

    xs = P.sb([128, 8, TT], F32, "xs")
    hT = P.sb([128, 8, TT], BF16, "hT")
    Bb = [P.sb([128, TT], BF16, f"B{i}") for i in range(12)]
    bring = Ring(Bb[8:12])
    vecs = P.sb([128, NV], F32, "vecs_s")
    rows = P.sb([128, NR], F32, "rows_s")
    neglam = P.sb([128, 1], F32, "neglam")
    ltmp = P.sb([128, 64], F32, "ltmp")
    lsum = P.sb([128, 2], F32, "lsum")


    Dsb = carve([128, 512], F32, "Dsb")
    Grep = carve([128, 512], F32, "Grep")
    Hs = carve([128, 512], F32, "Hs")
    HbB = carve([128, 8, 256], BF16, "HbB")
    HbC = [carve([128, 8, 128], BF16, f"HbC{i}") for i in range(2)]
    Vm = carve([128, 4, 256], BF16, "Vm")
    Vbd = carve([128, 4, 256], BF16, "Vbd")
    Qbd = [carve([128, 512], BF16, f"Qbd{i}") for i in range(2)]
    Qm = [carve([128, 4, 128], BF16, f"Qm{i}") for i in range(2)]
    khat = carve([128, 2, 256], BF16, "khat")
    Vtm = carve([128, 2, 256], BF16, "Vtm")
    sog = carve([128, 2, 256], F32, "sog")
    mtm = carve([128, 256], BF16, "mtm")
    Smk = carve([128, 4, 128], BF16, "Smk")
    pf = carve([128, TT + 2], F32, "pf")
    PRb = carve([128, 2, 256], BF16, "PRb")
    Ktb = carve([128, TT], BF16, "Ktb")
    Qtb = carve([128, TT], BF16, "Qtb")
    Khb = carve([128, TT], BF16, "Khb")
    Qhb = carve([128, TT], BF16, "Qhb")
    vpb = carve([128, TT], BF16, "vpb")
    Vdtm = carve([128, 128], BF16, "Vdtm")
    Ptm = carve([128, 128], BF16, "Ptm")
    Qhtm = carve([128, 128], BF16, "Qhtm")
    Khtm = carve([128, 128], BF16, "Khtm")
    AMb = [carve([128, 2, 256], BF16, f"AM{i}") for i in range(2)]
    Nb = [carve([128, 128], BF16, f"Nb{i}") for i in range(3)]
    Mb = [carve([128, 128], BF16, f"Mb{i}") for i in range(2)]
    Wb = [carve([128, 2, 128], BF16, f"Wb{i}") for i in range(2)]
    PRm = carve([128, 2, 256], BF16, "PRm")
    Rm = carve([128, 2, 128], BF16, "Rm")
    PH = carve([128, 128], BF16, "PH")
    PHT = carve([128, 128], BF16, "PHT")
    Umb = carve([128, 2, 128], BF16, "Umb")
    Ucomb = carve([128, 128], BF16, "Ucomb")
    Uhm = carve([128, 2, 128], BF16, "Uhm")
    Vmj = carve([128, 2, 128], BF16, "Vmj")
    Hexp = [carve([128, 128], F32, f"Hexp{i}") for i in range(2)]
    Hbf = [carve([128, 128], BF16, f"Hbf{i}") for i in range(3)]
    ysb = carve([128, 128], F32, "ysb")
    ynb = carve([128, 128], BF16, "ynb")
    prevc = P.sb([128, 9], F32, "prevc")
    lnst = P.sb([128, 16], F32, "lnst")
    ltot4 = P.sb([128, 4], F32, "ltot4")
    gam4 = [P.sb([128, 4], F32, f"gam4_{i}") for i in range(2)]
    carry = [P.sb([128, 64], F32, f"carry{i}") for i in range(3)]
    gam = [P.sb([128, 8], F32, f"gam{i}") for i in range(2)]
    ltot = P.sb([128, 8], F32, "ltot")
    st4 = P.sb([128, 16], F32, "st4")
    swb = P.sb([64, NSW], BF16, "swb")
    negb = P.sb([128, 1], F32, "negb")
    lbl = P.sb([128, 2 * L_total], F32, "lbl_s")
    lbT = P.sb([128, 2 * L_total], F32, "lbT")
    omlT = P.sb([128, 2 * L_total], F32, "omlT")
    lb_t = P.sb([128, 4], F32, "lb_t")
    ones4 = P.sb([128, L_total], F32, "ones4")
    P.dma("sp", lambda e: e.dma_start(out=lbl[:], in_=lbl_d[:, :]), writes=[lbl])
    P.op("pool", lambda e: e.memset(ones4[:], 1.0), writes=[ones4])
    Lt = L_total
    for rc in range(2):
        sl = slice(rc * Lt, (rc + 1) * Lt)
        P.op("dve", lambda e, sl=sl: e.tensor_reduce(out=lb_t[:, 0:1], in_=lbl[:, sl], axis=AX.X, op=ALU.max, negate=True),
             reads=[lbl], writes=[lb_t])
        P.op("act", lambda e, sl=sl: e.activation(out=lbT[:, sl], in_=lbl[:, sl], func=AF.Exp, bias=lb_t[:, 0:1], scale=1.0),
             reads=[lbl, lb_t], writes=[lbT])
        P.op("dve", lambda e, sl=sl: e.tensor_reduce(out=lb_t[:, 1:2], in_=lbT[:, sl], axis=AX.X, op=ALU.add),
             reads=[lbT], writes=[lb_t])
        P.op("dve", lambda e: e.reciprocal(out=lb_t[:, 2:3], in_=lb_t[:, 1:2]), reads=[lb_t], writes=[lb_t])
        P.op("dve", lambda e, sl=sl: e.tensor_scalar(out=lbT[:, sl], in0=lbT[:, sl], scalar1=lb_t[:, 2:3], scalar2=None,
                                                      op0=ALU.mult), reads=[lbT, lb_t], writes=[lbT])
        P.op("dve", lambda e, sl=sl: e.tensor_copy(out=lb_t[:, 3:4], in_=lbT[:, rc * Lt:rc * Lt + 1]), reads=[lbT], writes=[lb_t])
        P.op("dve", lambda e, sl=sl: e.tensor_tensor_scan(out=omlT[:, sl], data0=ones4[:], data1=lbT[:, sl], initial=0.0,
                                                           op0=ALU.mult, op1=ALU.add), reads=[lbT, ones4], writes=[omlT])
        P.op("dve", lambda e, sl=sl: e.tensor_scalar(out=lbT[:, sl], in0=omlT[:, sl], scalar1=lb_t[:, 3:4], scalar2=None,
                                                      op0=ALU.subtract), reads=[omlT, lb_t], writes=[lbT])
        P.op("dve", lambda e, sl=sl: e.tensor_scalar(out=omlT[:, sl], in0=lbT[:, sl], scalar1=-1.0, scalar2=1.0,
                                                      op0=ALU.mult, op1=ALU.add), reads=[lbT], writes=[omlT])

    def vcol(c, n=128):
        return vecs[0:n, c:c + 1]

    def barrier():
        evs = []
        for e in ENGS:
            if P.cnt[e] > 0:
                evs.append(("eng", e, P.cnt[e]))
        for si in range(N_DMA_SEMS):
            if P.dma_tot[si] > 0:
                evs.append(("dma", si, P.dma_tot[si]))
        for e in ENGS:
            P.wait_events(e, [ev for ev in evs if not (ev[0] == "eng" and ev[1] == e)])
        P.st.clear()

    def rmsnorm_tile(x_src, c0, n, gcol0, Ft):
        for k in range(8):
            P.dma("sp", lambda e, k=k: e.dma_start(
                out=xs[:, k, 0:n], in_=x_src[128 * k:128 * (k + 1), c0:c0 + n]), writes=[("xs", k)])
        ssp = pring.get()
        for k in range(8):
            sq = bring.get()
            P.op("act", lambda e, k=k, sq=sq: e.activation(out=sq[:, 0:n], in_=xs[:, k, 0:n], func=AF.Square),
                 reads=[("xs", k)], writes=[sq])
            P.op("pe", lambda e, k=k, sq=sq: e.matmul(ssp[:, 0:n], lhsT=ones_bf[:], rhs=sq[:, 0:n],
                                                      start=(k == 0), stop=(k == 7)),
                 reads=[ones_bf, sq], writes=[ssp])
        rstd = Ft
        P.op("act", lambda e: e.activation(out=rstd[:, 0:n], in_=ssp[:, 0:n], func=AF.Sqrt,
                                           scale=1.0 / D, bias=RMS_EPS), reads=[ssp], writes=[rstd])
        P.op("dve", lambda e: e.reciprocal(out=rstd[:, 0:n], in_=rstd[:, 0:n]), reads=[rstd], writes=[rstd])
        for k in range(8):
            P.op("dve", lambda e, k=k: e.scalar_tensor_tensor(
                out=hT[:, k, 0:n], in0=xs[:, k, 0:n], scalar=vcol(gcol0 + k), in1=rstd[:, 0:n],
                op0=ALU.mult, op1=ALU.mult), reads=[("xs", k), vecs, rstd], writes=[("hT", k)])

    x_src = xT_in
    for li in range(n_layers):
        lid = layer_ids[li]
        lam_init = 0.8 - 0.6 * math.exp(-0.3 * lid)
        x_mid = xa
        x_dst = yT if li == n_layers - 1 else xb
        barrier()
        P.dma("sp", lambda e, li=li: e.dma_start(out=vecs[:], in_=vecs_d[li, :, :]), writes=[vecs])
        P.dma("sp", lambda e, li=li: e.dma_start(out=rows[:], in_=rows_d[li, :, :]), writes=[rows])
        P.dma("pool", lambda e, li=li: e.dma_start(out=swb[:], in_=sw_d[li, :, :]), writes=[swb])
        P.op("dve", lambda e: e.tensor_scalar(out=negb[:], in0=vcol(V_GLAB), scalar1=-1.0, scalar2=None, op0=ALU.mult),
             reads=[vecs], writes=[negb])
        lam4 = rows[:, R_LAM:R_LAM + 128].rearrange("p (a t b) -> p a t b", a=2, t=2)
        P.op("dve", lambda e: e.tensor_tensor(
            out=ltmp[:].rearrange("p (a b) -> p a b", a=2), in0=lam4[:, :, 0, :], in1=lam4[:, :, 1, :],
            op=ALU.mult), reads=[rows], writes=[ltmp])
        P.op("dve", lambda e: e.tensor_reduce(out=lsum[:], in_=ltmp[:].rearrange("p (a b) -> p a b", a=2),
                                               axis=AX.X, op=ALU.add), reads=[ltmp], writes=[lsum])
        P.op("act", lambda e: e.activation(out=lsum[:], in_=lsum[:], func=AF.Exp), reads=[lsum], writes=[lsum])
        P.op("dve", lambda e, lam_init=lam_init: e.scalar_tensor_tensor(
            out=neglam[:], in0=lsum[:, 1:2], scalar=-lam_init, in1=lsum[:, 0:1], op0=ALU.add, op1=ALU.subtract),
            reads=[lsum], writes=[neglam])

        for phase in ("a", "b"):
            barrier()
            if phase == "a":
                win = winA
                P.dma("pool", lambda e, li=li: e.dma_start(
                    out=winA[:, :, :], in_=w_in_d[li, :, 0:768].rearrange("(k p) n -> p k n", p=128)), writes=["win"])
                P.dma("pool", lambda e, li=li: e.dma_start(
                    out=woA[0:64, :, :], in_=w_out_d[li, 0:256, :].rearrange("(h v) n -> v h n", v=64)), writes=["wo"])
                P.op("pool", lambda e: e.memset(Vc[:, :, :, 64:65], 1.0), writes=["Vones"])
            else:
                win = winB
                for cb in range(0, PIN, 512):
                    ce = min(PIN, cb + 512)
                    P.dma("pool", lambda e, li=li, cb=cb, ce=ce: e.dma_start(
                        out=winB[:, :, cb:ce], in_=w_in_d[li, :, cb:ce].rearrange("(k p) n -> p k n", p=128)),
                        writes=["win"])
                for c2 in range(6):
                    P.dma("pool", lambda e, li=li, c2=c2: e.dma_start(
                        out=woB[:, c2, :], in_=w_out_d[li, 256 + 128 * c2:256 + 128 * (c2 + 1), :]), writes=["wo"])
                for cr in carry:
                    P.op("pool", lambda e, cr=cr: e.memset(cr[:], 0.0), writes=[cr])
                P.op("pool", lambda e: e.memset(prevc[:], 0.0), writes=[prevc])
                for hx in Hexp:
                    P.op("pool", lambda e, hx=hx: e.memset(hx[:], 0.0), writes=[hx])
                P.op("pool", lambda e: e.memset(HbB[:], 0.0), writes=[HbB])
                for hb_ in HbC:
                    P.op("pool", lambda e, hb_=hb_: e.memset(hb_[:], 0.0), writes=[hb_])
            for t in range(NT):
                c0 = t * TT
                rmsnorm_tile(x_src, c0, TT, V_GMIX, F[15])
                hT_keys = [("hT", k) for k in range(8)]

                def fm_proj(col0, nrows):
                    ps = pring.get()
                    for k in range(8):
                        P.op("pe", lambda e, k=k, ps=ps: e.matmul(
                            ps[0:nrows, 0:TT], lhsT=win[:, k, col0:col0 + nrows], rhs=hT[:, k, :],
                            start=(k == 0), stop=(k == 7)), reads=["win"] + hT_keys, writes=[ps])
                    return ps

                def tm_proj(col0, ncols, sub):
                    ps = pring.get()
                    for k in range(8):
                        P.op("pe", lambda e, k=k, ps=ps: e.matmul(
                            ps[:, 0:ncols], lhsT=hT[:, k, 128 * sub:128 * (sub + 1)], rhs=win[:, k, col0:col0 + ncols],
                            start=(k == 0), stop=(k == 7)), reads=["win"] + hT_keys, writes=[ps])
                    return ps

                if phase == "a" and "A" in mixers:
                    for which in range(4):
                        isq = which < 2
                        ch = which % 2
                        ps = fm_proj((0 if isq else 256) + 128 * ch, 128)
                        qf, sq = F[0], Bb[0]
                        P.op("act", lambda e, ps=ps: e.activation(out=qf[:], in_=ps[:, 0:TT], func=AF.Copy),
                             reads=[ps], writes=[qf])
                        P.op("act", lambda e, ps=ps: e.activation(out=sq[:], in_=ps[:, 0:TT], func=AF.Square),
                             reads=[ps], writes=[sq])
                        gs = pring.get()
                        P.op("pe", lambda e, gs=gs: e.matmul(gs[:, 0:TT], lhsT=bo32[:], rhs=sq[:], start=True, stop=True),
                             reads=[bo32, sq], writes=[gs])
                        sd = F[1]
                        if isq:
                            P.op("act", lambda e, gs=gs: e.activation(out=sd[:], in_=gs[:, 0:TT], func=AF.Sqrt,
                                                                      scale=1.0, bias=32.0 * RMS_EPS),
                                 reads=[gs], writes=[sd])
                        else:
                            P.op("act", lambda e, gs=gs: e.activation(out=sd[:], in_=gs[:, 0:TT], func=AF.Sqrt,
                                                                      scale=1.0 / 32.0, bias=RMS_EPS),
                                 reads=[gs], writes=[sd])
                        P.op("dve", lambda e: e.reciprocal(out=sd[:], in_=sd[:]), reads=[sd], writes=[sd])
                        if isq:
                            P.op("dve", lambda e, ch=ch: e.scalar_tensor_tensor(
                                out=qn[:, ch, :], in0=qf[:], scalar=vcol(V_GQ), in1=sd[:], op0=ALU.mult, op1=ALU.mult),
                                reads=[qf, sd, vecs], writes=[("qn", ch)])
                        else:
                            P.op("dve", lambda e, ch=ch, c0=c0: e.scalar_tensor_tensor(
                                out=KT[:, ch, c0:c0 + TT], in0=qf[:], scalar=vcol(V_GK), in1=sd[:],
                                op0=ALU.mult, op1=ALU.mult),
                                reads=[qf, sd, vecs], writes=[("KT", ch, t)])
                    for sub in range(NDIAG):
                        ps = tm_proj(512, 256, sub)
                        P.op("act", lambda e, ps=ps, sub=sub, t=t: e.activation(
                            out=Vc[:, NDIAG * t + sub, :, 0:64], in_=ps[:, 0:256].rearrange("p (h v) -> p h v", h=4),
                            func=AF.Copy), reads=[ps, "Vones"], writes=[("Vc", NDIAG * t + sub)])

                    def attn_block(h, c, qlo, qn_cols, ktiles, bias_col_fn, o_ps, o_lo):
                        ch = (2 * h + c) // 4
                        off = 32 * ((2 * h + c) % 4)
                        first = True
                        for (kt, m) in ktiles:
                            cs = 0 if m is None else 128 * m
                            n = qn_cols - cs
                            sp_ = pring.get()
                            P.op("pe", lambda e, sp_=sp_, kt=kt, cs=cs, n=n: e.matmul(
                                sp_[:, 0:n], lhsT=KT[off:off + 32, ch, 128 * kt:128 * kt + 128],
                                rhs=qn[off:off + 32, ch, qlo + cs:qlo + cs + n], start=True, stop=True,
                                tile_position=(off, 0)),
                                reads=[("KT", ch, kt // NDIAG), ("qn", ch)], writes=[sp_])
                            pt = pT.get()
                            bc = bias_col_fn(kt)
                            P.op("act", lambda e, sp_=sp_, pt=pt, n=n, bc=bc: e.activation(
                                out=pt[:, 0:n], in_=sp_[:, 0:n], func=AF.Exp, bias=biasc[:, bc:bc + 1], scale=1.0),
                                reads=[sp_, biasc], writes=[pt])
                            if m is not None:
                                P.op("pool", lambda e, pt=pt, n=n: e.tensor_tensor(
                                    out=pt[:, 0:n], in0=pt[:, 0:n], in1=caus[:, 0:n], op=ALU.mult),
                                    reads=[pt, cstb], writes=[pt])
                            P.op("pe", lambda e, pt=pt, kt=kt, cs=cs, n=n, first=first: e.matmul(
                                o_ps[0:65, o_lo + cs:o_lo + cs + n], lhsT=Vc[:, kt, h, :], rhs=pt[:, 0:n],
                                start=first, stop=False), reads=[pt, ("Vc", kt)], writes=[o_ps])
                            first = False

                    for h in range(4):
                        onrm = [F[4], F[5]]
                        for c in range(2):
                            o_ps = acc[c]
                            if h == 0:
                                for qs in range(NDIAG):
                                    gq = NDIAG * t + qs
                                    kts = [(kt, None) for kt in range(max(0, gq - 4), gq)] + [(gq, 0)]
                                    attn_block(h, c, 128 * qs, 128, kts,
                                               lambda kt, gq=gq: (gq - kt), o_ps, 128 * qs)
                            else:
                                kts = [(kt, None) for kt in range(NDIAG * t)] + [(NDIAG * t + m, m) for m in range(NDIAG)]
                                attn_block(h, c, 0, TT, kts,
                                           lambda kt, h=h, t=t: 5 + (h - 1) * ND + (NDIAG * t - kt) + (NDIAG - 1),
                                           o_ps, 0)
                            oa = F[2 + c]
                            P.op("act", lambda e, o_ps=o_ps, oa=oa: e.activation(out=oa[0:65, :], in_=o_ps[0:65, 0:TT],
                                                                               func=AF.Copy),
                                 reads=[o_ps], writes=[oa])
                            dps = pring.get()
                            P.op("pe", lambda e, dps=dps, oa=oa: e.matmul(dps[0:64, 0:TT], lhsT=Esel[0:65, :],
                                                                          rhs=oa[0:65, :], start=True, stop=True),
                                 reads=[Esel, oa], writes=[dps])
                            rd = F[6]
                            P.op("dve", lambda e, dps=dps: e.reciprocal(out=rd[0:64, :], in_=dps[0:64, 0:TT]),
                                 reads=[dps], writes=[rd])
                            P.op("dve", lambda e, oa=oa, c=c: e.tensor_tensor(
                                out=onrm[c][0:64, :], in0=oa[0:64, :], in1=rd[0:64, :], op=ALU.mult),
                                reads=[oa, rd], writes=[onrm[c]])
                        df = F[7]
                        P.op("dve", lambda e: e.scalar_tensor_tensor(
                            out=df[0:64, :], in0=onrm[1][0:64, :], scalar=neglam[0:64, :], in1=onrm[0][0:64, :],
                            op0=ALU.mult, op1=ALU.add), reads=[onrm[0], onrm[1], neglam], writes=[df])
                        sq = Bb[1]
                        P.op("act", lambda e: e.activation(out=sq[0:64, :], in_=df[0:64, :], func=AF.Square),
                             reads=[df], writes=[sq])
                        mps = pring.get()
                        P.op("pe", lambda e, mps=mps: e.matmul(mps[0:64, 0:TT], lhsT=ones_bf[0:64, 0:64], rhs=sq[0:64, :],
                                                               start=True, stop=True), reads=[ones_bf, sq], writes=[mps])
                        sd = F[8]
                        s1 = 1.0 - lam_init
                        P.op("act", lambda e, mps=mps, s1=s1: e.activation(
                            out=sd[0:64, :], in_=mps[0:64, 0:TT], func=AF.Sqrt, scale=1.0 / (64.0 * s1 * s1),
                            bias=RMS_EPS / (s1 * s1)), reads=[mps], writes=[sd])
                        P.op("dve", lambda e: e.reciprocal(out=sd[0:64, :], in_=sd[0:64, :]), reads=[sd], writes=[sd])
                        P.op("dve", lambda e, h=h: e.scalar_tensor_tensor(
                            out=mixA[0:64, h, :], in0=df[0:64, :], scalar=vcol(V_GA, 64), in1=sd[0:64, :],
                            op0=ALU.mult, op1=ALU.mult), reads=[df, sd, vecs], writes=[("mixA", h)])
                elif phase == "a":
                    for h in range(4):
                        P.op("pool", lambda e, h=h: e.memset(mixA[0:64, h, :], 0.0), writes=[("mixA", h)])

                def transpose_to(out_ap, in_ap, out_key, in_key, eng="act"):
                    tp = pring.get()
                    tpb = tp[:].bitcast(BF16)
                    P.op("pe", lambda e: e.transpose(tpb[:, 0:128], in_ap, ident), reads=[in_key, cstb], writes=[tp])
                    if eng == "act":
                        P.op("act", lambda e: e.activation(out=out_ap, in_=tpb[:, 0:128], func=AF.Copy),
                             reads=[tp], writes=[out_key])
                    else:
                        P.op("dve", lambda e: e.tensor_copy(out=out_ap, in_=tpb[:, 0:128]), reads=[tp], writes=[out_key])

                def lin_attn(kind):
                    if kind == "B":
                        RC, Kh, qcol, kcol, cbase, sc = 1, 32, 768, 896, 0, -1.0 / 16.0
                        grow = rows[:, R_GB:R_GB + 256]
                    else:
                        RC, Kh, qcol, kcol, cbase, sc = 2, 64, 1552, 1808, 2, 1.0
                        grow = rows[:, R_GC:R_GC + 256]
                    hpc = 128 // Kh
                    qt_l, kt_l = [], []
                    for sub in range(NDIAG):
                        if kind == "B":
                            psv = tm_proj(1024, 256, sub)
                            P.op("act", lambda e, psv=psv, sub=sub: e.activation(out=Vtm[:, sub, :], in_=psv[:, 0:256], func=AF.Copy),
                                 reads=[psv], writes=[("Vtm", sub)])
                            pso = tm_proj(1296, 256, sub)
                            P.op("act", lambda e, pso=pso, sub=sub: e.activation(out=sog[:, sub, :], in_=pso[:, 0:256], func=AF.Silu),
                                 reads=[pso], writes=[("sog", sub)])
                        else:
                            psv = tm_proj(2064, 512, sub)
                            P.op("act", lambda e, psv=psv, sub=sub: e.activation(out=Vtm[:, sub, :], in_=psv[:, 0:256], func=AF.Copy),
                                 reads=[psv], writes=[("Vtm", sub)])
                            P.op("act", lambda e, psv=psv, sub=sub: e.activation(out=sog[:, sub, :], in_=psv[:, 256:512], func=AF.Silu),
                                 reads=[psv], writes=[("sog", sub)])
                    for rc in range(RC):
                        qt, ktl, kh_ = Bb[2 + rc], Bb[4 + rc], Bb[6]
                        qt_l.append(qt); kt_l.append(ktl)
                        lf, Lc, eq, ek, dd = F[0], F[1], F[2], F[3], F[4]
                        if kind == "B":
                            psg = fm_proj(1280, 16)
                            gdb = Bb[7]
                            P.op("act", lambda e, psg=psg: e.activation(out=gdb[0:16, :], in_=psg[0:16, 0:TT], func=AF.Copy),
                                 reads=[psg], writes=[gdb])
                            pre = pring.get()
                            P.op("pe", lambda e, pre=pre: e.matmul(pre[:, 0:TT], lhsT=swb[0:16, SW_GU:SW_GU + 128], rhs=gdb[0:16, :],
                                                                   start=True, stop=True), reads=[swb, gdb], writes=[pre])
                            e1 = F[5]
                            P.op("act", lambda e, pre=pre: e.activation(out=e1[:], in_=pre[:, 0:TT], func=AF.Exp, scale=-1.0,
                                                                        bias=negb[:, 0:1]), reads=[pre, negb], writes=[e1])
                            P.op("act", lambda e: e.activation(out=lf[:], in_=e1[:], func=AF.Ln, bias=1.0, scale=1.0),
                                 reads=[e1], writes=[lf])
                            psq = fm_proj(qcol, 128)
                            psk = fm_proj(kcol, 128)
                            kv = None
                        else:
                            psf = fm_proj(kcol + 128 * rc, 128)
                            sg, fg, kvf = F[5], F[6], F[7]
                            ez = F[11]
                            P.op("act", lambda e, psf=psf: e.activation(out=ez[:], in_=psf[:, 0:TT], func=AF.Exp, scale=-1.0),
                                 reads=[psf], writes=[ez])
                            P.op("dve", lambda e: e.tensor_scalar(out=sg[:], in0=ez[:], scalar1=1.0, scalar2=None, op0=ALU.add),
                                 reads=[ez], writes=[sg])
                            P.op("dve", lambda e: e.reciprocal(out=sg[:], in_=sg[:]), reads=[sg], writes=[sg])
                            lcol = rc * L_total + lid
                            P.op("dve", lambda e, lcol=lcol: e.tensor_scalar(
                                out=fg[:], in0=sg[:], scalar1=omlT[:, lcol:lcol + 1], scalar2=lbT[:, lcol:lcol + 1],
                                op0=ALU.mult, op1=ALU.add), reads=[sg, omlT, lbT], writes=[fg])
                            P.op("dve", lambda e: e.tensor_scalar(out=fg[:], in0=fg[:], scalar1=1e-30, scalar2=None, op0=ALU.max),
                                 reads=[fg], writes=[fg])
                            P.op("act", lambda e: e.activation(out=lf[:], in_=fg[:], func=AF.Ln), reads=[fg], writes=[lf])
                            P.op("dve", lambda e, lcol=lcol: e.scalar_tensor_tensor(
                                out=kvf[:], in0=ez[:], scalar=omlT[:, lcol:lcol + 1], in1=sg[:], op0=ALU.mult, op1=ALU.mult),
                                reads=[ez, sg, omlT], writes=[kvf])
                            psq = fm_proj(qcol + 128 * rc, 128)
                            qsl = F[8]
                            P.op("act", lambda e, psq=psq: e.activation(out=qsl[:], in_=psq[:, 0:TT], func=AF.Silu),
                                 reads=[psq], writes=[qsl])
                            kv = kvf
                        P.op("dve", lambda e: e.tensor_tensor_scan(out=Lc[:], data0=cstb[:, C_RS32:C_RS32 + TT], data1=lf[:],
                                                                   initial=0.0, op0=ALU.mult, op1=ALU.add),
                             reads=[cstb, lf], writes=[Lc])
                        Lc3 = Lc[:].rearrange("p (c j) -> p c j", j=32)
                        if debug and kind == "B" and t == 0:
                            P.op("dve", lambda e: e.tensor_copy(out=F[14][0:64, :], in_=swb[:, 0:256]), reads=[swb], writes=[F[14]])
                            P.dma("sp", lambda e: e.dma_start(out=dbg[768:832, 0:256], in_=F[14][0:64, :]), reads=[F[14]])
                            P.op("dve", lambda e: e.tensor_copy(out=F[13][0:32, :], in_=gdb[0:32, :]), reads=[gdb], writes=[F[13]])
                            P.dma("sp", lambda e: e.dma_start(out=dbg[832:864, 0:256], in_=F[13][0:32, :]), reads=[F[13]])
                        if debug and kind == "B" and False:
                            P.dma("sp", lambda e, c0=c0: e.dma_start(out=dbg[768:896, c0:c0 + TT], in_=Lc[:]), reads=[Lc])
                            P.dma("sp", lambda e, c0=c0: e.dma_start(out=dbg[896:1024, c0:c0 + TT], in_=lf[:]), reads=[lf])
                        P.op("dve", lambda e: e.tensor_copy(out=ltot[:], in_=Lc3[:, :, 31]), reads=[Lc], writes=[ltot])
                        P.op("act", lambda e: e.activation(out=eq[:], in_=Lc[:], func=AF.Exp, scale=sc), reads=[Lc], writes=[eq])
                        P.op("act", lambda e: e.activation(out=ek[:], in_=Lc[:], func=AF.Exp, scale=-sc), reads=[Lc], writes=[ek])
                        P.op("dve", lambda e: e.tensor_tensor(
                            out=dd[:].rearrange("p (c j) -> p c j", j=32), in0=ltot[:].unsqueeze(2).broadcast_to([128, 8, 32]),
                            in1=Lc3, op=ALU.subtract), reads=[ltot, Lc], writes=[dd])
                        P.op("act", lambda e: e.activation(out=dd[:], in_=dd[:], func=AF.Exp, scale=sc), reads=[dd], writes=[dd])
                        g_ = gam[rc]
                        P.op("act", lambda e, g_=g_: e.activation(out=g_[:], in_=ltot[:], func=AF.Exp, scale=sc),
                             reads=[ltot], writes=[g_])
                        if kind == "B":
                            P.op("dve", lambda e, psq=psq, qt=qt: e.scalar_tensor_tensor(
                                out=qt[:], in0=psq[:, 0:TT], scalar=32.0 ** -0.5, in1=eq[:], op0=ALU.mult, op1=ALU.mult),
                                reads=[psq, eq], writes=[qt])
                            P.op("dve", lambda e, psk=psk, ktl=ktl: e.tensor_tensor(out=ktl[:], in0=psk[:, 0:TT], in1=ek[:], op=ALU.mult),
                                 reads=[psk, ek], writes=[ktl])
                            P.op("dve", lambda e, psk=psk: e.tensor_tensor(out=kh_[:], in0=psk[:, 0:TT], in1=dd[:], op=ALU.mult),
                                 reads=[psk, dd], writes=[kh_])
                        else:
                            P.op("dve", lambda e, qt=qt: e.tensor_tensor(out=qt[:], in0=qsl[:], in1=eq[:], op=ALU.mult),
                                 reads=[qsl, eq], writes=[qt])
                            P.op("dve", lambda e, ktl=ktl: e.tensor_tensor(out=ktl[:], in0=kv[:], in1=ek[:], op=ALU.mult),
                                 reads=[kv, ek], writes=[ktl])
                            P.op("dve", lambda e: e.tensor_tensor(out=kh_[:], in0=kv[:], in1=dd[:], op=ALU.mult),
                                 reads=[kv, dd], writes=[kh_])
                        for sub in range(NDIAG):
                            transpose_to(khat[:, sub, 128 * rc:128 * rc + 128], kh_[:, 128 * sub:128 * sub + 128],
                                         ("khat", sub, rc), kh_)
                        W = 64 * hpc
                        wb = W * rc
                        D3 = Dsb[:].rearrange("p (v c) -> p v c", c=8)
                        G3 = Grep[:].rearrange("p (v c) -> p v c", c=8)
                        for sub in range(NDIAG):
                            P.op("pool", lambda e, sub=sub: e.tensor_tensor(
                                out=Vm[:], in0=Vtm[:, sub, :].unsqueeze(1).broadcast_to([128, 4, 256]),
                                in1=cstb[:, C_HM32:C_HM32 + 4].unsqueeze(2).broadcast_to([128, 4, 256]), op=ALU.mult),
                                reads=[("Vtm", sub), cstb], writes=[("Vm", 0)])
                            nslot = 512 // W
                            for c0_ in range(0, 4, nslot):
                                dps = pring.get()
                                for sl_ in range(nslot):
                                    cc = c0_ + sl_
                                    P.op("pe", lambda e, dps=dps, sl_=sl_, cc=cc, sub=sub: e.matmul(
                                        dps[:, sl_ * W:(sl_ + 1) * W], lhsT=khat[:, sub, 128 * rc:128 * rc + 128],
                                        rhs=Vm[:, cc, wb:wb + W], start=True, stop=True),
                                        reads=[("khat", sub, rc), ("Vm", 0)], writes=[dps])
                                for hh in range(hpc):
                                    hoff = Kh * hh
                                    cg = 4 * sub + c0_
                                    P.op("dve", lambda e, dps=dps, hh=hh, hoff=hoff, cg=cg: e.tensor_copy(
                                        out=D3[hoff:hoff + Kh, :, cg:cg + nslot],
                                        in_=dps[hoff:hoff + Kh, 0:nslot * W].rearrange("p (s w) -> p w s", w=W)[:, 64 * hh:64 * hh + 64, :]),
                                        reads=[dps], writes=[Dsb])
                            if kind == "C" and rc == 0 and sub == 0:
                                pass
                        cr = carry[(0 if kind == "B" else 1) + rc]
                        P.op("act", lambda e, g_=g_: e.activation(out=G3, in_=g_[:].unsqueeze(1).broadcast_to([128, 64, 8]),
                                                                  func=AF.Copy), reads=[g_], writes=[Grep])
                        P.op("pool", lambda e: e.memset(G3[:, :, 0:1], 0.0), reads=[], writes=[Grep])
                        P.op("dve", lambda e, cr=cr, g_=g_: e.scalar_tensor_tensor(
                            out=D3[:, :, 0], in0=cr[:], scalar=g_[:, 0:1], in1=D3[:, :, 0], op0=ALU.mult, op1=ALU.add),
                            reads=[cr, g_, Dsb], writes=[Dsb])
                        P.op("dve", lambda e: e.tensor_tensor_scan(out=Hs[:], data0=Grep[:], data1=Dsb[:], initial=0.0,
                                                                   op0=ALU.mult, op1=ALU.add), reads=[Grep, Dsb], writes=[Hs])
                        hb = HbB if kind == "B" else HbC[rc]
                        Hs_cv = Hs[:].rearrange("p (v c) -> p c v", c=8)
                        for hh in range(hpc):
                            hoff = Kh * hh
                            P.op("act", lambda e, hb=hb, cr=cr, hh=hh, hoff=hoff: e.activation(
                                out=hb[hoff:hoff + Kh, 0, 64 * hh:64 * hh + 64], in_=cr[hoff:hoff + Kh, :], func=AF.Copy),
                                reads=[cr], writes=[hb])
                            P.op("act", lambda e, hb=hb, hh=hh, hoff=hoff: e.activation(
                                out=hb[hoff:hoff + Kh, 1:8, 64 * hh:64 * hh + 64], in_=Hs_cv[hoff:hoff + Kh, 0:7, :], func=AF.Copy),
                                reads=[Hs], writes=[hb])
                        P.op("dve", lambda e, cr=cr: e.tensor_copy(out=cr[:], in_=Hs[:].rearrange("p (v c) -> p v c", c=8)[:, :, 7]),
                             reads=[Hs], writes=[cr])
                    W = 64 * hpc
                    hmK = cstb[:, C_HM32:C_HM32 + 4] if kind == "B" else cstb[:, C_HM64:C_HM64 + 2]
                    for sub in range(NDIAG):
                        sps = pring.get()
                        tsl = slice(128 * sub, 128 * sub + 128)
                        for rc in range(RC):
                            P.op("pool", lambda e, rc=rc, tsl=tsl: e.tensor_tensor(
                                out=Qbd[rc][:, 0:hpc * 128].rearrange("p (h i) -> p h i", h=hpc),
                                in0=qt_l[rc][:, tsl].unsqueeze(1).broadcast_to([128, hpc, 128]),
                                in1=hmK.unsqueeze(2).broadcast_to([128, hpc, 128]), op=ALU.mult),
                                reads=[qt_l[rc], cstb], writes=[Qbd[rc]])
                            P.op("pe", lambda e, rc=rc, tsl=tsl, sps=sps: e.matmul(
                                sps[:, 128 * hpc * rc:128 * hpc * (rc + 1)], lhsT=kt_l[rc][:, tsl], rhs=Qbd[rc][:, 0:hpc * 128],
                                start=True, stop=True), reads=[kt_l[rc], Qbd[rc]], writes=[sps])
                            P.op("pool", lambda e, rc=rc, tsl=tsl: e.tensor_tensor(
                                out=Qm[rc][:], in0=qt_l[rc][:, tsl].unsqueeze(1).broadcast_to([128, 4, 128]),
                                in1=cstb[:, C_TM4:C_TM4 + 512].rearrange("p (c i) -> p c i", c=4), op=ALU.mult),
                                reads=[qt_l[rc], cstb], writes=[Qm[rc]])
                        P.op("dve", lambda e, sps=sps: e.tensor_tensor(
                            out=Smk[:], in0=sps[:].rearrange("p (h i) -> p h i", h=4),
                            in1=cstb[:, C_BD32:C_BD32 + 128].unsqueeze(1).broadcast_to([128, 4, 128]), op=ALU.mult),
                            reads=[sps, cstb], writes=[Smk])
                        P.op("pool", lambda e, sub=sub: e.tensor_tensor(
                            out=Vbd[:], in0=Vtm[:, sub, :].unsqueeze(1).broadcast_to([128, 4, 256]),
                            in1=cstb[:, C_CM:C_CM + 1024].rearrange("p (h c) -> p h c", h=4), op=ALU.mult),
                            reads=[("Vtm", sub), cstb], writes=[Vbd])
                        ops_ = pring.get()
                        for h in range(4):
                            P.op("pe", lambda e, h=h, ops_=ops_: e.matmul(
                                ops_[:, 0:256], lhsT=Smk[:, h, :], rhs=Vbd[:, h, :], start=(h == 0), stop=False),
                                reads=[Smk, Vbd], writes=[ops_])
                        for rc in range(RC):
                            hb = HbB if kind == "B" else HbC[rc]
                            for cc in range(4):
                                last = (rc == RC - 1 and cc == 3)
                                P.op("pe", lambda e, rc=rc, cc=cc, hb=hb, last=last, ops_=ops_: e.matmul(
                                    ops_[:, W * rc:W * (rc + 1)], lhsT=Qm[rc][:, cc, :], rhs=hb[:, 4 * sub + cc, :],
                                    start=False, stop=last), reads=[Qm[rc], hb], writes=[ops_])
                        sqo, on = F[9], F[10]
                        P.op("act", lambda e, ops_=ops_: e.activation(out=sqo[:], in_=ops_[:, 0:256], func=AF.Square),
                             reads=[ops_], writes=[sqo])
                        P.op("dve", lambda e: e.tensor_reduce(out=st4[:, 0:4], in_=sqo[:].rearrange("p (h v) -> p h v", h=4),
                                                              axis=AX.X, op=ALU.add), reads=[sqo], writes=[st4])
                        P.op("act", lambda e: e.activation(out=st4[:, 4:8], in_=st4[:, 0:4], func=AF.Sqrt, scale=1.0 / 64.0,
                                                           bias=RMS_EPS), reads=[st4], writes=[st4])
                        P.op("dve", lambda e: e.reciprocal(out=st4[:, 8:12], in_=st4[:, 4:8]), reads=[st4], writes=[st4])
                        P.op("dve", lambda e, ops_=ops_: e.tensor_tensor(
                            out=on[:].rearrange("p (h v) -> p h v", h=4), in0=ops_[:, 0:256].rearrange("p (h v) -> p h v", h=4),
                            in1=st4[:, 8:12].unsqueeze(2).broadcast_to([128, 4, 64]), op=ALU.mult),
                            reads=[ops_, st4], writes=[on])
                        P.op("dve", lambda e: e.tensor_tensor(out=on[:], in0=on[:], in1=grow, op=ALU.mult),
                             reads=[on, rows], writes=[on])
                        P.op("dve", lambda e, sub=sub: e.tensor_tensor(out=mtm[:], in0=on[:], in1=sog[:, sub, :], op=ALU.mult),
                             reads=[on, ("sog", sub)], writes=[mtm])
                        for j in range(2):
                            transpose_to(mixB[:, cbase + j, 128 * sub:128 * sub + 128], mtm[:, 128 * j:128 * j + 128],
                                         ("mixB", cbase + j), mtm, eng="dve")

                if phase == "b" and "B" in mixers:
                    lin_attn("B")
                if phase == "b" and "C" in mixers:
                    lin_attn("C")
                def rwkv():
                    CD = 0.606531
                    hm64 = cstb[:, C_HM64:C_HM64 + 2]

                    def shifted(col0, nrows, mucol, pidx, out_ap, out_key):
                        ps = fm_proj(col0, nrows)
                        P.op("pool", lambda e: e.tensor_copy(out=pf[0:nrows, 0:1], in_=prevc[0:nrows, pidx:pidx + 1]),
                             reads=[prevc], writes=[pf])
                        P.op("act", lambda e: e.activation(out=pf[0:nrows, 1:TT + 1], in_=ps[0:nrows, 0:TT], func=AF.Copy),
                             reads=[ps], writes=[pf])
                        dtmp = F[15]
                        P.op("pool", lambda e: e.tensor_tensor(out=dtmp[0:nrows, :], in0=pf[0:nrows, 0:TT], in1=pf[0:nrows, 1:TT + 1],
                                                               op=ALU.subtract), reads=[pf], writes=[dtmp])
                        P.op("dve", lambda e: e.scalar_tensor_tensor(
                            out=out_ap, in0=dtmp[0:nrows, :], scalar=vcol(mucol, nrows), in1=pf[0:nrows, 1:TT + 1],
                            op0=ALU.mult, op1=ALU.add), reads=[dtmp, pf, vecs], writes=[out_key])
                        P.op("pool", lambda e: e.tensor_copy(out=prevc[0:nrows, pidx:pidx + 1], in_=pf[0:nrows, TT:TT + 1]),
                             reads=[pf], writes=[prevc])

                    tw, adb, sgd = Bb[0], Bb[1], Bb[2]
                    wdf = F[14]
                    shifted(3344, 32, V_MU + 6, 6, wdf[0:32, :], wdf)
                    P.op("act", lambda e: e.activation(out=tw[0:32, :], in_=wdf[0:32, :], func=AF.Tanh), reads=[wdf], writes=[tw])
                    shifted(3376, 32, V_MU + 7, 7, wdf[0:32, :], wdf)
                    P.op("act", lambda e: e.activation(out=adb[0:32, :], in_=wdf[0:32, :], func=AF.Copy), reads=[wdf], writes=[adb])
                    shifted(3408, 64, V_MU + 8, 8, wdf[0:64, :], wdf)
                    P.op("act", lambda e: e.activation(out=sgd[0:64, :], in_=wdf[0:64, :], func=AF.Sigmoid), reads=[wdf], writes=[sgd])

                    for rc in range(2):
                        rp, kp, vp, sgw, av, gT, kkn, kmod, bonus, Lw, Lx, eX, qa, dd = (F[i] for i in range(14))
                        shifted(2576 + 128 * rc, 128, V_MU + rc, rc, rp[:], rp)
                        shifted(2832 + 128 * rc, 128, V_MU + 2 + rc, 2 + rc, kp[:], kp)
                        shifted(3088 + 128 * rc, 128, V_MU + 4 + rc, 4 + rc, vp[:], vp)
                        P.op("act", lambda e: e.activation(out=vpb[:], in_=vp[:], func=AF.Copy), reads=[vp], writes=[vpb])
                        wps = pring.get()
                        P.op("pe", lambda e: e.matmul(wps[:, 0:TT], lhsT=swb[0:32, SW_WU + 128 * rc:SW_WU + 128 * rc + 128],
                                                      rhs=tw[0:32, :], start=True, stop=True), reads=[swb, tw], writes=[wps])
                        P.op("act", lambda e: e.activation(out=sgw[:], in_=wps[:, 0:TT], func=AF.Sigmoid, bias=vcol(V_W0 + rc), scale=1.0),
                             reads=[wps, vecs], writes=[sgw])
                        aps = pring.get()
                        P.op("pe", lambda e: e.matmul(aps[:, 0:TT], lhsT=swb[0:32, SW_AU + 128 * rc:SW_AU + 128 * rc + 128],
                                                      rhs=adb[0:32, :], start=True, stop=True), reads=[swb, adb], writes=[aps])
                        P.op("act", lambda e: e.activation(out=av[:], in_=aps[:, 0:TT], func=AF.Sigmoid, bias=vcol(V_A0 + rc), scale=1.0),
                             reads=[aps, vecs], writes=[av])
                        gps = pring.get()
                        P.op("pe", lambda e: e.matmul(gps[:, 0:TT], lhsT=swb[0:64, SW_GUP + 128 * rc:SW_GUP + 128 * rc + 128],
                                                      rhs=sgd[0:64, :], start=True, stop=True), reads=[swb, sgd], writes=[gps])
                        P.op("act", lambda e: e.activation(out=gT[:], in_=gps[:, 0:TT], func=AF.Copy), reads=[gps], writes=[gT])
                        P.op("dve", lambda e: e.tensor_scalar(out=kkn[:], in0=kp[:], scalar1=vcol(V_KK + rc), scalar2=None, op0=ALU.mult),
                             reads=[kp, vecs], writes=[kkn])
                        sqk = Bb[3]
                        P.op("act", lambda e: e.activation(out=sqk[:], in_=kkn[:], func=AF.Square), reads=[kkn], writes=[sqk])
                        sps_ = pring.get()
                        P.op("pe", lambda e: e.matmul(sps_[:, 0:TT], lhsT=bo64[:], rhs=sqk[:], start=True, stop=True),
                             reads=[bo64, sqk], writes=[sps_])
                        P.op("dve", lambda e: e.tensor_scalar(out=eX[:], in0=sps_[:, 0:TT], scalar1=1e-24, scalar2=None, op0=ALU.max),
                             reads=[sps_], writes=[eX])
                        P.op("act", lambda e: e.activation(out=eX[:], in_=eX[:], func=AF.Sqrt), reads=[eX], writes=[eX])
                        P.op("dve", lambda e: e.reciprocal(out=eX[:], in_=eX[:]), reads=[eX], writes=[eX])
                        P.op("dve", lambda e: e.tensor_tensor(out=kkn[:], in0=kkn[:], in1=eX[:], op=ALU.mult), reads=[kkn, eX], writes=[kkn])
                        P.op("dve", lambda e: e.tensor_scalar(out=kmod[:], in0=av[:], scalar1=-1.0, scalar2=vcol(V_KA + rc),
                                                              op0=ALU.add, op1=ALU.mult), reads=[av, vecs], writes=[kmod])
                        P.op("dve", lambda e: e.scalar_tensor_tensor(out=kmod[:], in0=kmod[:], scalar=1.0, in1=kp[:],
                                                                     op0=ALU.add, op1=ALU.mult), reads=[kmod, kp], writes=[kmod])
                        rkb = Bb[4]
                        P.op("dve", lambda e: e.scalar_tensor_tensor(out=rkb[:], in0=rp[:], scalar=vcol(V_RK + rc), in1=kmod[:],
                                                                     op0=ALU.mult, op1=ALU.mult), reads=[rp, kmod, vecs], writes=[rkb])
                        bps = pring.get()
                        P.op("pe", lambda e: e.matmul(bps[:, 0:TT], lhsT=bo64[:], rhs=rkb[:], start=True, stop=True),
                             reads=[bo64, rkb], writes=[bps])
                        P.op("dve", lambda e: e.tensor_tensor(out=bonus[:], in0=bps[:, 0:TT], in1=vp[:], op=ALU.mult),
                             reads=[bps, vp], writes=[bonus])
                        P.op("dve", lambda e: e.tensor_tensor_scan(out=Lw[:], data0=cstb[:, C_RS64:C_RS64 + TT], data1=sgw[:],
                                                                   initial=0.0, op0=ALU.mult, op1=ALU.add),
                             reads=[cstb, sgw], writes=[Lw])
                        P.op("pool", lambda e: e.tensor_tensor(out=Lx[:], in0=Lw[:], in1=sgw[:], op=ALU.subtract),
                             reads=[Lw, sgw], writes=[Lx])
                        Lw3 = Lw[:].rearrange("p (c j) -> p c j", j=64)
                        P.op("dve", lambda e: e.tensor_copy(out=ltot4[:], in_=Lw3[:, :, 63]), reads=[Lw], writes=[ltot4])
                        g4 = gam4[rc]
                        P.op("act", lambda e: e.activation(out=g4[:], in_=ltot4[:], func=AF.Exp, scale=-CD), reads=[ltot4], writes=[g4])
                        PR4 = PRb[:].rearrange("p s (w t) -> p s w t", w=2)
                        P.op("act", lambda e: e.activation(out=eX[:], in_=Lw[:], func=AF.Exp, scale=-CD), reads=[Lw], writes=[eX])
                        P.op("dve", lambda e: e.tensor_tensor(out=PR4[:, :, 1, :], in0=rp[:].rearrange("p (s t) -> p s t", s=2),
                                                              in1=eX[:].rearrange("p (s t) -> p s t", s=2), op=ALU.mult),
                             reads=[rp, eX], writes=[PRb])
                        P.op("act", lambda e: e.activation(out=eX[:], in_=Lw[:], func=AF.Exp, scale=CD), reads=[Lw], writes=[eX])
                        P.op("dve", lambda e: e.tensor_tensor(out=Ktb[:], in0=kmod[:], in1=eX[:], op=ALU.mult), reads=[kmod, eX], writes=[Ktb])
                        P.op("pool", lambda e: e.tensor_tensor(out=qa[:], in0=kkn[:], in1=av[:], op=ALU.mult), reads=[kkn, av], writes=[qa])
                        P.op("dve", lambda e: e.tensor_tensor(out=Qtb[:], in0=qa[:], in1=eX[:], op=ALU.mult), reads=[qa, eX], writes=[Qtb])
                        P.op("act", lambda e: e.activation(out=eX[:], in_=Lx[:], func=AF.Exp, scale=-CD), reads=[Lx], writes=[eX])
                        P.op("dve", lambda e: e.scalar_tensor_tensor(
                            out=PR4[:, :, 0, :], in0=kkn[:].rearrange("p (s t) -> p s t", s=2), scalar=-1.0,
                            in1=eX[:].rearrange("p (s t) -> p s t", s=2), op0=ALU.mult, op1=ALU.mult),
                            reads=[kkn, eX], writes=[PRb])
                        P.op("dve", lambda e: e.tensor_tensor(
                            out=dd[:].rearrange("p (c j) -> p c j", j=64), in0=ltot4[:].unsqueeze(2).broadcast_to([128, 4, 64]),
                            in1=Lw3, op=ALU.subtract), reads=[ltot4, Lw], writes=[dd])
                        P.op("act", lambda e: e.activation(out=dd[:], in_=dd[:], func=AF.Exp, scale=-CD), reads=[dd], writes=[dd])
                        P.op("dve", lambda e: e.tensor_tensor(out=Khb[:], in0=kmod[:], in1=dd[:], op=ALU.mult), reads=[kmod, dd], writes=[Khb])
                        P.op("pool", lambda e: e.tensor_tensor(out=Qhb[:], in0=qa[:], in1=dd[:], op=ALU.mult), reads=[qa, dd], writes=[Qhb])

                        hx = Hexp[rc]
                        hcur = [Hbf[0]]
                        hring = Ring(Hbf)
                        hring.i = 1
                        P.op("act", lambda e: e.activation(out=Hbf[0][:], in_=hx[:], func=AF.Copy), reads=[hx], writes=[Hbf[0]])
                        for sub in range(NDIAG):
                            tsl = slice(128 * sub, 128 * sub + 128)
                            transpose_to(Vdtm[:], vpb[:, tsl], Vdtm, vpb)
                            transpose_to(Ptm[:], PR4[:, sub, 0, :], Ptm, PRb)
                            transpose_to(Qhtm[:], Qhb[:, tsl], Qhtm, Qhb)
                            transpose_to(Khtm[:], Khb[:, tsl], Khtm, Khb)
                            P.op("pool", lambda e: e.tensor_tensor(
                                out=PRm[:], in0=PRb[:, sub, :].unsqueeze(1).broadcast_to([128, 2, 256]),
                                in1=hm64.unsqueeze(2).broadcast_to([128, 2, 256]), op=ALU.mult), reads=[PRb, cstb], writes=[PRm])
                            P.op("pool", lambda e: e.tensor_tensor(
                                out=Rm[:], in0=PR4[:, sub, 1, :].unsqueeze(1).broadcast_to([128, 2, 128]),
                                in1=cstb[:, C_TM2:C_TM2 + 256].rearrange("p (j t) -> p j t", j=2), op=ALU.mult),
                                reads=[PRb, cstb], writes=[Rm])
                            P.op("pool", lambda e: e.tensor_tensor(
                                out=Vmj[:], in0=Vdtm[:].unsqueeze(1).broadcast_to([128, 2, 128]),
                                in1=hm64.unsqueeze(2).broadcast_to([128, 2, 128]), op=ALU.mult), reads=[Vdtm, cstb], writes=[Vmj])
                            wcur = 0
                            for hh in range(2):
                                AM = AMb[hh]
                                mps = pring.get()
                                P.op("pe", lambda e: e.matmul(mps[:, 0:256], lhsT=Qtb[:, tsl], rhs=PRm[:, hh, :], start=True, stop=True),
                                     reads=[Qtb, PRm], writes=[mps])
                                P.op("pe", lambda e: e.matmul(mps[:, 256:512], lhsT=Ktb[:, tsl], rhs=PRm[:, hh, :], start=True, stop=True),
                                     reads=[Ktb, PRm], writes=[mps])
                                P.op("dve", lambda e: e.tensor_tensor(
                                    out=AM[:], in0=mps[:].rearrange("p (a b) -> p a b", a=2),
                                    in1=cstb[:, C_ST64:C_ST64 + 256].unsqueeze(1).broadcast_to([128, 2, 256]), op=ALU.mult),
                                    reads=[mps, cstb], writes=[AM])
                                lps = pring.get()
                                P.op("pe", lambda e: e.matmul(lps[:, 0:128], lhsT=PRm[:, hh, 0:128], rhs=Qtb[:, tsl], start=True, stop=True),
                                     reads=[Qtb, PRm], writes=[lps])
                                Ncur = Nb[2]
                                P.op("dve", lambda e: e.tensor_tensor(out=Ncur[:], in0=lps[:, 0:128], in1=cstb[:, C_LO64:C_LO64 + 128],
                                                                      op=ALU.mult), reads=[lps, cstb], writes=[Ncur])
                                Mcur_ap, Mcur_key = AM[:, 0, 0:128], AM
                                W0 = Wb[0]
                                avp = pring.get()
                                P.op("pe", lambda e: e.matmul(avp[:, 0:64], lhsT=AM[:, 1, 0:128], rhs=Vdtm[:, 64 * hh:64 * hh + 64],
                                                              start=True, stop=True), reads=[AM, Vdtm], writes=[avp])
                                P.op("act", lambda e: e.activation(out=W0[:, hh, 64:128], in_=avp[:, 0:64], func=AF.Copy),
                                     reads=[avp], writes=[(W0.name, hh)])
                                P.op("pool", lambda e: e.tensor_copy(out=W0[:, hh, 0:64], in_=Ptm[:, 64 * hh:64 * hh + 64]),
                                     reads=[Ptm], writes=[(W0.name, hh)])
                                wi = 0
                                for i in range(6):
                                    Wc, Wn = Wb[wi], Wb[1 - wi]
                                    ups = pring.get()
                                    P.op("pe", lambda e: e.matmul(ups[:, 0:128], lhsT=Mcur_ap, rhs=Wc[:, hh, :], start=True, stop=True),
                                         reads=[Mcur_key, (Wc.name, hh)], writes=[ups])
                                    P.op("dve", lambda e: e.tensor_tensor(out=Wn[:, hh, :], in0=ups[:, 0:128], in1=Wc[:, hh, :], op=ALU.add),
                                         reads=[ups, (Wc.name, hh)], writes=[(Wn.name, hh)])
                                    wi = 1 - wi
                                    if i < 5:
                                        Mn, Nn = Mb[i % 2], Nb[i % 2]
                                        m2 = pring.get()
                                        P.op("pe", lambda e: e.matmul(m2[:, 0:128], lhsT=Ncur[:], rhs=Mcur_ap, start=True, stop=True),
                                             reads=[Ncur, Mcur_key], writes=[m2])
                                        P.op("pe", lambda e: e.matmul(m2[:, 128:256], lhsT=Mcur_ap, rhs=Ncur[:], start=True, stop=True),
                                             reads=[Ncur, Mcur_key], writes=[m2])
                                        P.op("act", lambda e: e.activation(out=Mn[:], in_=m2[:, 0:128], func=AF.Copy), reads=[m2], writes=[Mn])
                                        P.op("act", lambda e: e.activation(out=Nn[:], in_=m2[:, 128:256], func=AF.Copy), reads=[m2], writes=[Nn])
                                        Mcur_ap, Mcur_key, Ncur = Mn[:], Mn, Nn
                                wcur = wi
                            Wf = Wb[wcur]
                            P.op("pool", lambda e: e.tensor_copy(out=PH[:].rearrange("p (h k) -> p h k", h=2), in_=Wf[:, :, 0:64]),
                                 reads=[(Wf.name, 0), (Wf.name, 1)], writes=[PH])
                            transpose_to(PHT[:], PH[:], PHT, PH)
                            P.op("pool", lambda e: e.tensor_tensor(
                                out=Uhm[:].rearrange("p j (h v) -> p j h v", h=2),
                                in0=Wf[:, :, 64:128].unsqueeze(1).broadcast_to([128, 2, 2, 64]),
                                in1=hm64.unsqueeze(2).unsqueeze(3).broadcast_to([128, 2, 2, 64]), op=ALU.mult),
                                reads=[(Wf.name, 0), (Wf.name, 1), cstb], writes=[Uhm])
                            hstart = []
                            for j in range(2):
                                hb_c = hcur[0]
                                hstart.append(hb_c)
                                ups = pring.get()
                                P.op("pe", lambda e: e.matmul(ups[:, 0:128], lhsT=PHT[:], rhs=hb_c[:], start=True, stop=True),
                                     reads=[PHT, hb_c], writes=[ups])
                                P.op("dve", lambda e: e.scalar_tensor_tensor(
                                    out=Umb[:, j, :], in0=ups[:, 0:128], scalar=hm64[:, j:j + 1], in1=Uhm[:, j, :],
                                    op0=ALU.mult, op1=ALU.add), reads=[ups, Uhm, cstb], writes=[("Umb", j)])
                                hps = pring.get()
                                P.op("pe", lambda e: e.matmul(hps[:, 0:128], lhsT=Qhtm[:], rhs=Umb[:, j, :], start=True, stop=False),
                                     reads=[Qhtm, ("Umb", j)], writes=[hps])
                                P.op("pe", lambda e: e.matmul(hps[:, 0:128], lhsT=Khtm[:], rhs=Vmj[:, j, :], start=False, stop=True),
                                     reads=[Khtm, Vmj], writes=[hps])
                                htmp = F[14]
                                P.op("dve", lambda e: e.tensor_tensor(out=htmp[:, 0:128], in0=hps[:, 0:128],
                                                                      in1=cstb[:, C_HM64B:C_HM64B + 128], op=ALU.mult),
                                     reads=[hps, cstb], writes=[htmp])
                                ci = 2 * sub + j
                                P.op("dve", lambda e: e.scalar_tensor_tensor(
                                    out=hx[:], in0=hx[:], scalar=g4[:, ci:ci + 1], in1=htmp[:, 0:128], op0=ALU.mult, op1=ALU.add),
                                    reads=[hx, g4, htmp], writes=[hx])
                                hb_n = hring.get()
                                P.op("act", lambda e: e.activation(out=hb_n[:], in_=hx[:], func=AF.Copy), reads=[hx], writes=[hb_n])
                                hcur[0] = hb_n
                            P.op("pool", lambda e: e.tensor_tensor(out=Ucomb[:], in0=Umb[:, 0, :], in1=Umb[:, 1, :], op=ALU.add),
                                 reads=[("Umb", 0), ("Umb", 1)], writes=[Ucomb])
                            yps = pring.get()
                            for j in range(2):
                                hs_ = hstart[j]
                                P.op("pe", lambda e: e.matmul(yps[:, 0:128], lhsT=Rm[:, j, :], rhs=hs_[:], start=(j == 0), stop=False),
                                     reads=[Rm, hs_], writes=[yps])
                            for hh in range(2):
                                AM = AMb[hh]
                                P.op("pe", lambda e: e.matmul(yps[:, 64 * hh:64 * hh + 64], lhsT=AM[:, 0, 128:256],
                                                              rhs=Ucomb[:, 64 * hh:64 * hh + 64], start=False, stop=False),
                                     reads=[AM, Ucomb], writes=[yps])
                                P.op("pe", lambda e: e.matmul(yps[:, 64 * hh:64 * hh + 64], lhsT=AM[:, 1, 128:256],
                                                              rhs=Vdtm[:, 64 * hh:64 * hh + 64], start=False, stop=(hh == 1)),
                                     reads=[AM, Vdtm], writes=[yps])
                            y3 = ysb[:].rearrange("p (h v) -> p h v", h=2)
                            sqy = F[15]
                            P.op("act", lambda e: e.activation(out=ysb[:], in_=yps[:, 0:128], func=AF.Copy), reads=[yps], writes=[ysb])
                            P.op("act", lambda e: e.activation(out=sqy[:, 0:128], in_=yps[:, 0:128], func=AF.Square), reads=[yps], writes=[sqy])
                            P.op("dve", lambda e: e.tensor_reduce(out=lnst[:, 0:2], in_=y3, axis=AX.X, op=ALU.add), reads=[ysb], writes=[lnst])
                            P.op("dve", lambda e: e.tensor_reduce(out=lnst[:, 2:4], in_=sqy[:, 0:128].rearrange("p (h v) -> p h v", h=2),
                                                                  axis=AX.X, op=ALU.add), reads=[sqy], writes=[lnst])
                            P.op("dve", lambda e: e.tensor_scalar(out=lnst[:, 4:6], in0=lnst[:, 0:2], scalar1=1.0 / 64.0, scalar2=None,
                                                                  op0=ALU.mult), reads=[lnst], writes=[lnst])
                            P.op("dve", lambda e: e.tensor_tensor(out=lnst[:, 6:8], in0=lnst[:, 4:6], in1=lnst[:, 4:6], op=ALU.mult),
                                 reads=[lnst], writes=[lnst])
                            P.op("dve", lambda e: e.scalar_tensor_tensor(out=lnst[:, 8:10], in0=lnst[:, 2:4], scalar=1.0 / 64.0,
                                                                         in1=lnst[:, 6:8], op0=ALU.mult, op1=ALU.subtract),
                                 reads=[lnst], writes=[lnst])
                            P.op("act", lambda e: e.activation(out=lnst[:, 10:12], in_=lnst[:, 8:10], func=AF.Sqrt, bias=64e-5, scale=1.0),
                                 reads=[lnst], writes=[lnst])
                            P.op("dve", lambda e: e.reciprocal(out=lnst[:, 12:14], in_=lnst[:, 10:12]), reads=[lnst], writes=[lnst])
                            P.op("dve", lambda e: e.tensor_tensor(out=y3, in0=y3, in1=lnst[:, 4:6].unsqueeze(2).broadcast_to([128, 2, 64]),
                                                                  op=ALU.subtract), reads=[ysb, lnst], writes=[ysb])
                            P.op("dve", lambda e: e.tensor_tensor(out=ynb[:].rearrange("p (h v) -> p h v", h=2), in0=y3,
                                                                  in1=lnst[:, 12:14].unsqueeze(2).broadcast_to([128, 2, 64]), op=ALU.mult),
                                 reads=[ysb, lnst], writes=[ynb])
                            tp = pring.get()
                            tpb = tp[:].bitcast(BF16)
                            P.op("pe", lambda e: e.transpose(tpb[:, 0:128], ynb[:], ident), reads=[ynb, cstb], writes=[tp])
                            o1 = F[15]
                            P.op("dve", lambda e: e.tensor_scalar(out=o1[:, 0:128], in0=tpb[:, 0:128], scalar1=vcol(V_LNG + rc),
                                                                  scalar2=vcol(V_LNB + rc), op0=ALU.mult, op1=ALU.add),
                                 reads=[tp, vecs], writes=[o1])
                            P.op("pool", lambda e: e.tensor_tensor(out=o1[:, 0:128], in0=o1[:, 0:128], in1=bonus[:, tsl], op=ALU.add),
                                 reads=[o1, bonus], writes=[o1])
                            P.op("dve", lambda e: e.tensor_tensor(out=mixB[:, 4 + rc, tsl], in0=o1[:, 0:128], in1=gT[:, tsl], op=ALU.mult),
                                 reads=[o1, gT], writes=[("mixB", 4 + rc)])

                if phase == "b" and "D" in mixers:
                    rwkv()
                for c2 in range(6):
                    mixer = "BBCCDD"[c2]
                    if phase == "b" and mixer not in mixers:
                        P.op("pool", lambda e, c2=c2: e.memset(mixB[:, c2, :], 0.0), writes=[("mixB", c2)])

                if debug and phase == "a":
                    for h in range(4):
                        dt_ = F[9]
                        P.op("dve", lambda e, h=h: e.tensor_copy(out=dt_[0:64, :], in_=mixA[0:64, h, :]),
                             reads=[("mixA", h)], writes=[dt_])
                        P.dma("sp", lambda e, h=h, c0=c0: e.dma_start(out=dbg[64 * h:64 * h + 64, c0:c0 + TT],
                                                                       in_=dt_[0:64, :]), reads=[dt_])
                if debug and phase == "b":
                    for c2 in range(6 if "D" in mixers else 4):
                        dt_ = F[9]
                        P.op("dve", lambda e, c2=c2: e.tensor_copy(out=dt_[:], in_=mixB[:, c2, :]),
                             reads=[("mixB", c2)], writes=[dt_])
                        P.dma("sp", lambda e, c2=c2, c0=c0: e.dma_start(
                            out=dbg[256 + 128 * c2:256 + 128 * (c2 + 1), c0:c0 + TT], in_=dt_[:]), reads=[dt_])

                for dc in range(8):
                    ps = pring.get()
                    if phase == "a":
                        for h in range(4):
                            P.op("pe", lambda e, ps=ps, h=h, dc=dc: e.matmul(
                                ps[:, 0:TT], lhsT=woA[0:64, h, 128 * dc:128 * (dc + 1)], rhs=mixA[0:64, h, :],
                                start=(h == 0), stop=(h == 3)), reads=["wo", ("mixA", h)], writes=[ps])
                        res_ap, res_key = xs[:, dc, :], ("xs", dc)
                    else:
                        for c2 in range(6):
                            P.op("pe", lambda e, ps=ps, c2=c2, dc=dc: e.matmul(
                                ps[:, 0:TT], lhsT=woB[:, c2, 128 * dc:128 * (dc + 1)], rhs=mixB[:, c2, :],
                                start=(c2 == 0), stop=(c2 == 5)), reads=["wo", ("mixB", c2)], writes=[ps])
                        xm = F[12 + dc % 2]
                        P.dma("sp", lambda e, dc=dc, c0=c0, xm=xm, x_mid=x_mid: e.dma_start(
                            out=xm[:], in_=x_mid[128 * dc:128 * (dc + 1), c0:c0 + TT]), reads=[("xmid", t, dc)], writes=[xm])
                        res_ap, res_key = xm[:], xm
                    xo = F[10 + dc % 2]
                    P.op("dve", lambda e, ps=ps, res_ap=res_ap, xo=xo: e.tensor_tensor(
                        out=xo[:], in0=ps[:, 0:TT], in1=res_ap, op=ALU.add), reads=[ps, res_key], writes=[xo])
                    P.dma("sp", lambda e, dc=dc, c0=c0, xo=xo, x_mid=x_mid: e.dma_start(
                        out=x_mid[128 * dc:128 * (dc + 1), c0:c0 + TT], in_=xo[:]), reads=[xo], writes=[("xmid", t, dc)])

        barrier()
        if not do_mlp:
            for t in range(NT):
                c0 = t * TT
                for k in range(8):
                    P.dma("sp", lambda e, k=k, c0=c0: e.dma_start(out=xs[:, k, :], in_=x_mid[128 * k:128 * (k + 1), c0:c0 + TT]),
                          writes=[("xs", k)])
                    P.dma("sp", lambda e, k=k, c0=c0, x_dst=x_dst: e.dma_start(
                        out=x_dst[128 * k:128 * (k + 1), c0:c0 + TT], in_=xs[:, k, :]), reads=[("xs", k)])
            x_src = x_dst
            continue
        T2 = TT
        for cb in range(0, DFF, 512):
            P.dma("pool", lambda e, li=li, cb=cb: e.dma_start(
                out=wup[:, :, cb:cb + 512], in_=w_up_d[li, :, cb:cb + 512].rearrange("(k p) n -> p k n", p=128)),
                writes=["wup"])
        for kb in range(0, 32, 4):
            P.dma("pool", lambda e, li=li, kb=kb: e.dma_start(
                out=wdn[:, kb:kb + 4, :], in_=w_dn_d[li, 128 * kb:128 * (kb + 4), :].rearrange("(k p) n -> p k n", p=128)),
                writes=["wdn"])
        for t in range(S // T2):
            c0 = t * T2
            rmsnorm_tile(x_mid, c0, T2, V_GMLP, FM[7])
            hT_keys = [("hT", k) for k in range(8)]
            for fc in range(32):
                ps = pring.get()
                for k in range(8):
                    P.op("pe", lambda e, k=k, ps=ps, fc=fc: e.matmul(
                        ps[:, 0:T2], lhsT=wup[:, k, 128 * fc:128 * (fc + 1)], rhs=hT[:, k, 0:T2],
                        start=(k == 0), stop=(k == 7)), reads=["wup"] + hT_keys, writes=[ps])
                r_ = FM[fc % 2]
                P.op("act", lambda e, ps=ps, r_=r_: e.activation(out=r_[:, 0:T2], in_=ps[:, 0:T2], func=AF.Relu),
                     reads=[ps], writes=[r_])
                P.op("dve", lambda e, fc=fc, r_=r_: e.tensor_tensor(out=uT[:, fc, :], in0=r_[:, 0:T2], in1=r_[:, 0:T2],
                                                                  op=ALU.mult), reads=[r_], writes=[("uT", fc)])
            for dc in range(8):
                ps = pring.get()
                for fc in range(32):
                    P.op("pe", lambda e, ps=ps, fc=fc, dc=dc: e.matmul(
                        ps[:, 0:T2], lhsT=wdn[:, fc, 128 * dc:128 * (dc + 1)], rhs=uT[:, fc, :],
                        start=(fc == 0), stop=(fc == 31)), reads=["wdn", ("uT", fc)], writes=[ps])
                xo = FM[2 + dc % 2]
                P.op("dve", lambda e, ps=ps, dc=dc, xo=xo: e.tensor_tensor(
                    out=xo[:, 0:T2], in0=ps[:, 0:T2], in1=xs[:, dc, 0:T2], op=ALU.add),
                    reads=[ps, ("xs", dc)], writes=[xo])
                P.dma("sp", lambda e, dc=dc, c0=c0, xo=xo, x_dst=x_dst: e.dma_start(
                    out=x_dst[128 * dc:128 * (dc + 1), c0:c0 + T2], in_=xo[:, 0:T2]), reads=[xo], writes=["xdst"])
        x_src = x_dst
    P.finalize()
    return nc, P


def make_inputs(inp, core, S, layer_ids):
    L = len(layer_ids)
    vs, rs, sws = [], [], []
    for l in layer_ids:
        v, r, sw = pack_layer_params(inp, l)
        vs.append(v); rs.append(r); sws.append(sw)
    lbl = np.ascontiguousarray(np.asarray(inp["hgrn_lb_logits"], np.float32).T.reshape(2, 128, -1).transpose(1, 0, 2).reshape(128, -1))
    m = {
        "xT": np.ascontiguousarray(np.asarray(inp["x"][core, :S], np.float32).T),
        "w_in": np.ascontiguousarray(inp["w_in"][layer_ids]),
        "w_out": np.ascontiguousarray(inp["w_out"][layer_ids]),
        "w_up": np.ascontiguousarray(inp["w_mlp_up"][layer_ids]),
        "w_dn": np.ascontiguousarray(inp["w_mlp_down"][layer_ids]),
        "vecs": np.stack(vs), "rows": np.stack(rs), "smallw": np.stack(sws),
        "lbl": lbl, "consts": make_consts(),
    }
    return m


FUSED = True
N_CORES = 8
SEQ = 8192
DEPTH = 4


def kernel(**inputs):
    inp = {k: np.asarray(v) for k, v in inputs.items()}
    L = DEPTH
    if FUSED:
        nc, _ = build_program(SEQ, L, L, list(range(L)))
        base = make_inputs(inp, 0, SEQ, list(range(L)))
        in_maps = []
        for c in range(N_CORES):
            m = dict(base)
            m["xT"] = np.ascontiguousarray(np.asarray(inp["x"][c], np.float32).T)
            in_maps.append(m)
        res = run_bass_kernel_spmd(nc, in_maps, core_ids=list(range(N_CORES)))
        outs = [res.results[c]["yT"] for c in range(N_CORES)]
    else:
        xT = [np.ascontiguousarray(np.asarray(inp["x"][c], np.float32).T) for c in range(N_CORES)]
        for l in range(L):
            nc, _ = build_program(SEQ, 1, L, [l])
            base = make_inputs(inp, 0, SEQ, [l])
            in_maps = []
            for c in range(N_CORES):
                m = dict(base)
                m["xT"] = xT[c]
                in_maps.append(m)
            res = run_bass_kernel_spmd(nc, in_maps, core_ids=list(range(N_CORES)))
            xT = [np.ascontiguousarray(res.results[c]["yT"]) for c in range(N_CORES)]
        outs = xT
    out = np.stack([np.asarray(o, np.float32).T for o in outs])
    return np.ascontiguousarray(out)
```

```python
import numpy as np
from contextlib import ExitStack
import concourse.bass as bass
import concourse.mybir as mybir

F32 = mybir.dt.float32
BF16 = mybir.dt.bfloat16
I32 = mybir.dt.int32
ALU = mybir.AluOpType
AF = mybir.ActivationFunctionType
AX = mybir.AxisListType

ENGS = ("pe", "act", "dve", "pool", "sp")
N_DMA_SEMS = 24


import types


def freeze(fn):
    if fn is None or fn.__closure__ is None:
        return fn
    cells = []
    for c in fn.__closure__:
        try:
            cells.append(types.CellType(c.cell_contents))
        except ValueError:
            cells.append(c)
    return types.FunctionType(fn.__code__, fn.__globals__, fn.__name__, fn.__defaults__, tuple(cells))


class Prog:
    def __init__(self, nc):
        self.nc = nc
        self.es = ExitStack()
        self.ops = {e: [] for e in ENGS}
        self.cnt = {e: 0 for e in ENGS}
        self.st = {}
        self.dma_tot = [0] * N_DMA_SEMS
        self.dma_rr = 0
        self.waited = {e: {} for e in ENGS}
        self.dma_events = []
        self.n_t = 0

    def sb(self, shape, dt, name=None):
        self.n_t += 1
        return self.es.enter_context(self.nc.sbuf_tensor(name or f"t{self.n_t}", list(shape), dt))

    def ps(self, shape, dt, name=None):
        self.n_t += 1
        return self.es.enter_context(self.nc.psum_tensor(name or f"p{self.n_t}", list(shape), dt))

    @staticmethod
    def K(k):
        if isinstance(k, (str, int)):
            return k
        if isinstance(k, tuple):
            return tuple(Prog.K(x) for x in k)
        return k.name

    def _deps(self, eng, reads, writes, is_dma):
        reads = [self.K(k) for k in reads]
        writes = [self.K(k) for k in writes]
        deps = []
        for k in reads:
            s = self.st.get(k)
            if s and s[0] is not None:
                deps.append((s[0], "raw"))
        for k in writes:
            s = self.st.get(k)
            if s:
                if s[0] is not None:
                    deps.append((s[0], "waw"))
                for r in s[1]:
                    deps.append((r, "war"))
        waits = []
        for ev, kind in deps:
            if ev[0] == "eng":
                _, e2, n = ev
                if e2 == eng and not is_dma:
                    if kind != "raw" or eng in ("pe", "sp"):
                        continue
                semkey = ("eng", e2)
                val = n
            else:
                _, si, tot = ev
                semkey = ("dma", si)
                val = tot
            if self.waited[eng].get(semkey, 0) >= val:
                continue
            self.waited[eng][semkey] = val
            waits.append((semkey, val))
        return waits

    def _commit(self, ev, reads, writes):
        reads = [self.K(k) for k in reads]
        writes = [self.K(k) for k in writes]
        for k in reads:
            s = self.st.setdefault(k, [None, []])
            s[1].append(ev)
        for k in writes:
            self.st[k] = [ev, []]

    def op(self, eng, fn, reads=(), writes=()):
        waits = self._deps(eng, reads, writes, False)
        self.cnt[eng] += 1
        ev = ("eng", eng, self.cnt[eng])
        self.ops[eng].append(dict(fn=freeze(fn), waits=waits, ev=ev))
        self._commit(ev, reads, writes)
        return ev

    def dma(self, eng, fn, reads=(), writes=()):
        si = self.dma_rr
        self.dma_rr = (self.dma_rr + 1) % N_DMA_SEMS
        waits = self._deps(eng, reads, writes, True)
        semkey = ("dma", si)
        prev = self.dma_tot[si]
        if prev > 0 and self.waited[eng].get(semkey, 0) < prev:
            self.waited[eng][semkey] = prev
            waits.append((semkey, prev))
        self.dma_tot[si] += 16
        ev = ("dma", si, self.dma_tot[si])
        self.ops[eng].append(dict(fn=freeze(fn), waits=waits, ev=ev))
        self._commit(ev, reads, writes)
        self.dma_events.append(ev)
        return ev

    def wait_events(self, eng, events):
        waits = []
        for ev in events:
            if ev[0] == "eng":
                semkey, val = ("eng", ev[1]), ev[2]
            else:
                semkey, val = ("dma", ev[1]), ev[2]
            if self.waited[eng].get(semkey, 0) >= val:
                continue
            self.waited[eng][semkey] = val
            waits.append((semkey, val))
        self.ops[eng].append(dict(fn=None, waits=waits, ev=None))

    def finalize(self):
        nc = self.nc
        fin = [("dma", si, self.dma_tot[si]) for si in range(N_DMA_SEMS) if self.dma_tot[si] > 0]
        self.wait_events("sp", fin)
        needed = {e: set() for e in ENGS}
        for e in ENGS:
            for o in self.ops[e]:
                for (semkey, val) in o["waits"]:
                    if semkey[0] == "eng":
                        needed[semkey[1]].add(val)
        rank = {}
        for e in ENGS:
            for i, v in enumerate(sorted(needed[e])):
                rank[(e, v)] = i + 1
        sems = {}
        for e in ENGS:
            sems[("eng", e)] = self.es.enter_context(nc.semaphore(f"s_{e}"))
        for si in range(N_DMA_SEMS):
            sems[("dma", si)] = self.es.enter_context(nc.semaphore(f"s_dma{si}"))
        handles = dict(pe="tensor", act="scalar", dve="vector", pool="gpsimd", sp="sync")
        self.n_inst = 0

        def emit_engine(e, eng):
            for o in self.ops[e]:
                for (semkey, val) in o["waits"]:
                    if semkey[0] == "eng":
                        val = rank[(semkey[1], val)]
                    eng.wait_ge(sems[semkey], val)
                    self.n_inst += 1
                if o["fn"] is None:
                    continue
                ins = o["fn"](eng)
                self.n_inst += 1
                ev = o["ev"]
                if ev[0] == "dma":
                    ins.then_inc(sems[("dma", ev[1])], 16)
                elif (e, ev[2]) in rank:
                    ins.then_inc(sems[("eng", e)], 1)

        with nc.Block() as block:
            @block.tensor
            def _(eng):
                emit_engine("pe", eng)

            @block.scalar
            def _(eng):
                emit_engine("act", eng)

            @block.vector
            def _(eng):
                emit_engine("dve", eng)

            @block.gpsimd
            def _(eng):
                emit_engine("pool", eng)

            @block.sync
            def _(eng):
                emit_engine("sp", eng)
        self.es.close()


import math
import numpy as np
import concourse.bass as bass
import concourse.mybir as mybir
from concourse.bass_utils import run_bass_kernel_spmd

D = 1024
PIN = 3472
DFF = 4096
TT = 256
NDIAG = TT // 128
RMS_EPS = 1e-6
SLOPES = [2.0 ** (-8.0 * (i + 1) / 4) for i in range(4)]
ND = 66

C_ID = 0
C_BD32 = 128
C_ST64 = 256
C_IN64 = 384
C_LO64 = 512
C_RS32 = 640
C_RS64 = 1152
C_CAUS = 1664
C_TM4 = 2176
C_CM = 2688
C_HM32 = 3712
C_HM64 = 3716
C_HM64B = 3718
C_TM2 = 3846
C_B0 = 4102
C_BH = C_B0 + 5
NCONST = C_BH + 3 * ND


def make_consts():
    c = np.zeros((128, NCONST), np.float32)
    p = np.arange(128)
    c[:, C_ID:C_ID + 128] = np.eye(128)
    same32 = (p[:, None] // 32) == (p[None, :] // 32)
    same64 = (p[:, None] // 64) == (p[None, :] // 64)
    c[:, C_BD32:C_BD32 + 128] = same32 & (p[:, None] <= p[None, :])
    c[:, C_ST64:C_ST64 + 128] = same64 & (p[:, None] < p[None, :])
    c[:, C_IN64:C_IN64 + 128] = same64 & (p[:, None] <= p[None, :])
    c[:, C_LO64:C_LO64 + 128] = same64 & (p[:, None] > p[None, :])
    t = np.arange(512)
    c[:, C_RS32:C_RS32 + 512] = (t % 32 != 0)[None, :]
    c[:, C_RS64:C_RS64 + 512] = (t % 64 != 0)[None, :]
    c[:, C_CAUS:C_CAUS + 512] = t[None, :] >= p[:, None]
    tok = np.arange(128)
    for cc in range(4):
        c[:, C_TM4 + 128 * cc:C_TM4 + 128 * (cc + 1)] = (tok // 32 == cc)[None, :]
    col = np.arange(256)
    for h in range(4):
        c[:, C_CM + 256 * h:C_CM + 256 * (h + 1)] = (col // 64 == h)[None, :]
        c[:, C_HM32 + h] = (p // 32 == h)
    for h in range(2):
        c[:, C_HM64 + h] = (p // 64 == h)
    c[:, C_HM64B:C_HM64B + 128] = (p[:, None] // 64) == (tok[None, :] // 64)
    for j in range(2):
        c[:, C_TM2 + 128 * j:C_TM2 + 128 * (j + 1)] = (tok // 64 == j)[None, :]
    for d in range(5):
        c[:, C_B0 + d] = SLOPES[0] * (p - 127 - 128 * d)
    for h in range(1, 4):
        for di in range(ND):
            delta = di - (NDIAG - 1)
            c[:, C_BH + (h - 1) * ND + di] = SLOPES[h] * (p - (TT - 1) - 128 * delta)
    return c


V_GMIX = 0
V_GMLP = 8
V_GQ = 16
V_GK = 17
V_GA = 18
V_GLAB = 19
V_MU = 20
V_W0 = 29
V_A0 = 31
V_KK = 33
V_KA = 35
V_RK = 37
V_LNG = 39
V_LNB = 41
NV = 43
R_LAM = 0
R_GB = 128
R_GC = 384
NR = 640
SW_GU = 0
SW_WU = 128
SW_AU = 384
SW_GUP = 640
NSW = 896


def pack_layer_params(inp, l):
    g = lambda n: np.asarray(inp[n][l], np.float32)
    v = np.zeros((128, NV), np.float32)
    v[:, V_GMIX:V_GMIX + 8] = g("norm_mix_g").reshape(8, 128).T
    v[:, V_GMLP:V_GMLP + 8] = g("norm_mlp_g").reshape(8, 128).T
    v[:, V_GQ] = np.tile(g("da_q_norm_g"), 4)
    v[:, V_GK] = np.tile(g("da_k_norm_g"), 4)
    v[:, V_GA] = np.tile(g("da_out_norm_g"), 2)
    v[:, V_GLAB] = g("gla_gate_b")
    mu = g("rw_shift_mu")
    for i in range(6):
        v[:, V_MU + i] = mu[128 * i:128 * (i + 1)]
    v[:32, V_MU + 6] = mu[768:800]
    v[:32, V_MU + 7] = mu[800:832]
    v[:64, V_MU + 8] = mu[832:896]
    for name, col in (("rw_w0", V_W0), ("rw_a0", V_A0), ("rw_k_k", V_KK), ("rw_k_a", V_KA),
                      ("rw_ln_g", V_LNG), ("rw_ln_b", V_LNB)):
        v[:, col:col + 2] = g(name).reshape(2, 128).T
    v[:, V_RK:V_RK + 2] = g("rw_r_k").reshape(2, 128).T
    r = np.zeros((128, NR), np.float32)
    lam = np.concatenate([g("da_lambda_q1"), g("da_lambda_k1"), g("da_lambda_q2"), g("da_lambda_k2")])
    r[:, R_LAM:R_LAM + 128] = lam[None, :]
    r[:, R_GB:R_GB + 256] = np.tile(g("gla_out_norm_g"), 4)[None, :]
    r[:, R_GC:R_GC + 256] = np.tile(g("hgrn_out_norm_g"), 4)[None, :]
    sw = np.zeros((64, NSW), np.float32)
    sw[:16, SW_GU:SW_GU + 128] = g("gla_gate_up")
    sw[:32, SW_WU:SW_WU + 256] = g("rw_w_up")
    sw[:32, SW_AU:SW_AU + 256] = g("rw_a_up")
    sw[:64, SW_GUP:SW_GUP + 256] = g("rw_g_up")
    return v, r, sw


class Ring:
    def __init__(self, items):
        self.items = items
        self.i = 0

    def get(self):
        x = self.items[self.i]
        self.i = (self.i + 1) % len(self.items)
        return x


class View:
    def __init__(self, ap, name):
        self.ap = ap
        self.name = name

    def __getitem__(self, k):
        return self.ap[k]


def build_program(S, n_layers, L_total, layer_ids, debug=False, mixers="ABCD", do_mlp=True):
    NT = S // TT
    nc = bass.Bass("TRN2", target_bir_lowering=False)
    dt_in = lambda name, shape: nc.dram_tensor(name, list(shape), F32, kind="ExternalInput").ap()
    xT_in = dt_in("xT", [D, S])
    w_in_d = dt_in("w_in", [n_layers, D, PIN])
    w_out_d = dt_in("w_out", [n_layers, D, D])
    w_up_d = dt_in("w_up", [n_layers, D, DFF])
    w_dn_d = dt_in("w_dn", [n_layers, DFF, D])
    vecs_d = dt_in("vecs", [n_layers, 128, NV])
    rows_d = dt_in("rows", [n_layers, 128, NR])
    sw_d = dt_in("smallw", [n_layers, 64, NSW])
    lbl_d = dt_in("lbl", [128, 2 * L_total])
    consts_d = dt_in("consts", [128, NCONST])
    yT = nc.dram_tensor("yT", [D, S], F32, kind="ExternalOutput").ap()
    xa = nc.dram_tensor("xa", [D, S], F32, kind="Internal").ap()
    xb = nc.dram_tensor("xb", [D, S], F32, kind="Internal").ap()
    dbg = nc.dram_tensor("dbg", [D, S], F32, kind="ExternalOutput").ap() if debug else None

    P = Prog(nc)
    cstb = P.sb([128, C_B0], BF16, "cstb")
    for cb in range(0, C_B0, 1024):
        ce = min(C_B0, cb + 1024)
        P.dma("pool", lambda e, cb=cb, ce=ce: e.dma_start(out=cstb[:, cb:ce], in_=consts_d[:, cb:ce]), writes=[cstb])
    biasc = P.sb([128, NCONST - C_B0], F32, "biasc")
    P.dma("sp", lambda e: e.dma_start(out=biasc[:], in_=consts_d[:, C_B0:NCONST]), writes=[biasc])
    ident = cstb[:, C_ID:C_ID + 128]
    caus = cstb[:, C_CAUS:C_CAUS + 512]
    ones_bf = P.sb([128, 128], BF16, "ones_bf")
    P.op("pool", lambda e: e.memset(ones_bf[:], 1.0), writes=[ones_bf])
    bo32 = P.sb([128, 128], BF16, "bo32")
    bo64 = P.sb([128, 128], BF16, "bo64")
    P.op("pool", lambda e: e.memset(bo32[:], 0.0), writes=[bo32])
    P.op("pool", lambda e: e.memset(bo64[:], 0.0), writes=[bo64])
    for b in range(4):
        P.op("pool", lambda e, b=b: e.memset(bo32[32 * b:32 * b + 32, 32 * b:32 * b + 32], 1.0), writes=[bo32])
    for b in range(2):
        P.op("pool", lambda e, b=b: e.memset(bo64[64 * b:64 * b + 64, 64 * b:64 * b + 64], 1.0), writes=[bo64])
    Esel = P.sb([128, 64], F32, "Esel")
    P.op("pool", lambda e: e.memset(Esel[:], 0.0), writes=[Esel])
    P.op("pool", lambda e: e.memset(Esel[64:65, :], 1.0), writes=[Esel])

    NKT = S // 128
    R1_EL = 65536
    R1 = P.sb([128, R1_EL], BF16, "R1")
    winA = R1[:, 0:6144].rearrange("p (k n) -> p k n", k=8)
    woA = R1[:, 6144:10240].rearrange("p (k n) -> p k n", k=4)
    o = 10240
    KT = R1[:, o:o + 2 * S].rearrange("p (c s) -> p c s", c=2); o += 2 * S
    Vc = R1[:, o:o + NKT * 260].rearrange("p (t h v) -> p t h v", h=4, v=65); o += NKT * 260
    assert o <= R1_EL
    winB = R1[:, 0:8 * PIN].rearrange("p (k n) -> p k n", k=8)
    woB = R1[:, 8 * PIN:8 * PIN + 6 * D].rearrange("p (k n) -> p k n", k=6)
    carve_o = [8 * PIN + 6 * D]

    def carve(shape, dt, name):
        n = int(np.prod(shape[1:]))
        nb = n if dt == BF16 else 2 * n
        a = carve_o[0]
        carve_o[0] += nb + (nb % 2)
        assert carve_o[0] <= R1_EL, (name, carve_o[0])
        ap = R1[:, a:a + nb]
        if dt != BF16:
            ap = ap.bitcast(dt)
        if len(shape) == 3:
            ap = ap.rearrange("p (a b) -> p a b", a=shape[1])
        return View(ap[0:shape[0]], name)
    wup = R1[:, 0:32768].rearrange("p (k n) -> p k n", k=8)
    wdn = R1[:, 32768:65536].rearrange("p (k n) -> p k n", k=32)

    NF = 16
    R2 = P.sb([128, 6144], F32, "R2")
    F = [View(R2[:, TT * i:TT * (i + 1)], f"F{i}") for i in range(NF)]
    R2b = R2[:, 4096:6144].bitcast(BF16)
    pT = Ring([View(R2b[:, TT * i:TT * (i + 1)], f"pT{i}") for i in range(4)])
    qn = R2b[:, 1024:1536].rearrange("p (c n) -> p c n", c=2)
    mixA = R2b[:, 1536:2560].rearrange("p (c n) -> p c n", c=4)
    mixB = R2b[:, 2560:4096].rearrange("p (c n) -> p c n", c=6)
    uT = R2[:, 0:4096].bitcast(BF16).rearrange("p (k n) -> p k n", k=32)
    FM = [View(R2[:, 4096 + TT * i:4096 + TT * (i + 1)], f"FM{i}") for i in range(8)]

    banks = [P.ps([128, 512], F32, f"bank{i}") for i in range(8)]
    pring = Ring(banks[0:4])
    acc = banks[4:8]

    xs = P.sb([128, 8, TT], F32, "xs")
    hT = P.sb([128, 8, TT], BF16, "hT")
    Bb = [P.sb([128, TT], BF16, f"B{i}") for i in range(12)]
    bring = Ring(Bb[8:12])
    vecs = P.sb([128, NV], F32, "vecs_s")
    rows = P.sb([128, NR], F32, "rows_s")
    neglam = P.sb([128, 1], F32, "neglam")
    ltmp = P.sb([128, 64], F32, "ltmp")
    lsum = P.sb([128, 2], F32, "lsum")


    Dsb = carve([128, 512], F32, "Dsb")
    Grep = carve([128, 512], F32, "Grep")
    Hs = carve([128, 512], F32, "Hs")
    HbB = carve([128, 8, 256], BF16, "HbB")
    HbC = [carve([128, 8, 128], BF16, f"HbC{i}") for i in range(2)]
    Vm = carve([128, 4, 256], BF16, "Vm")
    Vbd = carve([128, 4, 256], BF16, "Vbd")
    Qbd = [carve([128, 512], BF16, f"Qbd{i}") for i in range(2)]
    Qm = [carve([128, 4, 128], BF16, f"Qm{i}") for i in range(2)]
    khat = carve([128, 2, 256], BF16, "khat")
    Vtm = carve([128, 2, 256], BF16, "Vtm")
    sog = carve([128, 2, 256], F32, "sog")
    mtm = carve([128, 256], BF16, "mtm")
    Smk = carve([128, 4, 128], BF16, "Smk")
    pf = carve([128, TT + 2], F32, "pf")
    PRb = carve([128, 2, 256], BF16, "PRb")
    Ktb = carve([128, TT], BF16, "Ktb")
    Qtb = carve([128, TT], BF16, "Qtb")
    Khb = carve([128, TT], BF16, "Khb")
    Qhb = carve([128, TT], BF16, "Qhb")
    vpb = carve([128, TT], BF16, "vpb")
    Vdtm = carve([128, 128], BF16, "Vdtm")
    Ptm = carve([128, 128], BF16, "Ptm")
    Qhtm = carve([128, 128], BF16, "Qhtm")
    Khtm = carve([128, 128], BF16, "Khtm")
    AMb = [carve([128, 2, 256], BF16, f"AM{i}") for i in range(2)]
    Nb = [[carve([128, 128], BF16, f"Nb{h}_{i}") for i in range(3)] for h in range(2)]
    Mb = [[carve([128, 128], BF16, f"Mb{h}_{i}") for i in range(2)] for h in range(2)]
    Wb = [carve([128, 2, 128], BF16, f"Wb{i}") for i in range(2)]
    PRm = carve([128, 2, 256], BF16, "PRm")
    Rm = carve([128, 2, 128], BF16, "Rm")
    PH = carve([128, 128], BF16, "PH")
    PHT = carve([128, 128], BF16, "PHT")
    Umb = carve([128, 2, 128], BF16, "Umb")
    Ucomb = carve([128, 128], BF16, "Ucomb")
    Uhm = carve([128, 2, 128], BF16, "Uhm")
    Vmj = carve([128, 2, 128], BF16, "Vmj")
    Hexp = [carve([128, 128], F32, f"Hexp{i}") for i in range(2)]
    Hbf = [carve([128, 128], BF16, f"Hbf{i}") for i in range(3)]
    ysb = carve([128, 128], F32, "ysb")
    ynb = carve([128, 128], BF16, "ynb")
    prevc = P.sb([128, 9], F32, "prevc")
    lnst = P.sb([128, 16], F32, "lnst")
    ltot4 = P.sb([128, 4], F32, "ltot4")
    gam4 = [P.sb([128, 4], F32, f"gam4_{i}") for i in range(2)]
    carry = [P.sb([128, 64], F32, f"carry{i}") for i in range(3)]
    gam = [P.sb([128, 8], F32, f"gam{i}") for i in range(2)]
    ltot = P.sb([128, 8], F32, "ltot")
    st4 = P.sb([128, 16], F32, "st4")
    swb = P.sb([64, NSW], BF16, "swb")
    negb = P.sb([128, 1], F32, "negb")
    lbl = P.sb([128, 2 * L_total], F32, "lbl_s")
    lbT = P.sb([128, 2 * L_total], F32, "lbT")
    omlT = P.sb([128, 2 * L_total], F32, "omlT")
    lb_t = P.sb([128, 4], F32, "lb_t")
    ones4 = P.sb([128, L_total], F32, "ones4")
    P.dma("sp", lambda e: e.dma_start(out=lbl[:], in_=lbl_d[:, :]), writes=[lbl])
    P.op("pool", lambda e: e.memset(ones4[:], 1.0), writes=[ones4])
    Lt = L_total
    for rc in range(2):
        sl = slice(rc * Lt, (rc + 1) * Lt)
        P.op("dve", lambda e, sl=sl: e.tensor_reduce(out=lb_t[:, 0:1], in_=lbl[:, sl], axis=AX.X, op=ALU.max, negate=True),
             reads=[lbl], writes=[lb_t])
        P.op("act", lambda e, sl=sl: e.activation(out=lbT[:, sl], in_=lbl[:, sl], func=AF.Exp, bias=lb_t[:, 0:1], scale=1.0),
             reads=[lbl, lb_t], writes=[lbT])
        P.op("dve", lambda e, sl=sl: e.tensor_reduce(out=lb_t[:, 1:2], in_=lbT[:, sl], axis=AX.X, op=ALU.add),
             reads=[lbT], writes=[lb_t])
        P.op("dve", lambda e: e.reciprocal(out=lb_t[:, 2:3], in_=lb_t[:, 1:2]), reads=[lb_t], writes=[lb_t])
        P.op("dve", lambda e, sl=sl: e.tensor_scalar(out=lbT[:, sl], in0=lbT[:, sl], scalar1=lb_t[:, 2:3], scalar2=None,
                                                      op0=ALU.mult), reads=[lbT, lb_t], writes=[lbT])
        P.op("dve", lambda e, sl=sl: e.tensor_copy(out=lb_t[:, 3:4], in_=lbT[:, rc * Lt:rc * Lt + 1]), reads=[lbT], writes=[lb_t])
        P.op("dve", lambda e, sl=sl: e.tensor_tensor_scan(out=omlT[:, sl], data0=ones4[:], data1=lbT[:, sl], initial=0.0,
                                                           op0=ALU.mult, op1=ALU.add), reads=[lbT, ones4], writes=[omlT])
        P.op("dve", lambda e, sl=sl: e.tensor_scalar(out=lbT[:, sl], in0=omlT[:, sl], scalar1=lb_t[:, 3:4], scalar2=None,
                                                      op0=ALU.subtract), reads=[omlT, lb_t], writes=[lbT])
        P.op("dve", lambda e, sl=sl: e.tensor_scalar(out=omlT[:, sl], in0=lbT[:, sl], scalar1=-1.0, scalar2=1.0,
                                                      op0=ALU.mult, op1=ALU.add), reads=[lbT], writes=[omlT])

    def vcol(c, n=128):
        return vecs[0:n, c:c + 1]

    def barrier():
        evs = []
        for e in ENGS:
            if P.cnt[e] > 0:
                evs.append(("eng", e, P.cnt[e]))
        for si in range(N_DMA_SEMS):
            if P.dma_tot[si] > 0:
                evs.append(("dma", si, P.dma_tot[si]))
        for e in ENGS:
            P.wait_events(e, [ev for ev in evs if not (ev[0] == "eng" and ev[1] == e)])
        P.st.clear()

    def rmsnorm_tile(x_src, c0, n, gcol0, Ft):
        for k in range(8):
            P.dma("sp", lambda e, k=k: e.dma_start(
                out=xs[:, k, 0:n], in_=x_src[128 * k:128 * (k + 1), c0:c0 + n]), writes=[("xs", k)])
        ssp = pring.get()
        for k in range(8):
            sq = bring.get()
            P.op("act", lambda e, k=k, sq=sq: e.activation(out=sq[:, 0:n], in_=xs[:, k, 0:n], func=AF.Square),
                 reads=[("xs", k)], writes=[sq])
            P.op("pe", lambda e, k=k, sq=sq: e.matmul(ssp[:, 0:n], lhsT=ones_bf[:], rhs=sq[:, 0:n],
                                                      start=(k == 0), stop=(k == 7)),
                 reads=[ones_bf, sq], writes=[ssp])
        rstd = Ft
        P.op("act", lambda e: e.activation(out=rstd[:, 0:n], in_=ssp[:, 0:n], func=AF.Sqrt,
                                           scale=1.0 / D, bias=RMS_EPS), reads=[ssp], writes=[rstd])
        P.op("dve", lambda e: e.reciprocal(out=rstd[:, 0:n], in_=rstd[:, 0:n]), reads=[rstd], writes=[rstd])
        for k in range(8):
            P.op("dve", lambda e, k=k: e.scalar_tensor_tensor(
                out=hT[:, k, 0:n], in0=xs[:, k, 0:n], scalar=vcol(gcol0 + k), in1=rstd[:, 0:n],
                op0=ALU.mult, op1=ALU.mult), reads=[("xs", k), vecs, rstd], writes=[("hT", k)])

    x_src = xT_in
    for li in range(n_layers):
        lid = layer_ids[li]
        lam_init = 0.8 - 0.6 * math.exp(-0.3 * lid)
        x_mid = xa
        x_dst = yT if li == n_layers - 1 else xb
        barrier()
        P.dma("sp", lambda e, li=li: e.dma_start(out=vecs[:], in_=vecs_d[li, :, :]), writes=[vecs])
        P.dma("sp", lambda e, li=li: e.dma_start(out=rows[:], in_=rows_d[li, :, :]), writes=[rows])
        P.dma("pool", lambda e, li=li: e.dma_start(out=swb[:], in_=sw_d[li, :, :]), writes=[swb])
        P.op("dve", lambda e: e.tensor_scalar(out=negb[:], in0=vcol(V_GLAB), scalar1=-1.0, scalar2=None, op0=ALU.mult),
             reads=[vecs], writes=[negb])
        lam4 = rows[:, R_LAM:R_LAM + 128].rearrange("p (a t b) -> p a t b", a=2, t=2)
        P.op("dve", lambda e: e.tensor_tensor(
            out=ltmp[:].rearrange("p (a b) -> p a b", a=2), in0=lam4[:, :, 0, :], in1=lam4[:, :, 1, :],
            op=ALU.mult), reads=[rows], writes=[ltmp])
        P.op("dve", lambda e: e.tensor_reduce(out=lsum[:], in_=ltmp[:].rearrange("p (a b) -> p a b", a=2),
                                               axis=AX.X, op=ALU.add), reads=[ltmp], writes=[lsum])
        P.op("act", lambda e: e.activation(out=lsum[:], in_=lsum[:], func=AF.Exp), reads=[lsum], writes=[lsum])
        P.op("dve", lambda e, lam_init=lam_init: e.scalar_tensor_tensor(
            out=neglam[:], in0=lsum[:, 1:2], scalar=-lam_init, in1=lsum[:, 0:1], op0=ALU.add, op1=ALU.subtract),
            reads=[lsum], writes=[neglam])

        for phase in ("a", "b"):
            barrier()
            if phase == "a":
                win = winA
                P.dma("pool", lambda e, li=li: e.dma_start(
                    out=winA[:, :, :], in_=w_in_d[li, :, 0:768].rearrange("(k p) n -> p k n", p=128)), writes=["win"])
                P.dma("pool", lambda e, li=li: e.dma_start(
                    out=woA[0:64, :, :], in_=w_out_d[li, 0:256, :].rearrange("(h v) n -> v h n", v=64)), writes=["wo"])
                P.op("pool", lambda e: e.memset(Vc[:, :, :, 64:65], 1.0), writes=["Vones"])
            else:
                win = winB
                for cb in range(0, PIN, 512):
                    ce = min(PIN, cb + 512)
                    P.dma("pool", lambda e, li=li, cb=cb, ce=ce: e.dma_start(
                        out=winB[:, :, cb:ce], in_=w_in_d[li, :, cb:ce].rearrange("(k p) n -> p k n", p=128)),
                        writes=["win"])
                for c2 in range(6):
                    P.dma("pool", lambda e, li=li, c2=c2: e.dma_start(
                        out=woB[:, c2, :], in_=w_out_d[li, 256 + 128 * c2:256 + 128 * (c2 + 1), :]), writes=["wo"])
                for cr in carry:
                    P.op("pool", lambda e, cr=cr: e.memset(cr[:], 0.0), writes=[cr])
                P.op("pool", lambda e: e.memset(prevc[:], 0.0), writes=[prevc])
                for hx in Hexp:
                    P.op("pool", lambda e, hx=hx: e.memset(hx[:], 0.0), writes=[hx])
                P.op("pool", lambda e: e.memset(HbB[:], 0.0), writes=[HbB])
                for hb_ in HbC:
                    P.op("pool", lambda e, hb_=hb_: e.memset(hb_[:], 0.0), writes=[hb_])
            for t in range(NT):
                c0 = t * TT
                rmsnorm_tile(x_src, c0, TT, V_GMIX, F[15])
                hT_keys = [("hT", k) for k in range(8)]

                def fm_proj(col0, nrows):
                    ps = pring.get()
                    for k in range(8):
                        P.op("pe", lambda e, k=k, ps=ps: e.matmul(
                            ps[0:nrows, 0:TT], lhsT=win[:, k, col0:col0 + nrows], rhs=hT[:, k, :],
                            start=(k == 0), stop=(k == 7)), reads=["win"] + hT_keys, writes=[ps])
                    return ps

                def tm_proj(col0, ncols, sub):
                    ps = pring.get()
                    for k in range(8):
                        P.op("pe", lambda e, k=k, ps=ps: e.matmul(
                            ps[:, 0:ncols], lhsT=hT[:, k, 128 * sub:128 * (sub + 1)], rhs=win[:, k, col0:col0 + ncols],
                            start=(k == 0), stop=(k == 7)), reads=["win"] + hT_keys, writes=[ps])
                    return ps

                if phase == "a" and "A" in mixers:
                    for which in range(4):
                        isq = which < 2
                        ch = which % 2
                        ps = fm_proj((0 if isq else 256) + 128 * ch, 128)
                        qf, sq = F[0], Bb[0]
                        P.op("act", lambda e, ps=ps: e.activation(out=qf[:], in_=ps[:, 0:TT], func=AF.Copy),
                             reads=[ps], writes=[qf])
                        P.op("act", lambda e, ps=ps: e.activation(out=sq[:], in_=ps[:, 0:TT], func=AF.Square),
                             reads=[ps], writes=[sq])
                        gs = pring.get()
                        P.op("pe", lambda e, gs=gs: e.matmul(gs[:, 0:TT], lhsT=bo32[:], rhs=sq[:], start=True, stop=True),
                             reads=[bo32, sq], writes=[gs])
                        sd = F[1]
                        if isq:
                            P.op("act", lambda e, gs=gs: e.activation(out=sd[:], in_=gs[:, 0:TT], func=AF.Sqrt,
                                                                      scale=1.0, bias=32.0 * RMS_EPS),
                                 reads=[gs], writes=[sd])
                        else:
                            P.op("act", lambda e, gs=gs: e.activation(out=sd[:], in_=gs[:, 0:TT], func=AF.Sqrt,
                                                                      scale=1.0 / 32.0, bias=RMS_EPS),
                                 reads=[gs], writes=[sd])
                        P.op("dve", lambda e: e.reciprocal(out=sd[:], in_=sd[:]), reads=[sd], writes=[sd])
                        if isq:
                            P.op("dve", lambda e, ch=ch: e.scalar_tensor_tensor(
                                out=qn[:, ch, :], in0=qf[:], scalar=vcol(V_GQ), in1=sd[:], op0=ALU.mult, op1=ALU.mult),
                                reads=[qf, sd, vecs], writes=[("qn", ch)])
                        else:
                            P.op("dve", lambda e, ch=ch, c0=c0: e.scalar_tensor_tensor(
                                out=KT[:, ch, c0:c0 + TT], in0=qf[:], scalar=vcol(V_GK), in1=sd[:],
                                op0=ALU.mult, op1=ALU.mult),
                                reads=[qf, sd, vecs], writes=[("KT", ch, t)])
                    for sub in range(NDIAG):
                        ps = tm_proj(512, 256, sub)
                        P.op("act", lambda e, ps=ps, sub=sub, t=t: e.activation(
                            out=Vc[:, NDIAG * t + sub, :, 0:64], in_=ps[:, 0:256].rearrange("p (h v) -> p h v", h=4),
                            func=AF.Copy), reads=[ps, "Vones"], writes=[("Vc", NDIAG * t + sub)])

                    def attn_block(h, c, qlo, qn_cols, ktiles, bias_col_fn, o_ps, o_lo):
                        ch = (2 * h + c) // 4
                        off = 32 * ((2 * h + c) % 4)
                        LA = 2
                        pend = []
                        nk = len(ktiles)
                        for idx in range(nk + LA):
                            if idx < nk:
                                kt, m = ktiles[idx]
                                cs = 0 if m is None else 128 * m
                                n = qn_cols - cs
                                sp_ = pring.get()
                                P.op("pe", lambda e: e.matmul(
                                    sp_[:, 0:n], lhsT=KT[off:off + 32, ch, 128 * kt:128 * kt + 128],
                                    rhs=qn[off:off + 32, ch, qlo + cs:qlo + cs + n], start=True, stop=True,
                                    tile_position=(off, 0)),
                                    reads=[("KT", ch, kt // NDIAG), ("qn", ch)], writes=[sp_])
                                pt = pT.get()
                                bc = bias_col_fn(kt)
                                P.op("act", lambda e: e.activation(
                                    out=pt[:, 0:n], in_=sp_[:, 0:n], func=AF.Exp, bias=biasc[:, bc:bc + 1], scale=1.0),
                                    reads=[sp_, biasc], writes=[pt])
                                if m is not None:
                                    P.op("pool", lambda e: e.tensor_tensor(
                                        out=pt[:, 0:n], in0=pt[:, 0:n], in1=caus[:, 0:n], op=ALU.mult),
                                        reads=[pt, cstb], writes=[pt])
                                pend.append((pt, kt, cs, n))
                            if idx >= LA:
                                pt, kt, cs, n = pend[idx - LA]
                                first = (idx == LA)
                                P.op("pe", lambda e: e.matmul(
                                    o_ps[0:65, o_lo + cs:o_lo + cs + n], lhsT=Vc[:, kt, h, :], rhs=pt[:, 0:n],
                                    start=first, stop=False), reads=[pt, ("Vc", kt)], writes=[o_ps])

                    for h in range(4):
                        onrm = [F[4], F[5]]
                        for c in range(2):
                            o_ps = acc[c]
                            if h == 0:
                                for qs in range(NDIAG):
                                    gq = NDIAG * t + qs
                                    kts = [(kt, None) for kt in range(max(0, gq - 4), gq)] + [(gq, 0)]
                                    attn_block(h, c, 128 * qs, 128, kts,
                                               lambda kt, gq=gq: (gq - kt), o_ps, 128 * qs)
                            else:
                                lo_kt = max(0, NDIAG * t - 16) if h == 1 else 0
                                kts = [(kt, None) for kt in range(lo_kt, NDIAG * t)] + [(NDIAG * t + m, m) for m in range(NDIAG)]
                                attn_block(h, c, 0, TT, kts,
                                           lambda kt, h=h, t=t: 5 + (h - 1) * ND + (NDIAG * t - kt) + (NDIAG - 1),
                                           o_ps, 0)
                            oa = F[2 + c]
                            P.op("act", lambda e, o_ps=o_ps, oa=oa: e.activation(out=oa[0:65, :], in_=o_ps[0:65, 0:TT],
                                                                               func=AF.Copy),
                                 reads=[o_ps], writes=[oa])
                            dps = pring.get()
                            P.op("pe", lambda e, dps=dps, oa=oa: e.matmul(dps[0:64, 0:TT], lhsT=Esel[0:65, :],
                                                                          rhs=oa[0:65, :], start=True, stop=True),
                                 reads=[Esel, oa], writes=[dps])
                            rd = F[6]
                            P.op("dve", lambda e, dps=dps: e.reciprocal(out=rd[0:64, :], in_=dps[0:64, 0:TT]),
                                 reads=[dps], writes=[rd])
                            P.op("dve", lambda e, oa=oa, c=c: e.tensor_tensor(
                                out=onrm[c][0:64, :], in0=oa[0:64, :], in1=rd[0:64, :], op=ALU.mult),
                                reads=[oa, rd], writes=[onrm[c]])
                        df = F[7]
                        P.op("dve", lambda e: e.scalar_tensor_tensor(
                            out=df[0:64, :], in0=onrm[1][0:64, :], scalar=neglam[0:64, :], in1=onrm[0][0:64, :],
                            op0=ALU.mult, op1=ALU.add), reads=[onrm[0], onrm[1], neglam], writes=[df])
                        sq = Bb[1]
                        P.op("act", lambda e: e.activation(out=sq[0:64, :], in_=df[0:64, :], func=AF.Square),
                             reads=[df], writes=[sq])
                        mps = pring.get()
                        P.op("pe", lambda e, mps=mps: e.matmul(mps[0:64, 0:TT], lhsT=ones_bf[0:64, 0:64], rhs=sq[0:64, :],
                                                               start=True, stop=True), reads=[ones_bf, sq], writes=[mps])
                        sd = F[8]
                        s1 = 1.0 - lam_init
                        P.op("act", lambda e, mps=mps, s1=s1: e.activation(
                            out=sd[0:64, :], in_=mps[0:64, 0:TT], func=AF.Sqrt, scale=1.0 / (64.0 * s1 * s1),
                            bias=RMS_EPS / (s1 * s1)), reads=[mps], writes=[sd])
                        P.op("dve", lambda e: e.reciprocal(out=sd[0:64, :], in_=sd[0:64, :]), reads=[sd], writes=[sd])
                        P.op("dve", lambda e, h=h: e.scalar_tensor_tensor(
                            out=mixA[0:64, h, :], in0=df[0:64, :], scalar=vcol(V_GA, 64), in1=sd[0:64, :],
                            op0=ALU.mult, op1=ALU.mult), reads=[df, sd, vecs], writes=[("mixA", h)])
                elif phase == "a":
                    for h in range(4):
                        P.op("pool", lambda e, h=h: e.memset(mixA[0:64, h, :], 0.0), writes=[("mixA", h)])

                def transpose_to(out_ap, in_ap, out_key, in_key, eng="act"):
                    tp = pring.get()
                    tpb = tp[:].bitcast(BF16)
                    P.op("pe", lambda e: e.transpose(tpb[:, 0:128], in_ap, ident), reads=[in_key, cstb], writes=[tp])
                    if eng == "act":
                        P.op("act", lambda e: e.activation(out=out_ap, in_=tpb[:, 0:128], func=AF.Copy),
                             reads=[tp], writes=[out_key])
                    else:
                        P.op("dve", lambda e: e.tensor_copy(out=out_ap, in_=tpb[:, 0:128]), reads=[tp], writes=[out_key])

                def lin_attn(kind):
                    if kind == "B":
                        RC, Kh, qcol, kcol, cbase, sc = 1, 32, 768, 896, 0, -1.0 / 16.0
                        grow = rows[:, R_GB:R_GB + 256]
                    else:
                        RC, Kh, qcol, kcol, cbase, sc = 2, 64, 1552, 1808, 2, 1.0
                        grow = rows[:, R_GC:R_GC + 256]
                    hpc = 128 // Kh
                    qt_l, kt_l = [], []
                    for sub in range(NDIAG):
                        if kind == "B":
                            psv = tm_proj(1024, 256, sub)
                            P.op("act", lambda e, psv=psv, sub=sub: e.activation(out=Vtm[:, sub, :], in_=psv[:, 0:256], func=AF.Copy),
                                 reads=[psv], writes=[("Vtm", sub)])
                            pso = tm_proj(1296, 256, sub)
                            P.op("act", lambda e, pso=pso, sub=sub: e.activation(out=sog[:, sub, :], in_=pso[:, 0:256], func=AF.Silu),
                                 reads=[pso], writes=[("sog", sub)])
                        else:
                            psv = tm_proj(2064, 512, sub)
                            P.op("act", lambda e, psv=psv, sub=sub: e.activation(out=Vtm[:, sub, :], in_=psv[:, 0:256], func=AF.Copy),
                                 reads=[psv], writes=[("Vtm", sub)])
                            P.op("act", lambda e, psv=psv, sub=sub: e.activation(out=sog[:, sub, :], in_=psv[:, 256:512], func=AF.Silu),
                                 reads=[psv], writes=[("sog", sub)])
                    for rc in range(RC):
                        qt, ktl, kh_ = Bb[2 + rc], Bb[4 + rc], Bb[6]
                        qt_l.append(qt); kt_l.append(ktl)
                        lf, Lc, eq, ek, dd = F[0], F[1], F[2], F[3], F[4]
                        if kind == "B":
                            psg = fm_proj(1280, 16)
                            gdb = Bb[7]
                            P.op("act", lambda e, psg=psg: e.activation(out=gdb[0:16, :], in_=psg[0:16, 0:TT], func=AF.Copy),
                                 reads=[psg], writes=[gdb])
                            pre = pring.get()
                            P.op("pe", lambda e, pre=pre: e.matmul(pre[:, 0:TT], lhsT=swb[0:16, SW_GU:SW_GU + 128], rhs=gdb[0:16, :],
                                                                   start=True, stop=True), reads=[swb, gdb], writes=[pre])
                            e1 = F[5]
                            P.op("act", lambda e, pre=pre: e.activation(out=e1[:], in_=pre[:, 0:TT], func=AF.Exp, scale=-1.0,
                                                                        bias=negb[:, 0:1]), reads=[pre, negb], writes=[e1])
                            P.op("act", lambda e: e.activation(out=lf[:], in_=e1[:], func=AF.Ln, bias=1.0, scale=1.0),
                                 reads=[e1], writes=[lf])
                            psq = fm_proj(qcol, 128)
                            psk = fm_proj(kcol, 128)
                            kv = None
                        else:
                            psf = fm_proj(kcol + 128 * rc, 128)
                            sg, fg, kvf = F[5], F[6], F[7]
                            ez = F[11]
                            P.op("act", lambda e, psf=psf: e.activation(out=ez[:], in_=psf[:, 0:TT], func=AF.Exp, scale=-1.0),
                                 reads=[psf], writes=[ez])
                            P.op("dve", lambda e: e.tensor_scalar(out=sg[:], in0=ez[:], scalar1=1.0, scalar2=None, op0=ALU.add),
                                 reads=[ez], writes=[sg])
                            P.op("dve", lambda e: e.reciprocal(out=sg[:], in_=sg[:]), reads=[sg], writes=[sg])
                            lcol = rc * L_total + lid
                            P.op("dve", lambda e, lcol=lcol: e.tensor_scalar(
                                out=fg[:], in0=sg[:], scalar1=omlT[:, lcol:lcol + 1], scalar2=lbT[:, lcol:lcol + 1],
                                op0=ALU.mult, op1=ALU.add), reads=[sg, omlT, lbT], writes=[fg])
                            P.op("dve", lambda e: e.tensor_scalar(out=fg[:], in0=fg[:], scalar1=1e-30, scalar2=None, op0=ALU.max),
                                 reads=[fg], writes=[fg])
                            P.op("act", lambda e: e.activation(out=lf[:], in_=fg[:], func=AF.Ln), reads=[fg], writes=[lf])
                            P.op("dve", lambda e, lcol=lcol: e.scalar_tensor_tensor(
                                out=kvf[:], in0=ez[:], scalar=omlT[:, lcol:lcol + 1], in1=sg[:], op0=ALU.mult, op1=ALU.mult),
                                reads=[ez, sg, omlT], writes=[kvf])
                            psq = fm_proj(qcol + 128 * rc, 128)
                            qsl = F[8]
                            P.op("act", lambda e, psq=psq: e.activation(out=qsl[:], in_=psq[:, 0:TT], func=AF.Silu),
                                 reads=[psq], writes=[qsl])
                            kv = kvf
                        P.op("dve", lambda e: e.tensor_tensor_scan(out=Lc[:], data0=cstb[:, C_RS32:C_RS32 + TT], data1=lf[:],
                                                                   initial=0.0, op0=ALU.mult, op1=ALU.add),
                             reads=[cstb, lf], writes=[Lc])
                        Lc3 = Lc[:].rearrange("p (c j) -> p c j", j=32)
                        if debug and kind == "B" and t == 0:
                            P.op("dve", lambda e: e.tensor_copy(out=F[14][0:64, :], in_=swb[:, 0:256]), reads=[swb], writes=[F[14]])
                            P.dma("sp", lambda e: e.dma_start(out=dbg[768:832, 0:256], in_=F[14][0:64, :]), reads=[F[14]])
                            P.op("dve", lambda e: e.tensor_copy(out=F[13][0:32, :], in_=gdb[0:32, :]), reads=[gdb], writes=[F[13]])
                            P.dma("sp", lambda e: e.dma_start(out=dbg[832:864, 0:256], in_=F[13][0:32, :]), reads=[F[13]])
                        if debug and kind == "B" and False:
                            P.dma("sp", lambda e, c0=c0: e.dma_start(out=dbg[768:896, c0:c0 + TT], in_=Lc[:]), reads=[Lc])
                            P.dma("sp", lambda e, c0=c0: e.dma_start(out=dbg[896:1024, c0:c0 + TT], in_=lf[:]), reads=[lf])
                        P.op("dve", lambda e: e.tensor_copy(out=ltot[:], in_=Lc3[:, :, 31]), reads=[Lc], writes=[ltot])
                        P.op("act", lambda e: e.activation(out=eq[:], in_=Lc[:], func=AF.Exp, scale=sc), reads=[Lc], writes=[eq])
                        P.op("act", lambda e: e.activation(out=ek[:], in_=Lc[:], func=AF.Exp, scale=-sc), reads=[Lc], writes=[ek])
                        P.op("dve", lambda e: e.tensor_tensor(
                            out=dd[:].rearrange("p (c j) -> p c j", j=32), in0=ltot[:].unsqueeze(2).broadcast_to([128, 8, 32]),
                            in1=Lc3, op=ALU.subtract), reads=[ltot, Lc], writes=[dd])
                        P.op("act", lambda e: e.activation(out=dd[:], in_=dd[:], func=AF.Exp, scale=sc), reads=[dd], writes=[dd])
                        g_ = gam[rc]
                        P.op("act", lambda e, g_=g_: e.activation(out=g_[:], in_=ltot[:], func=AF.Exp, scale=sc),
                             reads=[ltot], writes=[g_])
                        if kind == "B":
                            P.op("dve", lambda e, psq=psq, qt=qt: e.scalar_tensor_tensor(
                                out=qt[:], in0=psq[:, 0:TT], scalar=32.0 ** -0.5, in1=eq[:], op0=ALU.mult, op1=ALU.mult),
                                reads=[psq, eq], writes=[qt])
                            P.op("dve", lambda e, psk=psk, ktl=ktl: e.tensor_tensor(out=ktl[:], in0=psk[:, 0:TT], in1=ek[:], op=ALU.mult),
                                 reads=[psk, ek], writes=[ktl])
                            P.op("dve", lambda e, psk=psk: e.tensor_tensor(out=kh_[:], in0=psk[:, 0:TT], in1=dd[:], op=ALU.mult),
                                 reads=[psk, dd], writes=[kh_])
                        else:
                            P.op("dve", lambda e, qt=qt: e.tensor_tensor(out=qt[:], in0=qsl[:], in1=eq[:], op=ALU.mult),
                                 reads=[qsl, eq], writes=[qt])
                            P.op("dve", lambda e, ktl=ktl: e.tensor_tensor(out=ktl[:], in0=kv[:], in1=ek[:], op=ALU.mult),
                                 reads=[kv, ek], writes=[ktl])
                            P.op("dve", lambda e: e.tensor_tensor(out=kh_[:], in0=kv[:], in1=dd[:], op=ALU.mult),
                                 reads=[kv, dd], writes=[kh_])
                        for sub in range(NDIAG):
                            transpose_to(khat[:, sub, 128 * rc:128 * rc + 128], kh_[:, 128 * sub:128 * sub + 128],
                                         ("khat", sub, rc), kh_)
                        W = 64 * hpc
                        wb = W * rc
                        D3 = Dsb[:].rearrange("p (v c) -> p v c", c=8)
                        G3 = Grep[:].rearrange("p (v c) -> p v c", c=8)
                        for sub in range(NDIAG):
                            P.op("pool", lambda e, sub=sub: e.tensor_tensor(
                                out=Vm[:], in0=Vtm[:, sub, :].unsqueeze(1).broadcast_to([128, 4, 256]),
                                in1=cstb[:, C_HM32:C_HM32 + 4].unsqueeze(2).broadcast_to([128, 4, 256]), op=ALU.mult),
                                reads=[("Vtm", sub), cstb], writes=[("Vm", 0)])
                            nslot = 512 // W
                            for c0_ in range(0, 4, nslot):
                                dps = pring.get()
                                for sl_ in range(nslot):
                                    cc = c0_ + sl_
                                    P.op("pe", lambda e, dps=dps, sl_=sl_, cc=cc, sub=sub: e.matmul(
                                        dps[:, sl_ * W:(sl_ + 1) * W], lhsT=khat[:, sub, 128 * rc:128 * rc + 128],
                                        rhs=Vm[:, cc, wb:wb + W], start=True, stop=True),
                                        reads=[("khat", sub, rc), ("Vm", 0)], writes=[dps])
                                for hh in range(hpc):
                                    hoff = Kh * hh
                                    cg = 4 * sub + c0_
                                    P.op("dve", lambda e, dps=dps, hh=hh, hoff=hoff, cg=cg: e.tensor_copy(
                                        out=D3[hoff:hoff + Kh, :, cg:cg + nslot],
                                        in_=dps[hoff:hoff + Kh, 0:nslot * W].rearrange("p (s w) -> p w s", w=W)[:, 64 * hh:64 * hh + 64, :]),
                                        reads=[dps], writes=[Dsb])
                            if kind == "C" and rc == 0 and sub == 0:
                                pass
                        cr = carry[(0 if kind == "B" else 1) + rc]
                        P.op("act", lambda e, g_=g_: e.activation(out=G3, in_=g_[:].unsqueeze(1).broadcast_to([128, 64, 8]),
                                                                  func=AF.Copy), reads=[g_], writes=[Grep])
                        P.op("pool", lambda e: e.memset(G3[:, :, 0:1], 0.0), reads=[], writes=[Grep])
                        P.op("dve", lambda e, cr=cr, g_=g_: e.scalar_tensor_tensor(
                            out=D3[:, :, 0], in0=cr[:], scalar=g_[:, 0:1], in1=D3[:, :, 0], op0=ALU.mult, op1=ALU.add),
                            reads=[cr, g_, Dsb], writes=[Dsb])
                        P.op("dve", lambda e: e.tensor_tensor_scan(out=Hs[:], data0=Grep[:], data1=Dsb[:], initial=0.0,
                                                                   op0=ALU.mult, op1=ALU.add), reads=[Grep, Dsb], writes=[Hs])
                        hb = HbB if kind == "B" else HbC[rc]
                        Hs_cv = Hs[:].rearrange("p (v c) -> p c v", c=8)
                        for hh in range(hpc):
                            hoff = Kh * hh
                            P.op("act", lambda e, hb=hb, cr=cr, hh=hh, hoff=hoff: e.activation(
                                out=hb[hoff:hoff + Kh, 0, 64 * hh:64 * hh + 64], in_=cr[hoff:hoff + Kh, :], func=AF.Copy),
                                reads=[cr], writes=[hb])
                            P.op("act", lambda e, hb=hb, hh=hh, hoff=hoff: e.activation(
                                out=hb[hoff:hoff + Kh, 1:8, 64 * hh:64 * hh + 64], in_=Hs_cv[hoff:hoff + Kh, 0:7, :], func=AF.Copy),
                                reads=[Hs], writes=[hb])
                        P.op("dve", lambda e, cr=cr: e.tensor_copy(out=cr[:], in_=Hs[:].rearrange("p (v c) -> p v c", c=8)[:, :, 7]),
                             reads=[Hs], writes=[cr])
                    W = 64 * hpc
                    hmK = cstb[:, C_HM32:C_HM32 + 4] if kind == "B" else cstb[:, C_HM64:C_HM64 + 2]
                    for sub in range(NDIAG):
                        sps = pring.get()
                        tsl = slice(128 * sub, 128 * sub + 128)
                        for rc in range(RC):
                            P.op("pool", lambda e, rc=rc, tsl=tsl: e.tensor_tensor(
                                out=Qbd[rc][:, 0:hpc * 128].rearrange("p (h i) -> p h i", h=hpc),
                                in0=qt_l[rc][:, tsl].unsqueeze(1).broadcast_to([128, hpc, 128]),
                                in1=hmK.unsqueeze(2).broadcast_to([128, hpc, 128]), op=ALU.mult),
                                reads=[qt_l[rc], cstb], writes=[Qbd[rc]])
                            P.op("pe", lambda e, rc=rc, tsl=tsl, sps=sps: e.matmul(
                                sps[:, 128 * hpc * rc:128 * hpc * (rc + 1)], lhsT=kt_l[rc][:, tsl], rhs=Qbd[rc][:, 0:hpc * 128],
                                start=True, stop=True), reads=[kt_l[rc], Qbd[rc]], writes=[sps])
                            P.op("pool", lambda e, rc=rc, tsl=tsl: e.tensor_tensor(
                                out=Qm[rc][:], in0=qt_l[rc][:, tsl].unsqueeze(1).broadcast_to([128, 4, 128]),
                                in1=cstb[:, C_TM4:C_TM4 + 512].rearrange("p (c i) -> p c i", c=4), op=ALU.mult),
                                reads=[qt_l[rc], cstb], writes=[Qm[rc]])
                        P.op("dve", lambda e, sps=sps: e.tensor_tensor(
                            out=Smk[:], in0=sps[:].rearrange("p (h i) -> p h i", h=4),
                            in1=cstb[:, C_BD32:C_BD32 + 128].unsqueeze(1).broadcast_to([128, 4, 128]), op=ALU.mult),
                            reads=[sps, cstb], writes=[Smk])
                        P.op("pool", lambda e, sub=sub: e.tensor_tensor(
                            out=Vbd[:], in0=Vtm[:, sub, :].unsqueeze(1).broadcast_to([128, 4, 256]),
                            in1=cstb[:, C_CM:C_CM + 1024].rearrange("p (h c) -> p h c", h=4), op=ALU.mult),
                            reads=[("Vtm", sub), cstb], writes=[Vbd])
                        ops_ = pring.get()
                        for h in range(4):
                            P.op("pe", lambda e, h=h, ops_=ops_: e.matmul(
                                ops_[:, 0:256], lhsT=Smk[:, h, :], rhs=Vbd[:, h, :], start=(h == 0), stop=False),
                                reads=[Smk, Vbd], writes=[ops_])
                        for rc in range(RC):
                            hb = HbB if kind == "B" else HbC[rc]
                            for cc in range(4):
                                last = (rc == RC - 1 and cc == 3)
                                P.op("pe", lambda e, rc=rc, cc=cc, hb=hb, last=last, ops_=ops_: e.matmul(
                                    ops_[:, W * rc:W * (rc + 1)], lhsT=Qm[rc][:, cc, :], rhs=hb[:, 4 * sub + cc, :],
                                    start=False, stop=last), reads=[Qm[rc], hb], writes=[ops_])
                        sqo, on = F[9], F[10]
                        P.op("act", lambda e, ops_=ops_: e.activation(out=sqo[:], in_=ops_[:, 0:256], func=AF.Square),
                             reads=[ops_], writes=[sqo])
                        P.op("dve", lambda e: e.tensor_reduce(out=st4[:, 0:4], in_=sqo[:].rearrange("p (h v) -> p h v", h=4),
                                                              axis=AX.X, op=ALU.add), reads=[sqo], writes=[st4])
                        P.op("act", lambda e: e.activation(out=st4[:, 4:8], in_=st4[:, 0:4], func=AF.Sqrt, scale=1.0 / 64.0,
                                                           bias=RMS_EPS), reads=[st4], writes=[st4])
                        P.op("dve", lambda e: e.reciprocal(out=st4[:, 8:12], in_=st4[:, 4:8]), reads=[st4], writes=[st4])
                        P.op("dve", lambda e, ops_=ops_: e.tensor_tensor(
                            out=on[:].rearrange("p (h v) -> p h v", h=4), in0=ops_[:, 0:256].rearrange("p (h v) -> p h v", h=4),
                            in1=st4[:, 8:12].unsqueeze(2).broadcast_to([128, 4, 64]), op=ALU.mult),
                            reads=[ops_, st4], writes=[on])
                        P.op("dve", lambda e: e.tensor_tensor(out=on[:], in0=on[:], in1=grow, op=ALU.mult),
                             reads=[on, rows], writes=[on])
                        P.op("dve", lambda e, sub=sub: e.tensor_tensor(out=mtm[:], in0=on[:], in1=sog[:, sub, :], op=ALU.mult),
                             reads=[on, ("sog", sub)], writes=[mtm])
                        for j in range(2):
                            transpose_to(mixB[:, cbase + j, 128 * sub:128 * sub + 128], mtm[:, 128 * j:128 * j + 128],
                                         ("mixB", cbase + j), mtm, eng="dve")

                if phase == "b" and "B" in mixers:
                    lin_attn("B")
                if phase == "b" and "C" in mixers:
                    lin_attn("C")
                def rwkv():
                    CD = 0.606531
                    hm64 = cstb[:, C_HM64:C_HM64 + 2]

                    def shifted(col0, nrows, mucol, pidx, out_ap, out_key):
                        ps = fm_proj(col0, nrows)
                        P.op("pool", lambda e: e.tensor_copy(out=pf[0:nrows, 0:1], in_=prevc[0:nrows, pidx:pidx + 1]),
                             reads=[prevc], writes=[pf])
                        P.op("act", lambda e: e.activation(out=pf[0:nrows, 1:TT + 1], in_=ps[0:nrows, 0:TT], func=AF.Copy),
                             reads=[ps], writes=[pf])
                        dtmp = F[15]
                        P.op("pool", lambda e: e.tensor_tensor(out=dtmp[0:nrows, :], in0=pf[0:nrows, 0:TT], in1=pf[0:nrows, 1:TT + 1],
                                                               op=ALU.subtract), reads=[pf], writes=[dtmp])
                        P.op("dve", lambda e: e.scalar_tensor_tensor(
                            out=out_ap, in0=dtmp[0:nrows, :], scalar=vcol(mucol, nrows), in1=pf[0:nrows, 1:TT + 1],
                            op0=ALU.mult, op1=ALU.add), reads=[dtmp, pf, vecs], writes=[out_key])
                        P.op("pool", lambda e: e.tensor_copy(out=prevc[0:nrows, pidx:pidx + 1], in_=pf[0:nrows, TT:TT + 1]),
                             reads=[pf], writes=[prevc])

                    tw, adb, sgd = Bb[0], Bb[1], Bb[2]
                    wdf = F[14]
                    shifted(3344, 32, V_MU + 6, 6, wdf[0:32, :], wdf)
                    P.op("act", lambda e: e.activation(out=tw[0:32, :], in_=wdf[0:32, :], func=AF.Tanh), reads=[wdf], writes=[tw])
                    shifted(3376, 32, V_MU + 7, 7, wdf[0:32, :], wdf)
                    P.op("act", lambda e: e.activation(out=adb[0:32, :], in_=wdf[0:32, :], func=AF.Copy), reads=[wdf], writes=[adb])
                    shifted(3408, 64, V_MU + 8, 8, wdf[0:64, :], wdf)
                    P.op("act", lambda e: e.activation(out=sgd[0:64, :], in_=wdf[0:64, :], func=AF.Sigmoid), reads=[wdf], writes=[sgd])

                    for rc in range(2):
                        rp, kp, vp, sgw, av, gT, kkn, kmod, bonus, Lw, Lx, eX, qa, dd = (F[i] for i in range(14))
                        shifted(2576 + 128 * rc, 128, V_MU + rc, rc, rp[:], rp)
                        shifted(2832 + 128 * rc, 128, V_MU + 2 + rc, 2 + rc, kp[:], kp)
                        shifted(3088 + 128 * rc, 128, V_MU + 4 + rc, 4 + rc, vp[:], vp)
                        P.op("act", lambda e: e.activation(out=vpb[:], in_=vp[:], func=AF.Copy), reads=[vp], writes=[vpb])
                        wps = pring.get()
                        P.op("pe", lambda e: e.matmul(wps[:, 0:TT], lhsT=swb[0:32, SW_WU + 128 * rc:SW_WU + 128 * rc + 128],
                                                      rhs=tw[0:32, :], start=True, stop=True), reads=[swb, tw], writes=[wps])
                        P.op("act", lambda e: e.activation(out=sgw[:], in_=wps[:, 0:TT], func=AF.Sigmoid, bias=vcol(V_W0 + rc), scale=1.0),
                             reads=[wps, vecs], writes=[sgw])
                        aps = pring.get()
                        P.op("pe", lambda e: e.matmul(aps[:, 0:TT], lhsT=swb[0:32, SW_AU + 128 * rc:SW_AU + 128 * rc + 128],
                                                      rhs=adb[0:32, :], start=True, stop=True), reads=[swb, adb], writes=[aps])
                        P.op("act", lambda e: e.activation(out=av[:], in_=aps[:, 0:TT], func=AF.Sigmoid, bias=vcol(V_A0 + rc), scale=1.0),
                             reads=[aps, vecs], writes=[av])
                        gps = pring.get()
                        P.op("pe", lambda e: e.matmul(gps[:, 0:TT], lhsT=swb[0:64, SW_GUP + 128 * rc:SW_GUP + 128 * rc + 128],
                                                      rhs=sgd[0:64, :], start=True, stop=True), reads=[swb, sgd], writes=[gps])
                        P.op("act", lambda e: e.activation(out=gT[:], in_=gps[:, 0:TT], func=AF.Copy), reads=[gps], writes=[gT])
                        P.op("dve", lambda e: e.tensor_scalar(out=kkn[:], in0=kp[:], scalar1=vcol(V_KK + rc), scalar2=None, op0=ALU.mult),
                             reads=[kp, vecs], writes=[kkn])
                        sqk = Bb[3]
                        P.op("act", lambda e: e.activation(out=sqk[:], in_=kkn[:], func=AF.Square), reads=[kkn], writes=[sqk])
                        sps_ = pring.get()
                        P.op("pe", lambda e: e.matmul(sps_[:, 0:TT], lhsT=bo64[:], rhs=sqk[:], start=True, stop=True),
                             reads=[bo64, sqk], writes=[sps_])
                        P.op("dve", lambda e: e.tensor_scalar(out=eX[:], in0=sps_[:, 0:TT], scalar1=1e-24, scalar2=None, op0=ALU.max),
                             reads=[sps_], writes=[eX])
                        P.op("act", lambda e: e.activation(out=eX[:], in_=eX[:], func=AF.Sqrt), reads=[eX], writes=[eX])
                        P.op("dve", lambda e: e.reciprocal(out=eX[:], in_=eX[:]), reads=[eX], writes=[eX])
                        P.op("dve", lambda e: e.tensor_tensor(out=kkn[:], in0=kkn[:], in1=eX[:], op=ALU.mult), reads=[kkn, eX], writes=[kkn])
                        P.op("dve", lambda e: e.tensor_scalar(out=kmod[:], in0=av[:], scalar1=-1.0, scalar2=vcol(V_KA + rc),
                                                              op0=ALU.add, op1=ALU.mult), reads=[av, vecs], writes=[kmod])
                        P.op("dve", lambda e: e.scalar_tensor_tensor(out=kmod[:], in0=kmod[:], scalar=1.0, in1=kp[:],
                                                                     op0=ALU.add, op1=ALU.mult), reads=[kmod, kp], writes=[kmod])
                        rkb = Bb[4]
                        P.op("dve", lambda e: e.scalar_tensor_tensor(out=rkb[:], in0=rp[:], scalar=vcol(V_RK + rc), in1=kmod[:],
                                                                     op0=ALU.mult, op1=ALU.mult), reads=[rp, kmod, vecs], writes=[rkb])
                        bps = pring.get()
                        P.op("pe", lambda e: e.matmul(bps[:, 0:TT], lhsT=bo64[:], rhs=rkb[:], start=True, stop=True),
                             reads=[bo64, rkb], writes=[bps])
                        P.op("dve", lambda e: e.tensor_tensor(out=bonus[:], in0=bps[:, 0:TT], in1=vp[:], op=ALU.mult),
                             reads=[bps, vp], writes=[bonus])
                        P.op("dve", lambda e: e.tensor_tensor_scan(out=Lw[:], data0=cstb[:, C_RS64:C_RS64 + TT], data1=sgw[:],
                                                                   initial=0.0, op0=ALU.mult, op1=ALU.add),
                             reads=[cstb, sgw], writes=[Lw])
                        P.op("pool", lambda e: e.tensor_tensor(out=Lx[:], in0=Lw[:], in1=sgw[:], op=ALU.subtract),
                             reads=[Lw, sgw], writes=[Lx])
                        Lw3 = Lw[:].rearrange("p (c j) -> p c j", j=64)
                        P.op("dve", lambda e: e.tensor_copy(out=ltot4[:], in_=Lw3[:, :, 63]), reads=[Lw], writes=[ltot4])
                        g4 = gam4[rc]
                        P.op("act", lambda e: e.activation(out=g4[:], in_=ltot4[:], func=AF.Exp, scale=-CD), reads=[ltot4], writes=[g4])
                        PR4 = PRb[:].rearrange("p s (w t) -> p s w t", w=2)
                        P.op("act", lambda e: e.activation(out=eX[:], in_=Lw[:], func=AF.Exp, scale=-CD), reads=[Lw], writes=[eX])
                        P.op("dve", lambda e: e.tensor_tensor(out=PR4[:, :, 1, :], in0=rp[:].rearrange("p (s t) -> p s t", s=2),
                                                              in1=eX[:].rearrange("p (s t) -> p s t", s=2), op=ALU.mult),
                             reads=[rp, eX], writes=[PRb])
                        P.op("act", lambda e: e.activation(out=eX[:], in_=Lw[:], func=AF.Exp, scale=CD), reads=[Lw], writes=[eX])
                        P.op("dve", lambda e: e.tensor_tensor(out=Ktb[:], in0=kmod[:], in1=eX[:], op=ALU.mult), reads=[kmod, eX], writes=[Ktb])
                        P.op("pool", lambda e: e.tensor_tensor(out=qa[:], in0=kkn[:], in1=av[:], op=ALU.mult), reads=[kkn, av], writes=[qa])
                        P.op("dve", lambda e: e.tensor_tensor(out=Qtb[:], in0=qa[:], in1=eX[:], op=ALU.mult), reads=[qa, eX], writes=[Qtb])
                        P.op("act", lambda e: e.activation(out=eX[:], in_=Lx[:], func=AF.Exp, scale=-CD), reads=[Lx], writes=[eX])
                        P.op("dve", lambda e: e.scalar_tensor_tensor(
                            out=PR4[:, :, 0, :], in0=kkn[:].rearrange("p (s t) -> p s t", s=2), scalar=-1.0,
                            in1=eX[:].rearrange("p (s t) -> p s t", s=2), op0=ALU.mult, op1=ALU.mult),
                            reads=[kkn, eX], writes=[PRb])
                        P.op("dve", lambda e: e.tensor_tensor(
                            out=dd[:].rearrange("p (c j) -> p c j", j=64), in0=ltot4[:].unsqueeze(2).broadcast_to([128, 4, 64]),
                            in1=Lw3, op=ALU.subtract), reads=[ltot4, Lw], writes=[dd])
                        P.op("act", lambda e: e.activation(out=dd[:], in_=dd[:], func=AF.Exp, scale=-CD), reads=[dd], writes=[dd])
                        P.op("dve", lambda e: e.tensor_tensor(out=Khb[:], in0=kmod[:], in1=dd[:], op=ALU.mult), reads=[kmod, dd], writes=[Khb])
                        P.op("pool", lambda e: e.tensor_tensor(out=Qhb[:], in0=qa[:], in1=dd[:], op=ALU.mult), reads=[qa, dd], writes=[Qhb])

                        hx = Hexp[rc]
                        hcur = [Hbf[0]]
                        hring = Ring(Hbf)
                        hring.i = 1
                        P.op("act", lambda e: e.activation(out=Hbf[0][:], in_=hx[:], func=AF.Copy), reads=[hx], writes=[Hbf[0]])
                        for sub in range(NDIAG):
                            tsl = slice(128 * sub, 128 * sub + 128)
                            transpose_to(Vdtm[:], vpb[:, tsl], Vdtm, vpb)
                            transpose_to(Ptm[:], PR4[:, sub, 0, :], Ptm, PRb)
                            transpose_to(Qhtm[:], Qhb[:, tsl], Qhtm, Qhb)
                            transpose_to(Khtm[:], Khb[:, tsl], Khtm, Khb)
                            P.op("pool", lambda e: e.tensor_tensor(
                                out=PRm[:], in0=PRb[:, sub, :].unsqueeze(1).broadcast_to([128, 2, 256]),
                                in1=hm64.unsqueeze(2).broadcast_to([128, 2, 256]), op=ALU.mult), reads=[PRb, cstb], writes=[PRm])
                            P.op("pool", lambda e: e.tensor_tensor(
                                out=Rm[:], in0=PR4[:, sub, 1, :].unsqueeze(1).broadcast_to([128, 2, 128]),
                                in1=cstb[:, C_TM2:C_TM2 + 256].rearrange("p (j t) -> p j t", j=2), op=ALU.mult),
                                reads=[PRb, cstb], writes=[Rm])
                            P.op("pool", lambda e: e.tensor_tensor(
                                out=Vmj[:], in0=Vdtm[:].unsqueeze(1).broadcast_to([128, 2, 128]),
                                in1=hm64.unsqueeze(2).broadcast_to([128, 2, 128]), op=ALU.mult), reads=[Vdtm, cstb], writes=[Vmj])
                            wcur = 0
                            Mcur = [None, None]
                            Ncur = [None, None]
                            W0 = Wb[0]
                            for hh in range(2):
                                AM = AMb[hh]
                                mps = pring.get()
                                P.op("pe", lambda e: e.matmul(mps[:, 0:256], lhsT=Qtb[:, tsl], rhs=PRm[:, hh, :], start=True, stop=True),
                                     reads=[Qtb, PRm], writes=[mps])
                                P.op("pe", lambda e: e.matmul(mps[:, 256:512], lhsT=Ktb[:, tsl], rhs=PRm[:, hh, :], start=True, stop=True),
                                     reads=[Ktb, PRm], writes=[mps])
                                P.op("dve", lambda e: e.tensor_tensor(
                                    out=AM[:], in0=mps[:].rearrange("p (a b) -> p a b", a=2),
                                    in1=cstb[:, C_ST64:C_ST64 + 256].unsqueeze(1).broadcast_to([128, 2, 256]), op=ALU.mult),
                                    reads=[mps, cstb], writes=[AM])
                                lps = pring.get()
                                P.op("pe", lambda e: e.matmul(lps[:, 0:128], lhsT=PRm[:, hh, 0:128], rhs=Qtb[:, tsl], start=True, stop=True),
                                     reads=[Qtb, PRm], writes=[lps])
                                N0 = Nb[hh][2]
                                P.op("dve", lambda e: e.tensor_tensor(out=N0[:], in0=lps[:, 0:128], in1=cstb[:, C_LO64:C_LO64 + 128],
                                                                      op=ALU.mult), reads=[lps, cstb], writes=[N0])
                                Ncur[hh] = N0
                                Mcur[hh] = (AM[:, 0, 0:128], AM)
                                avp = pring.get()
                                P.op("pe", lambda e: e.matmul(avp[:, 0:64], lhsT=AM[:, 1, 0:128], rhs=Vdtm[:, 64 * hh:64 * hh + 64],
                                                              start=True, stop=True), reads=[AM, Vdtm], writes=[avp])
                                P.op("act", lambda e: e.activation(out=W0[:, hh, 64:128], in_=avp[:, 0:64], func=AF.Copy),
                                     reads=[avp], writes=[(W0.name, hh)])
                                P.op("pool", lambda e: e.tensor_copy(out=W0[:, hh, 0:64], in_=Ptm[:, 64 * hh:64 * hh + 64]),
                                     reads=[Ptm], writes=[(W0.name, hh)])
                            wi = 0
                            for i in range(6):
                                Wc, Wn = Wb[wi], Wb[1 - wi]
                                for hh in range(2):
                                    Mcur_ap, Mcur_key = Mcur[hh]
                                    Nc = Ncur[hh]
                                    ups = pring.get()
                                    P.op("pe", lambda e: e.matmul(ups[:, 0:128], lhsT=Mcur_ap, rhs=Wc[:, hh, :], start=True, stop=True),
                                         reads=[Mcur_key, (Wc.name, hh)], writes=[ups])
                                    P.op("dve", lambda e: e.tensor_tensor(out=Wn[:, hh, :], in0=ups[:, 0:128], in1=Wc[:, hh, :], op=ALU.add),
                                         reads=[ups, (Wc.name, hh)], writes=[(Wn.name, hh)])
                                    if i < 5:
                                        Mn, Nn = Mb[hh][i % 2], Nb[hh][i % 2]
                                        m2 = pring.get()
                                        P.op("pe", lambda e: e.matmul(m2[:, 0:128], lhsT=Nc[:], rhs=Mcur_ap, start=True, stop=True),
                                             reads=[Nc, Mcur_key], writes=[m2])
                                        P.op("pe", lambda e: e.matmul(m2[:, 128:256], lhsT=Mcur_ap, rhs=Nc[:], start=True, stop=True),
                                             reads=[Nc, Mcur_key], writes=[m2])
                                        P.op("act", lambda e: e.activation(
                                            out=Mn[:], in_=m2[:, 0:128], func=AF.Copy), reads=[m2], writes=[Mn])
                                        P.op("act", lambda e: e.activation(
                                            out=Nn[:], in_=m2[:, 128:256], func=AF.Copy), reads=[m2], writes=[Nn])
                                        Mcur[hh] = (Mn[:], Mn)
                                        Ncur[hh] = Nn
                                wi = 1 - wi
                            wcur = wi
                            Wf = Wb[wcur]
                            P.op("pool", lambda e: e.tensor_copy(out=PH[:].rearrange("p (h k) -> p h k", h=2), in_=Wf[:, :, 0:64]),
                                 reads=[(Wf.name, 0), (Wf.name, 1)], writes=[PH])
                            transpose_to(PHT[:], PH[:], PHT, PH)
                            P.op("pool", lambda e: e.tensor_tensor(
                                out=Uhm[:].rearrange("p j (h v) -> p j h v", h=2),
                                in0=Wf[:, :, 64:128].unsqueeze(1).broadcast_to([128, 2, 2, 64]),
                                in1=hm64.unsqueeze(2).unsqueeze(3).broadcast_to([128, 2, 2, 64]), op=ALU.mult),
                                reads=[(Wf.name, 0), (Wf.name, 1), cstb], writes=[Uhm])
                            hstart = []
                            for j in range(2):
                                hb_c = hcur[0]
                                hstart.append(hb_c)
                                ups = pring.get()
                                P.op("pe", lambda e: e.matmul(ups[:, 0:128], lhsT=PHT[:], rhs=hb_c[:], start=True, stop=True),
                                     reads=[PHT, hb_c], writes=[ups])
                                P.op("dve", lambda e: e.scalar_tensor_tensor(
                                    out=Umb[:, j, :], in0=ups[:, 0:128], scalar=hm64[:, j:j + 1], in1=Uhm[:, j, :],
                                    op0=ALU.mult, op1=ALU.add), reads=[ups, Uhm, cstb], writes=[("Umb", j)])
                                hps = pring.get()
                                P.op("pe", lambda e: e.matmul(hps[:, 0:128], lhsT=Qhtm[:], rhs=Umb[:, j, :], start=True, stop=False),
                                     reads=[Qhtm, ("Umb", j)], writes=[hps])
                                P.op("pe", lambda e: e.matmul(hps[:, 0:128], lhsT=Khtm[:], rhs=Vmj[:, j, :], start=False, stop=True),
                                     reads=[Khtm, Vmj], writes=[hps])
                                htmp = F[14]
                                P.op("dve", lambda e: e.tensor_tensor(out=htmp[:, 0:128], in0=hps[:, 0:128],
                                                                      in1=cstb[:, C_HM64B:C_HM64B + 128], op=ALU.mult),
                                     reads=[hps, cstb], writes=[htmp])
                                ci = 2 * sub + j
                                P.op("dve", lambda e: e.scalar_tensor_tensor(
                                    out=hx[:], in0=hx[:], scalar=g4[:, ci:ci + 1], in1=htmp[:, 0:128], op0=ALU.mult, op1=ALU.add),
                                    reads=[hx, g4, htmp], writes=[hx])
                                hb_n = hring.get()
                                P.op("act", lambda e: e.activation(out=hb_n[:], in_=hx[:], func=AF.Copy), reads=[hx], writes=[hb_n])
                                hcur[0] = hb_n
                            P.op("pool", lambda e: e.tensor_tensor(out=Ucomb[:], in0=Umb[:, 0, :], in1=Umb[:, 1, :], op=ALU.add),
                                 reads=[("Umb", 0), ("Umb", 1)], writes=[Ucomb])
                            yps = pring.get()
                            for j in range(2):
                                hs_ = hstart[j]
                                P.op("pe", lambda e: e.matmul(yps[:, 0:128], lhsT=Rm[:, j, :], rhs=hs_[:], start=(j == 0), stop=False),
                                     reads=[Rm, hs_], writes=[yps])
                            for hh in range(2):
                                AM = AMb[hh]
                                P.op("pe", lambda e: e.matmul(yps[:, 64 * hh:64 * hh + 64], lhsT=AM[:, 0, 128:256],
                                                              rhs=Ucomb[:, 64 * hh:64 * hh + 64], start=False, stop=False),
                                     reads=[AM, Ucomb], writes=[yps])
                                P.op("pe", lambda e: e.matmul(yps[:, 64 * hh:64 * hh + 64], lhsT=AM[:, 1, 128:256],
                                                              rhs=Vdtm[:, 64 * hh:64 * hh + 64], start=False, stop=(hh == 1)),
                                     reads=[AM, Vdtm], writes=[yps])
                            y3 = ysb[:].rearrange("p (h v) -> p h v", h=2)
                            sqy = F[15]
                            P.op("act", lambda e: e.activation(out=ysb[:], in_=yps[:, 0:128], func=AF.Copy), reads=[yps], writes=[ysb])
                            P.op("act", lambda e: e.activation(out=sqy[:, 0:128], in_=yps[:, 0:128], func=AF.Square), reads=[yps], writes=[sqy])
                            P.op("dve", lambda e: e.tensor_reduce(out=lnst[:, 0:2], in_=y3, axis=AX.X, op=ALU.add), reads=[ysb], writes=[lnst])
                            P.op("dve", lambda e: e.tensor_reduce(out=lnst[:, 2:4], in_=sqy[:, 0:128].rearrange("p (h v) -> p h v", h=2),
                                                                  axis=AX.X, op=ALU.add), reads=[sqy], writes=[lnst])
                            P.op("dve", lambda e: e.tensor_scalar(out=lnst[:, 4:6], in0=lnst[:, 0:2], scalar1=1.0 / 64.0, scalar2=None,
                                                                  op0=ALU.mult), reads=[lnst], writes=[lnst])
                            P.op("dve", lambda e: e.tensor_tensor(out=lnst[:, 6:8], in0=lnst[:, 4:6], in1=lnst[:, 4:6], op=ALU.mult),
                                 reads=[lnst], writes=[lnst])
                            P.op("dve", lambda e: e.scalar_tensor_tensor(out=lnst[:, 8:10], in0=lnst[:, 2:4], scalar=1.0 / 64.0,
                                                                         in1=lnst[:, 6:8], op0=ALU.mult, op1=ALU.subtract),
                                 reads=[lnst], writes=[lnst])
                            P.op("act", lambda e: e.activation(out=lnst[:, 10:12], in_=lnst[:, 8:10], func=AF.Sqrt, bias=64e-5, scale=1.0),
                                 reads=[lnst], writes=[lnst])
                            P.op("dve", lambda e: e.reciprocal(out=lnst[:, 12:14], in_=lnst[:, 10:12]), reads=[lnst], writes=[lnst])
                            P.op("dve", lambda e: e.tensor_tensor(out=y3, in0=y3, in1=lnst[:, 4:6].unsqueeze(2).broadcast_to([128, 2, 64]),
                                                                  op=ALU.subtract), reads=[ysb, lnst], writes=[ysb])
                            P.op("dve", lambda e: e.tensor_tensor(out=ynb[:].rearrange("p (h v) -> p h v", h=2), in0=y3,
                                                                  in1=lnst[:, 12:14].unsqueeze(2).broadcast_to([128, 2, 64]), op=ALU.mult),
                                 reads=[ysb, lnst], writes=[ynb])
                            tp = pring.get()
                            tpb = tp[:].bitcast(BF16)
                            P.op("pe", lambda e: e.transpose(tpb[:, 0:128], ynb[:], ident), reads=[ynb, cstb], writes=[tp])
                            o1 = F[15]
                            P.op("dve", lambda e: e.tensor_scalar(out=o1[:, 0:128], in0=tpb[:, 0:128], scalar1=vcol(V_LNG + rc),
                                                                  scalar2=vcol(V_LNB + rc), op0=ALU.mult, op1=ALU.add),
                                 reads=[tp, vecs], writes=[o1])
                            P.op("pool", lambda e: e.tensor_tensor(out=o1[:, 0:128], in0=o1[:, 0:128], in1=bonus[:, tsl], op=ALU.add),
                                 reads=[o1, bonus], writes=[o1])
                            P.op("dve", lambda e: e.tensor_tensor(out=mixB[:, 4 + rc, tsl], in0=o1[:, 0:128], in1=gT[:, tsl], op=ALU.mult),
                                 reads=[o1, gT], writes=[("mixB", 4 + rc)])

                if phase == "b" and "D" in mixers:
                    rwkv()
                for c2 in range(6):
                    mixer = "BBCCDD"[c2]
                    if phase == "b" and mixer not in mixers:
                        P.op("pool", lambda e, c2=c2: e.memset(mixB[:, c2, :], 0.0), writes=[("mixB", c2)])

                if debug and phase == "a":
                    for h in range(4):
                        dt_ = F[9]
                        P.op("dve", lambda e, h=h: e.tensor_copy(out=dt_[0:64, :], in_=mixA[0:64, h, :]),
                             reads=[("mixA", h)], writes=[dt_])
                        P.dma("sp", lambda e, h=h, c0=c0: e.dma_start(out=dbg[64 * h:64 * h + 64, c0:c0 + TT],
                                                                       in_=dt_[0:64, :]), reads=[dt_])
                if debug and phase == "b":
                    for c2 in range(6 if "D" in mixers else 4):
                        dt_ = F[9]
                        P.op("dve", lambda e, c2=c2: e.tensor_copy(out=dt_[:], in_=mixB[:, c2, :]),
                             reads=[("mixB", c2)], writes=[dt_])
                        P.dma("sp", lambda e, c2=c2, c0=c0: e.dma_start(
                            out=dbg[256 + 128 * c2:256 + 128 * (c2 + 1), c0:c0 + TT], in_=dt_[:]), reads=[dt_])

                for dc in range(8):
                    ps = pring.get()
                    if phase == "a":
                        for h in range(4):
                            P.op("pe", lambda e, ps=ps, h=h, dc=dc: e.matmul(
                                ps[:, 0:TT], lhsT=woA[0:64, h, 128 * dc:128 * (dc + 1)], rhs=mixA[0:64, h, :],
                                start=(h == 0), stop=(h == 3)), reads=["wo", ("mixA", h)], writes=[ps])
                        res_ap, res_key = xs[:, dc, :], ("xs", dc)
                    else:
                        for c2 in range(6):
                            P.op("pe", lambda e, ps=ps, c2=c2, dc=dc: e.matmul(
                                ps[:, 0:TT], lhsT=woB[:, c2, 128 * dc:128 * (dc + 1)], rhs=mixB[:, c2, :],
                                start=(c2 == 0), stop=(c2 == 5)), reads=["wo", ("mixB", c2)], writes=[ps])
                        xm = F[12 + dc % 2]
                        P.dma("sp", lambda e, dc=dc, c0=c0, xm=xm, x_mid=x_mid: e.dma_start(
                            out=xm[:], in_=x_mid[128 * dc:128 * (dc + 1), c0:c0 + TT]), reads=[("xmid", t, dc)], writes=[xm])
                        res_ap, res_key = xm[:], xm
                    xo = F[10 + dc % 2]
                    P.op("dve", lambda e, ps=ps, res_ap=res_ap, xo=xo: e.tensor_tensor(
                        out=xo[:], in0=ps[:, 0:TT], in1=res_ap, op=ALU.add), reads=[ps, res_key], writes=[xo])
                    P.dma("sp", lambda e, dc=dc, c0=c0, xo=xo, x_mid=x_mid: e.dma_start(
                        out=x_mid[128 * dc:128 * (dc + 1), c0:c0 + TT], in_=xo[:]), reads=[xo], writes=[("xmid", t, dc)])

        barrier()
        if not do_mlp:
            for t in range(NT):
                c0 = t * TT
                for k in range(8):
                    P.dma("sp", lambda e, k=k, c0=c0: e.dma_start(out=xs[:, k, :], in_=x_mid[128 * k:128 * (k + 1), c0:c0 + TT]),
                          writes=[("xs", k)])
                    P.dma("sp", lambda e, k=k, c0=c0, x_dst=x_dst: e.dma_start(
                        out=x_dst[128 * k:128 * (k + 1), c0:c0 + TT], in_=xs[:, k, :]), reads=[("xs", k)])
            x_src = x_dst
            continue
        T2 = TT
        for cb in range(0, DFF, 512):
            P.dma("pool", lambda e, li=li, cb=cb: e.dma_start(
                out=wup[:, :, cb:cb + 512], in_=w_up_d[li, :, cb:cb + 512].rearrange("(k p) n -> p k n", p=128)),
                writes=["wup"])
        for kb in range(0, 32, 4):
            P.dma("pool", lambda e, li=li, kb=kb: e.dma_start(
                out=wdn[:, kb:kb + 4, :], in_=w_dn_d[li, 128 * kb:128 * (kb + 4), :].rearrange("(k p) n -> p k n", p=128)),
                writes=["wdn"])
        for t in range(S // T2):
            c0 = t * T2
            rmsnorm_tile(x_mid, c0, T2, V_GMLP, FM[7])
            hT_keys = [("hT", k) for k in range(8)]
            for fc in range(32):
                ps = pring.get()
                for k in range(8):
                    P.op("pe", lambda e, k=k, ps=ps, fc=fc: e.matmul(
                        ps[:, 0:T2], lhsT=wup[:, k, 128 * fc:128 * (fc + 1)], rhs=hT[:, k, 0:T2],
                        start=(k == 0), stop=(k == 7)), reads=["wup"] + hT_keys, writes=[ps])
                r_ = FM[fc % 2]
                P.op("act", lambda e, ps=ps, r_=r_: e.activation(out=r_[:, 0:T2], in_=ps[:, 0:T2], func=AF.Relu),
                     reads=[ps], writes=[r_])
                P.op("dve", lambda e, fc=fc, r_=r_: e.tensor_tensor(out=uT[:, fc, :], in0=r_[:, 0:T2], in1=r_[:, 0:T2],
                                                                  op=ALU.mult), reads=[r_], writes=[("uT", fc)])
            for dc in range(8):
                ps = pring.get()
                for fc in range(32):
                    P.op("pe", lambda e, ps=ps, fc=fc, dc=dc: e.matmul(
                        ps[:, 0:T2], lhsT=wdn[:, fc, 128 * dc:128 * (dc + 1)], rhs=uT[:, fc, :],
                        start=(fc == 0), stop=(fc == 31)), reads=["wdn", ("uT", fc)], writes=[ps])
                xo = FM[2 + dc % 2]
                P.op("dve", lambda e, ps=ps, dc=dc, xo=xo: e.tensor_tensor(
                    out=xo[:, 0:T2], in0=ps[:, 0:T2], in1=xs[:, dc, 0:T2], op=ALU.add),
                    reads=[ps, ("xs", dc)], writes=[xo])
                P.dma("sp", lambda e, dc=dc, c0=c0, xo=xo, x_dst=x_dst: e.dma_start(
                    out=x_dst[128 * dc:128 * (dc + 1), c0:c0 + T2], in_=xo[:, 0:T2]), reads=[xo], writes=["xdst"])
        x_src = x_dst
    P.finalize()
    return nc, P


def make_inputs(inp, core, S, layer_ids):
    L = len(layer_ids)
    vs, rs, sws = [], [], []
    for l in layer_ids:
        v, r, sw = pack_layer_params(inp, l)
        vs.append(v); rs.append(r); sws.append(sw)
    lbl = np.ascontiguousarray(np.asarray(inp["hgrn_lb_logits"], np.float32).T.reshape(2, 128, -1).transpose(1, 0, 2).reshape(128, -1))
    m = {
        "xT": np.ascontiguousarray(np.asarray(inp["x"][core, :S], np.float32).T),
        "w_in": np.ascontiguousarray(inp["w_in"][layer_ids]),
        "w_out": np.ascontiguousarray(inp["w_out"][layer_ids]),
        "w_up": np.ascontiguousarray(inp["w_mlp_up"][layer_ids]),
        "w_dn": np.ascontiguousarray(inp["w_mlp_down"][layer_ids]),
        "vecs": np.stack(vs), "rows": np.stack(rs), "smallw": np.stack(sws),
        "lbl": lbl, "consts": make_consts(),
    }
    return m


FUSED = True
N_CORES = 8
SEQ = 8192
DEPTH = 4


def kernel(**inputs):
    inp = {k: np.asarray(v) for k, v in inputs.items()}
    L = DEPTH
    if FUSED:
        nc, _ = build_program(SEQ, L, L, list(range(L)))
        base = make_inputs(inp, 0, SEQ, list(range(L)))
        in_maps = []
        for c in range(N_CORES):
            m = dict(base)
            m["xT"] = np.ascontiguousarray(np.asarray(inp["x"][c], np.float32).T)
            in_maps.append(m)
        res = run_bass_kernel_spmd(nc, in_maps, core_ids=list(range(N_CORES)))
        outs = [res.results[c]["yT"] for c in range(N_CORES)]
    else:
        xT = [np.ascontiguousarray(np.asarray(inp["x"][c], np.float32).T) for c in range(N_CORES)]
        for l in range(L):
            nc, _ = build_program(SEQ, 1, L, [l])
            base = make_inputs(inp, 0, SEQ, [l])
            in_maps = []
            for c in range(N_CORES):
                m = dict(base)
                m["xT"] = xT[c]
                in_maps.append(m)
            res = run_bass_kernel_spmd(nc, in_maps, core_ids=list(range(N_CORES)))
            xT = [np.ascontiguousarray(res.results[c]["yT"]) for c in range(N_CORES)]
        outs = xT
    out = np.stack([np.asarray(o, np.float32).T for o in outs])
    return np.ascontiguousarray(out)
```

```python
import numpy as np
from contextlib import ExitStack
import concourse.bass as bass
import concourse.mybir as mybir

F32 = mybir.dt.float32
BF16 = mybir.dt.bfloat16
I32 = mybir.dt.int32
ALU = mybir.AluOpType
AF = mybir.ActivationFunctionType
AX = mybir.AxisListType

ENGS = ("pe", "act", "dve", "pool", "sp")
N_DMA_SEMS = 24


import types


def freeze(fn):
    if fn is None or fn.__closure__ is None:
        return fn
    cells = []
    for c in fn.__closure__:
        try:
            cells.append(types.CellType(c.cell_contents))
        except ValueError:
            cells.append(c)
    return types.FunctionType(fn.__code__, fn.__globals__, fn.__name__, fn.__defaults__, tuple(cells))


class Prog:
    def __init__(self, nc):
        self.nc = nc
        self.es = ExitStack()
        self.ops = {e: [] for e in ENGS}
        self.cnt = {e: 0 for e in ENGS}
        self.st = {}
        self.dma_tot = [0] * N_DMA_SEMS
        self.dma_rr = 0
        self.waited = {e: {} for e in ENGS}
        self.dma_events = []
        self.n_t = 0

    def sb(self, shape, dt, name=None):
        self.n_t += 1
        return self.es.enter_context(self.nc.sbuf_tensor(name or f"t{self.n_t}", list(shape), dt))

    def ps(self, shape, dt, name=None):
        self.n_t += 1
        return self.es.enter_context(self.nc.psum_tensor(name or f"p{self.n_t}", list(shape), dt))

    @staticmethod
    def K(k):
        if isinstance(k, (str, int)):
            return k
        if isinstance(k, tuple):
            return tuple(Prog.K(x) for x in k)
        return k.name

    def _deps(self, eng, reads, writes, is_dma):
        reads = [self.K(k) for k in reads]
        writes = [self.K(k) for k in writes]
        deps = []
        for k in reads:
            s = self.st.get(k)
            if s and s[0] is not None:
                deps.append((s[0], "raw"))
        for k in writes:
            s = self.st.get(k)
            if s:
                if s[0] is not None:
                    deps.append((s[0], "waw"))
                for r in s[1]:
                    deps.append((r, "war"))
        waits = []
        for ev, kind in deps:
            if ev[0] == "eng":
                _, e2, n = ev
                if e2 == eng and not is_dma:
                    if kind != "raw" or eng in ("pe", "sp"):
                        continue
                semkey = ("eng", e2)
                val = n
            else:
                _, si, tot = ev
                semkey = ("dma", si)
                val = tot
            if self.waited[eng].get(semkey, 0) >= val:
                continue
            self.waited[eng][semkey] = val
            waits.append((semkey, val))
        return waits

    def _commit(self, ev, reads, writes):
        reads = [self.K(k) for k in reads]
        writes = [self.K(k) for k in writes]
        for k in reads:
            s = self.st.setdefault(k, [None, []])
            s[1].append(ev)
        for k in writes:
            self.st[k] = [ev, []]

    def op(self, eng, fn, reads=(), writes=()):
        waits = self._deps(eng, reads, writes, False)
        self.cnt[eng] += 1
        ev = ("eng", eng, self.cnt[eng])
        self.ops[eng].append(dict(fn=freeze(fn), waits=waits, ev=ev))
        self._commit(ev, reads, writes)
        return ev

    def dma(self, eng, fn, reads=(), writes=()):
        si = self.dma_rr
        self.dma_rr = (self.dma_rr + 1) % N_DMA_SEMS
        waits = self._deps(eng, reads, writes, True)
        semkey = ("dma", si)
        prev = self.dma_tot[si]
        if prev > 0 and self.waited[eng].get(semkey, 0) < prev:
            self.waited[eng][semkey] = prev
            waits.append((semkey, prev))
        self.dma_tot[si] += 16
        ev = ("dma", si, self.dma_tot[si])
        self.ops[eng].append(dict(fn=freeze(fn), waits=waits, ev=ev))
        self._commit(ev, reads, writes)
        self.dma_events.append(ev)
        return ev

    def wait_events(self, eng, events):
        waits = []
        for ev in events:
            if ev[0] == "eng":
                semkey, val = ("eng", ev[1]), ev[2]
            else:
                semkey, val = ("dma", ev[1]), ev[2]
            if self.waited[eng].get(semkey, 0) >= val:
                continue
            self.waited[eng][semkey] = val
            waits.append((semkey, val))
        self.ops[eng].append(dict(fn=None, waits=waits, ev=None))

    def finalize(self):
        nc = self.nc
        fin = [("dma", si, self.dma_tot[si]) for si in range(N_DMA_SEMS) if self.dma_tot[si] > 0]
        self.wait_events("sp", fin)
        needed = {e: set() for e in ENGS}
        for e in ENGS:
            for o in self.ops[e]:
                for (semkey, val) in o["waits"]:
                    if semkey[0] == "eng":
                        needed[semkey[1]].add(val)
        rank = {}
        for e in ENGS:
            for i, v in enumerate(sorted(needed[e])):
                rank[(e, v)] = i + 1
        sems = {}
        for e in ENGS:
            sems[("eng", e)] = self.es.enter_context(nc.semaphore(f"s_{e}"))
        for si in range(N_DMA_SEMS):
            sems[("dma", si)] = self.es.enter_context(nc.semaphore(f"s_dma{si}"))
        handles = dict(pe="tensor", act="scalar", dve="vector", pool="gpsimd", sp="sync")
        self.n_inst = 0

        def emit_engine(e, eng):
            for o in self.ops[e]:
                for (semkey, val) in o["waits"]:
                    if semkey[0] == "eng":
                        val = rank[(semkey[1], val)]
                    eng.wait_ge(sems[semkey], val)
                    self.n_inst += 1
                if o["fn"] is None:
                    continue
                ins = o["fn"](eng)
                self.n_inst += 1
                ev = o["ev"]
                if ev[0] == "dma":
                    ins.then_inc(sems[("dma", ev[1])], 16)
                elif (e, ev[2]) in rank:
                    ins.then_inc(sems[("eng", e)], 1)

        with nc.Block() as block:
            @block.tensor
            def _(eng):
                emit_engine("pe", eng)

            @block.scalar
            def _(eng):
                emit_engine("act", eng)

            @block.vector
            def _(eng):
                emit_engine("dve", eng)

            @block.gpsimd
            def _(eng):
                emit_engine("pool", eng)

            @block.sync
            def _(eng):
                emit_engine("sp", eng)
        self.es.close()


import math
import numpy as np
import concourse.bass as bass
import concourse.mybir as mybir
from concourse.bass_utils import run_bass_kernel_spmd

D = 1024
PIN = 3472
DFF = 4096
TT = 256
NDIAG = TT // 128
RMS_EPS = 1e-6
SLOPES = [2.0 ** (-8.0 * (i + 1) / 4) for i in range(4)]
ND = 66

C_ID = 0
C_BD32 = 128
C_ST64 = 256
C_IN64 = 384
C_LO64 = 512
C_RS32 = 640
C_RS64 = 1152
C_CAUS = 1664
C_TM4 = 2176
C_CM = 2688
C_HM32 = 3712
C_HM64 = 3716
C_HM64B = 3718
C_TM2 = 3846
C_CM2 = 4102
C_B0 = 4104
C_BH = C_B0 + 5
NCONST = C_BH + 3 * ND


def make_consts():
    c = np.zeros((128, NCONST), np.float32)
    p = np.arange(128)
    c[:, C_ID:C_ID + 128] = np.eye(128)
    same32 = (p[:, None] // 32) == (p[None, :] // 32)
    same64 = (p[:, None] // 64) == (p[None, :] // 64)
    c[:, C_BD32:C_BD32 + 128] = same32 & (p[:, None] <= p[None, :])
    c[:, C_ST64:C_ST64 + 128] = same64 & (p[:, None] < p[None, :])
    c[:, C_IN64:C_IN64 + 128] = same64 & (p[:, None] <= p[None, :])
    c[:, C_LO64:C_LO64 + 128] = same64 & (p[:, None] > p[None, :])
    t = np.arange(512)
    c[:, C_RS32:C_RS32 + 512] = (t % 32 != 0)[None, :]
    c[:, C_RS64:C_RS64 + 512] = (t % 64 != 0)[None, :]
    c[:, C_CAUS:C_CAUS + 512] = t[None, :] >= p[:, None]
    tok = np.arange(128)
    for cc in range(4):
        c[:, C_TM4 + 128 * cc:C_TM4 + 128 * (cc + 1)] = (tok // 32 == cc)[None, :]
    col = np.arange(256)
    for h in range(4):
        c[:, C_CM + 256 * h:C_CM + 256 * (h + 1)] = (col // 64 == h)[None, :]
        c[:, C_HM32 + h] = (p // 32 == h)
    for h in range(2):
        c[:, C_HM64 + h] = (p // 64 == h)
    c[:, C_HM64B:C_HM64B + 128] = (p[:, None] // 64) == (tok[None, :] // 64)
    for j in range(2):
        c[:, C_TM2 + 128 * j:C_TM2 + 128 * (j + 1)] = (tok // 64 == j)[None, :]
    for j in range(2):
        c[:, C_CM2 + j] = ((p // 32) % 2 == j)
    for d in range(5):
        c[:, C_B0 + d] = SLOPES[0] * (p - 127 - 128 * d)
    for h in range(1, 4):
        for di in range(ND):
            delta = di - (NDIAG - 1)
            c[:, C_BH + (h - 1) * ND + di] = SLOPES[h] * (p - (TT - 1) - 128 * delta)
    return c


V_GMIX = 0
V_GMLP = 8
V_GQ = 16
V_GK = 17
V_GA = 18
V_GLAB = 19
V_MU = 20
V_W0 = 29
V_A0 = 31
V_KK = 33
V_KA = 35
V_RK = 37
V_LNG = 39
V_LNB = 41
NV = 43
R_LAM = 0
R_GB = 128
R_GC = 384
NR = 640
SW_GU = 0
SW_WU = 128
SW_AU = 384
SW_GUP = 640
NSW = 896


def pack_layer_params(inp, l):
    g = lambda n: np.asarray(inp[n][l], np.float32)
    v = np.zeros((128, NV), np.float32)
    v[:, V_GMIX:V_GMIX + 8] = g("norm_mix_g").reshape(8, 128).T
    v[:, V_GMLP:V_GMLP + 8] = g("norm_mlp_g").reshape(8, 128).T
    v[:, V_GQ] = np.tile(g("da_q_norm_g"), 4)
    v[:, V_GK] = np.tile(g("da_k_norm_g"), 4)
    v[:, V_GA] = np.tile(g("da_out_norm_g"), 2)
    v[:, V_GLAB] = g("gla_gate_b")
    mu = g("rw_shift_mu")
    for i in range(6):
        v[:, V_MU + i] = mu[128 * i:128 * (i + 1)]
    v[:32, V_MU + 6] = mu[768:800]
    v[:32, V_MU + 7] = mu[800:832]
    v[:64, V_MU + 8] = mu[832:896]
    for name, col in (("rw_w0", V_W0), ("rw_a0", V_A0), ("rw_k_k", V_KK), ("rw_k_a", V_KA),
                      ("rw_ln_g", V_LNG), ("rw_ln_b", V_LNB)):
        v[:, col:col + 2] = g(name).reshape(2, 128).T
    v[:, V_RK:V_RK + 2] = g("rw_r_k").reshape(2, 128).T
    r = np.zeros((128, NR), np.float32)
    lam = np.concatenate([g("da_lambda_q1"), g("da_lambda_k1"), g("da_lambda_q2"), g("da_lambda_k2")])
    r[:, R_LAM:R_LAM + 128] = lam[None, :]
    r[:, R_GB:R_GB + 256] = np.tile(g("gla_out_norm_g"), 4)[None, :]
    r[:, R_GC:R_GC + 256] = np.tile(g("hgrn_out_norm_g"), 4)[None, :]
    sw = np.zeros((64, NSW), np.float32)
    sw[:16, SW_GU:SW_GU + 128] = g("gla_gate_up")
    sw[:32, SW_WU:SW_WU + 256] = g("rw_w_up")
    sw[:32, SW_AU:SW_AU + 256] = g("rw_a_up")
    sw[:64, SW_GUP:SW_GUP + 256] = g("rw_g_up")
    return v, r, sw


class Ring:
    def __init__(self, items):
        self.items = items
        self.i = 0

    def get(self):
        x = self.items[self.i]
        self.i = (self.i + 1) % len(self.items)
        return x


class View:
    def __init__(self, ap, name):
        self.ap = ap
        self.name = name

    def __getitem__(self, k):
        return self.ap[k]


def build_program(S, n_layers, L_total, layer_ids, debug=False, mixers="ABCD", do_mlp=True):
    NT = S // TT
    nc = bass.Bass("TRN2", target_bir_lowering=False)
    dt_in = lambda name, shape: nc.dram_tensor(name, list(shape), F32, kind="ExternalInput").ap()
    xT_in = dt_in("xT", [D, S])
    w_in_d = dt_in("w_in", [n_layers, D, PIN])
    w_out_d = dt_in("w_out", [n_layers, D, D])
    w_up_d = dt_in("w_up", [n_layers, D, DFF])
    w_dn_d = dt_in("w_dn", [n_layers, DFF, D])
    vecs_d = dt_in("vecs", [n_layers, 128, NV])
    rows_d = dt_in("rows", [n_layers, 128, NR])
    sw_d = dt_in("smallw", [n_layers, 64, NSW])
    lbl_d = dt_in("lbl", [128, 2 * L_total])
    consts_d = dt_in("consts", [128, NCONST])
    yT = nc.dram_tensor("yT", [D, S], F32, kind="ExternalOutput").ap()
    xa = nc.dram_tensor("xa", [D, S], F32, kind="Internal").ap()
    xb = nc.dram_tensor("xb", [D, S], F32, kind="Internal").ap()
    dbg = nc.dram_tensor("dbg", [D, S], F32, kind="ExternalOutput").ap() if debug else None

    P = Prog(nc)
    cstb = P.sb([128, C_B0], BF16, "cstb")
    for cb in range(0, C_B0, 1024):
        ce = min(C_B0, cb + 1024)
        P.dma("pool", lambda e, cb=cb, ce=ce: e.dma_start(out=cstb[:, cb:ce], in_=consts_d[:, cb:ce]), writes=[cstb])
    biasc = P.sb([128, NCONST - C_B0], F32, "biasc")
    P.dma("sp", lambda e: e.dma_start(out=biasc[:], in_=consts_d[:, C_B0:NCONST]), writes=[biasc])
    ident = cstb[:, C_ID:C_ID + 128]
    caus = cstb[:, C_CAUS:C_CAUS + 512]
    ones_bf = P.sb([128, 128], BF16, "ones_bf")
    P.op("pool", lambda e: e.memset(ones_bf[:], 1.0), writes=[ones_bf])
    bo32 = P.sb([128, 128], BF16, "bo32")
    bo64 = P.sb([128, 128], BF16, "bo64")
    P.op("pool", lambda e: e.memset(bo32[:], 0.0), writes=[bo32])
    P.op("pool", lambda e: e.memset(bo64[:], 0.0), writes=[bo64])
    for b in range(4):
        P.op("pool", lambda e, b=b: e.memset(bo32[32 * b:32 * b + 32, 32 * b:32 * b + 32], 1.0), writes=[bo32])
    for b in range(2):
        P.op("pool", lambda e, b=b: e.memset(bo64[64 * b:64 * b + 64, 64 * b:64 * b + 64], 1.0), writes=[bo64])
    Esel = P.sb([128, 64], F32, "Esel")
    P.op("pool", lambda e: e.memset(Esel[:], 0.0), writes=[Esel])
    P.op("pool", lambda e: e.memset(Esel[64:65, :], 1.0), writes=[Esel])

    NKT = S // 128
    R1_EL = 65536
    R1 = P.sb([128, R1_EL], BF16, "R1")
    winA = R1[:, 0:6144].rearrange("p (k n) -> p k n", k=8)
    woA = R1[:, 6144:10240].rearrange("p (k n) -> p k n", k=4)
    o = 10240
    KT = R1[:, o:o + 2 * S].rearrange("p (c s) -> p c s", c=2); o += 2 * S
    Vc = R1[:, o:o + NKT * 260].rearrange("p (t h v) -> p t h v", h=4, v=65); o += NKT * 260
    assert o <= R1_EL
    winB = R1[:, 0:8 * PIN].rearrange("p (k n) -> p k n", k=8)
    woB = R1[:, 8 * PIN:8 * PIN + 6 * D].rearrange("p (k n) -> p k n", k=6)
    carve_o = [8 * PIN + 6 * D]

    def carve(shape, dt, name):
        n = int(np.prod(shape[1:]))
        nb = n if dt == BF16 else 2 * n
        a = carve_o[0]
        carve_o[0] += nb + (nb % 2)
        assert carve_o[0] <= R1_EL, (name, carve_o[0])
        ap = R1[:, a:a + nb]
        if dt != BF16:
            ap = ap.bitcast(dt)
        if len(shape) == 3:
            ap = ap.rearrange("p (a b) -> p a b", a=shape[1])
        return View(ap[0:shape[0]], name)
    wup = R1[:, 0:32768].rearrange("p (k n) -> p k n", k=8)
    wdn = R1[:, 32768:65536].rearrange("p (k n) -> p k n", k=32)

    NF = 16
    R2 = P.sb([128, 6144], F32, "R2")
    F = [View(R2[:, TT * i:TT * (i + 1)], f"F{i}") for i in range(NF)]
    R2b = R2[:, 4096:6144].bitcast(BF16)
    pT = Ring([View(R2b[:, 512 * i:512 * (i + 1)], f"pT{i}") for i in range(3)]
              + [P.sb([128, 512], BF16, f"pTx{i}") for i in range(2)])
    mixA = R2b[:, 1536:2560].rearrange("p (c n) -> p c n", c=4)
    mixB = R2b[:, 2560:4096].rearrange("p (c n) -> p c n", c=6)
    uT = R2[:, 0:4096].bitcast(BF16).rearrange("p (k n) -> p k n", k=32)
    FM = [View(R2[:, 4096 + TT * i:4096 + TT * (i + 1)], f"FM{i}") for i in range(8)]

    banks = [P.ps([128, 512], F32, f"bank{i}") for i in range(8)]
    pring = Ring(banks[0:4])
    acc = banks[4:8]

    xs = P.sb([128, 8, TT], F32, "xs")
    qbd = P.sb([128, 2, 2, TT], BF16, "qbd")
    hT_l = [P.sb([128, 8, TT], BF16, f"hT{i}") for i in range(2)]
    Bb = [P.sb([128, TT], BF16, f"B{i}") for i in range(12)]
    bring = Ring(Bb[8:12])
    vecs = P.sb([128, NV], F32, "vecs_s")
    rows = P.sb([128, NR], F32, "rows_s")
    neglam = P.sb([128, 1], F32, "neglam")
    ltmp = P.sb([128, 64], F32, "ltmp")
    lsum = P.sb([128, 2], F32, "lsum")


    Dsb = carve([128, 512], F32, "Dsb")
    Grep = carve([128, 512], F32, "Grep")
    Hs = carve([128, 512], F32, "Hs")
    HbB = carve([128, 8, 256], BF16, "HbB")
    HbC = [carve([128, 8, 128], BF16, f"HbC{i}") for i in range(2)]
    Vm = carve([128, 4, 256], BF16, "Vm")
    Vbd = carve([128, 4, 256], BF16, "Vbd")
    Qbd = [carve([128, 512], BF16, f"Qbd{i}") for i in range(2)]
    Qm = [carve([128, 4, 128], BF16, f"Qm{i}") for i in range(2)]
    khat = carve([128, 2, 256], BF16, "khat")
    Vtm = carve([128, 2, 256], BF16, "Vtm")
    sog = carve([128, 2, 256], F32, "sog")
    mtm = carve([128, 256], BF16, "mtm")
    Smk = carve([128, 4, 128], BF16, "Smk")
    pf = carve([128, TT + 2], F32, "pf")
    PRb = carve([128, 2, 256], BF16, "PRb")
    Ktb = carve([128, TT], BF16, "Ktb")
    Qtb = carve([128, TT], BF16, "Qtb")
    Khb = carve([128, TT], BF16, "Khb")
    Qhb = carve([128, TT], BF16, "Qhb")
    vpb = carve([128, TT], BF16, "vpb")
    Vdtm = carve([128, 128], BF16, "Vdtm")
    Ptm = carve([128, 128], BF16, "Ptm")
    Qhtm = carve([128, 128], BF16, "Qhtm")
    Khtm = carve([128, 128], BF16, "Khtm")
    AMb = [carve([128, 2, 256], BF16, f"AM{i}") for i in range(2)]
    Nb = [[carve([128, 128], BF16, f"Nb{h}_{i}") for i in range(3)] for h in range(2)]
    Mb = [[carve([128, 128], BF16, f"Mb{h}_{i}") for i in range(2)] for h in range(2)]
    Wb = [carve([128, 2, 128], BF16, f"Wb{i}") for i in range(2)]
    PRm = carve([128, 2, 256], BF16, "PRm")
    Rm = carve([128, 2, 128], BF16, "Rm")
    PH = carve([128, 128], BF16, "PH")
    PHT = carve([128, 128], BF16, "PHT")
    Umb = carve([128, 2, 128], BF16, "Umb")
    Ucomb = carve([128, 128], BF16, "Ucomb")
    Uhm = carve([128, 2, 128], BF16, "Uhm")
    Vmj = carve([128, 2, 128], BF16, "Vmj")
    Hexp = [carve([128, 128], F32, f"Hexp{i}") for i in range(2)]
    Hbf = [carve([128, 128], BF16, f"Hbf{i}") for i in range(3)]
    ysb = carve([128, 128], F32, "ysb")
    ynb = carve([128, 128], BF16, "ynb")
    prevc = P.sb([128, 9], F32, "prevc")
    lnst = P.sb([128, 16], F32, "lnst")
    ltot4 = P.sb([128, 4], F32, "ltot4")
    gam4 = [P.sb([128, 4], F32, f"gam4_{i}") for i in range(2)]
    carry = [P.sb([128, 64], F32, f"carry{i}") for i in range(3)]
    gam = [P.sb([128, 8], F32, f"gam{i}") for i in range(2)]
    ltot = P.sb([128, 8], F32, "ltot")
    st4 = P.sb([128, 16], F32, "st4")
    swb = P.sb([64, NSW], BF16, "swb")
    negb = P.sb([128, 1], F32, "negb")
    lbl = P.sb([128, 2 * L_total], F32, "lbl_s")
    lbT = P.sb([128, 2 * L_total], F32, "lbT")
    omlT = P.sb([128, 2 * L_total], F32, "omlT")
    lb_t = P.sb([128, 4], F32, "lb_t")
    ones4 = P.sb([128, L_total], F32, "ones4")
    P.dma("sp", lambda e: e.dma_start(out=lbl[:], in_=lbl_d[:, :]), writes=[lbl])
    P.op("pool", lambda e: e.memset(ones4[:], 1.0), writes=[ones4])
    Lt = L_total
    for rc in range(2):
        sl = slice(rc * Lt, (rc + 1) * Lt)
        P.op("dve", lambda e, sl=sl: e.tensor_reduce(out=lb_t[:, 0:1], in_=lbl[:, sl], axis=AX.X, op=ALU.max, negate=True),
             reads=[lbl], writes=[lb_t])
        P.op("act", lambda e, sl=sl: e.activation(out=lbT[:, sl], in_=lbl[:, sl], func=AF.Exp, bias=lb_t[:, 0:1], scale=1.0),
             reads=[lbl, lb_t], writes=[lbT])
        P.op("dve", lambda e, sl=sl: e.tensor_reduce(out=lb_t[:, 1:2], in_=lbT[:, sl], axis=AX.X, op=ALU.add),
             reads=[lbT], writes=[lb_t])
        P.op("dve", lambda e: e.reciprocal(out=lb_t[:, 2:3], in_=lb_t[:, 1:2]), reads=[lb_t], writes=[lb_t])
        P.op("dve", lambda e, sl=sl: e.tensor_scalar(out=lbT[:, sl], in0=lbT[:, sl], scalar1=lb_t[:, 2:3], scalar2=None,
                                                      op0=ALU.mult), reads=[lbT, lb_t], writes=[lbT])
        P.op("dve", lambda e, sl=sl: e.tensor_copy(out=lb_t[:, 3:4], in_=lbT[:, rc * Lt:rc * Lt + 1]), reads=[lbT], writes=[lb_t])
        P.op("dve", lambda e, sl=sl: e.tensor_tensor_scan(out=omlT[:, sl], data0=ones4[:], data1=lbT[:, sl], initial=0.0,
                                                           op0=ALU.mult, op1=ALU.add), reads=[lbT, ones4], writes=[omlT])
        P.op("dve", lambda e, sl=sl: e.tensor_scalar(out=lbT[:, sl], in0=omlT[:, sl], scalar1=lb_t[:, 3:4], scalar2=None,
                                                      op0=ALU.subtract), reads=[omlT, lb_t], writes=[lbT])
        P.op("dve", lambda e, sl=sl: e.tensor_scalar(out=omlT[:, sl], in0=lbT[:, sl], scalar1=-1.0, scalar2=1.0,
                                                      op0=ALU.mult, op1=ALU.add), reads=[lbT], writes=[omlT])

    def vcol(c, n=128):
        return vecs[0:n, c:c + 1]

    def barrier():
        evs = []
        for e in ENGS:
            if P.cnt[e] > 0:
                evs.append(("eng", e, P.cnt[e]))
        for si in range(N_DMA_SEMS):
            if P.dma_tot[si] > 0:
                evs.append(("dma", si, P.dma_tot[si]))
        for e in ENGS:
            P.wait_events(e, [ev for ev in evs if not (ev[0] == "eng" and ev[1] == e)])
        P.st.clear()

    def rmsnorm_tile(x_src, c0, n, gcol0, Ft, bi):
        hT = hT_l[bi]
        for k in range(8):
            P.dma("sp", lambda e, k=k: e.dma_start(
                out=xs[:, k, 0:n], in_=x_src[128 * k:128 * (k + 1), c0:c0 + n]), writes=[("xs", k)])
        ssp = pring.get()
        for k in range(8):
            sq = bring.get()
            P.op("act", lambda e, k=k, sq=sq: e.activation(out=sq[:, 0:n], in_=xs[:, k, 0:n], func=AF.Square),
                 reads=[("xs", k)], writes=[sq])
            P.op("pe", lambda e, k=k, sq=sq: e.matmul(ssp[:, 0:n], lhsT=ones_bf[:], rhs=sq[:, 0:n],
                                                      start=(k == 0), stop=(k == 7)),
                 reads=[ones_bf, sq], writes=[ssp])
        rstd = Ft
        P.op("act", lambda e: e.activation(out=rstd[:, 0:n], in_=ssp[:, 0:n], func=AF.Sqrt,
                                           scale=1.0 / D, bias=RMS_EPS), reads=[ssp], writes=[rstd])
        P.op("dve", lambda e: e.reciprocal(out=rstd[:, 0:n], in_=rstd[:, 0:n]), reads=[rstd], writes=[rstd])
        for k in range(8):
            P.op("dve", lambda e, k=k: e.scalar_tensor_tensor(
                out=hT[:, k, 0:n], in0=xs[:, k, 0:n], scalar=vcol(gcol0 + k), in1=rstd[:, 0:n],
                op0=ALU.mult, op1=ALU.mult), reads=[("xs", k), vecs, rstd], writes=[("hT", bi, k)])

    x_src = xT_in
    for li in range(n_layers):
        lid = layer_ids[li]
        lam_init = 0.8 - 0.6 * math.exp(-0.3 * lid)
        x_mid = xa
        x_dst = yT if li == n_layers - 1 else xb
        barrier()
        P.dma("sp", lambda e, li=li: e.dma_start(out=vecs[:], in_=vecs_d[li, :, :]), writes=[vecs])
        P.dma("sp", lambda e, li=li: e.dma_start(out=rows[:], in_=rows_d[li, :, :]), writes=[rows])
        P.dma("pool", lambda e, li=li: e.dma_start(out=swb[:], in_=sw_d[li, :, :]), writes=[swb])
        P.op("dve", lambda e: e.tensor_scalar(out=negb[:], in0=vcol(V_GLAB), scalar1=-1.0, scalar2=None, op0=ALU.mult),
             reads=[vecs], writes=[negb])
        lam4 = rows[:, R_LAM:R_LAM + 128].rearrange("p (a t b) -> p a t b", a=2, t=2)
        P.op("dve", lambda e: e.tensor_tensor(
            out=ltmp[:].rearrange("p (a b) -> p a b", a=2), in0=lam4[:, :, 0, :], in1=lam4[:, :, 1, :],
            op=ALU.mult), reads=[rows], writes=[ltmp])
        P.op("dve", lambda e: e.tensor_reduce(out=lsum[:], in_=ltmp[:].rearrange("p (a b) -> p a b", a=2),
                                               axis=AX.X, op=ALU.add), reads=[ltmp], writes=[lsum])
        P.op("act", lambda e: e.activation(out=lsum[:], in_=lsum[:], func=AF.Exp), reads=[lsum], writes=[lsum])
        P.op("dve", lambda e, lam_init=lam_init: e.scalar_tensor_tensor(
            out=neglam[:], in0=lsum[:, 1:2], scalar=-lam_init, in1=lsum[:, 0:1], op0=ALU.add, op1=ALU.subtract),
            reads=[lsum], writes=[neglam])

        for phase in ("a", "b"):
            barrier()
            if phase == "a":
                win = winA
                P.dma("pool", lambda e, li=li: e.dma_start(
                    out=winA[:, :, :], in_=w_in_d[li, :, 0:768].rearrange("(k p) n -> p k n", p=128)), writes=["win"])
                P.dma("pool", lambda e, li=li: e.dma_start(
                    out=woA[0:64, :, :], in_=w_out_d[li, 0:256, :].rearrange("(h v) n -> v h n", v=64)), writes=["wo"])
                P.op("pool", lambda e: e.memset(Vc[:, :, :, 64:65], 1.0), writes=["Vones"])
            else:
                win = winB
                for cb in range(0, PIN, 512):
                    ce = min(PIN, cb + 512)
                    P.dma("pool", lambda e, li=li, cb=cb, ce=ce: e.dma_start(
                        out=winB[:, :, cb:ce], in_=w_in_d[li, :, cb:ce].rearrange("(k p) n -> p k n", p=128)),
                        writes=["win"])
                for c2 in range(6):
                    P.dma("pool", lambda e, li=li, c2=c2: e.dma_start(
                        out=woB[:, c2, :], in_=w_out_d[li, 256 + 128 * c2:256 + 128 * (c2 + 1), :]), writes=["wo"])
                for cr in carry:
                    P.op("pool", lambda e, cr=cr: e.memset(cr[:], 0.0), writes=[cr])
                P.op("pool", lambda e: e.memset(prevc[:], 0.0), writes=[prevc])
                for hx in Hexp:
                    P.op("pool", lambda e, hx=hx: e.memset(hx[:], 0.0), writes=[hx])
                P.op("pool", lambda e: e.memset(HbB[:], 0.0), writes=[HbB])
                for hb_ in HbC:
                    P.op("pool", lambda e, hb_=hb_: e.memset(hb_[:], 0.0), writes=[hb_])
            for t in range(NT):
                c0 = t * TT
                bi = t % 2
                if t == 0:
                    rmsnorm_tile(x_src, c0, TT, V_GMIX, F[15], 0)
                hT = hT_l[bi]
                hT_keys = [("hT", bi, k) for k in range(8)]
                pf_done = [False]

                def prefetch():
                    if not pf_done[0] and t + 1 < NT:
                        rmsnorm_tile(x_src, (t + 1) * TT, TT, V_GMIX, F[15], (t + 1) % 2)
                    pf_done[0] = True

                def fm_proj(col0, nrows):
                    ps = pring.get()
                    for k in range(8):
                        P.op("pe", lambda e, k=k, ps=ps: e.matmul(
                            ps[0:nrows, 0:TT], lhsT=win[:, k, col0:col0 + nrows], rhs=hT[:, k, :],
                            start=(k == 0), stop=(k == 7)), reads=["win"] + hT_keys, writes=[ps])
                    return ps

                def tm_proj(col0, ncols, sub):
                    ps = pring.get()
                    for k in range(8):
                        P.op("pe", lambda e, k=k, ps=ps: e.matmul(
                            ps[:, 0:ncols], lhsT=hT[:, k, 128 * sub:128 * (sub + 1)], rhs=win[:, k, col0:col0 + ncols],
                            start=(k == 0), stop=(k == 7)), reads=["win"] + hT_keys, writes=[ps])
                    return ps

                if phase == "a" and "A" in mixers:
                    for which in range(4):
                        isq = which < 2
                        ch = which % 2
                        ps = fm_proj((0 if isq else 256) + 128 * ch, 128)
                        qf, sq = F[0], Bb[0]
                        P.op("act", lambda e, ps=ps: e.activation(out=qf[:], in_=ps[:, 0:TT], func=AF.Copy),
                             reads=[ps], writes=[qf])
                        P.op("act", lambda e, ps=ps: e.activation(out=sq[:], in_=ps[:, 0:TT], func=AF.Square),
                             reads=[ps], writes=[sq])
                        gs = pring.get()
                        P.op("pe", lambda e, gs=gs: e.matmul(gs[:, 0:TT], lhsT=bo32[:], rhs=sq[:], start=True, stop=True),
                             reads=[bo32, sq], writes=[gs])
                        sd = F[1]
                        if isq:
                            P.op("act", lambda e, gs=gs: e.activation(out=sd[:], in_=gs[:, 0:TT], func=AF.Sqrt,
                                                                      scale=1.0, bias=32.0 * RMS_EPS),
                                 reads=[gs], writes=[sd])
                        else:
                            P.op("act", lambda e, gs=gs: e.activation(out=sd[:], in_=gs[:, 0:TT], func=AF.Sqrt,
                                                                      scale=1.0 / 32.0, bias=RMS_EPS),
                                 reads=[gs], writes=[sd])
                        P.op("dve", lambda e: e.reciprocal(out=sd[:], in_=sd[:]), reads=[sd], writes=[sd])
                        if isq:
                            qtmp = Bb[2]
                            P.op("dve", lambda e, ch=ch: e.scalar_tensor_tensor(
                                out=qtmp[:], in0=qf[:], scalar=vcol(V_GQ), in1=sd[:], op0=ALU.mult, op1=ALU.mult),
                                reads=[qf, sd, vecs], writes=[qtmp])
                            P.op("pool", lambda e, ch=ch: e.tensor_tensor(
                                out=qbd[:, ch, :, :], in0=qtmp[:].unsqueeze(1).broadcast_to([128, 2, TT]),
                                in1=cstb[:, C_CM2:C_CM2 + 2].unsqueeze(2).broadcast_to([128, 2, TT]), op=ALU.mult),
                                reads=[qtmp, cstb], writes=[("qbd", ch)])
                        else:
                            P.op("dve", lambda e, ch=ch, c0=c0: e.scalar_tensor_tensor(
                                out=KT[:, ch, c0:c0 + TT], in0=qf[:], scalar=vcol(V_GK), in1=sd[:],
                                op0=ALU.mult, op1=ALU.mult),
                                reads=[qf, sd, vecs], writes=[("KT", ch, t)])
                    for sub in range(NDIAG):
                        ps = tm_proj(512, 256, sub)
                        P.op("act", lambda e, ps=ps, sub=sub, t=t: e.activation(
                            out=Vc[:, NDIAG * t + sub, :, 0:64], in_=ps[:, 0:256].rearrange("p (h v) -> p h v", h=4),
                            func=AF.Copy), reads=[ps, "Vones"], writes=[("Vc", NDIAG * t + sub)])

                    prefetch()

                    def attn_block(h, qlo, qn_cols, ktiles, bias_col_fn, o_lo):
                        ch = h // 2
                        off2 = 64 * (h % 2)
                        LA = 3
                        pend = []
                        nk = len(ktiles)
                        for idx in range(nk + LA):
                            if idx < nk:
                                kt, m = ktiles[idx]
                                cs = 0 if m is None else 128 * m
                                n = qn_cols - cs
                                sp_ = pring.get()
                                P.op("pe", lambda e: e.matmul(
                                    sp_[:, 0:2 * n], lhsT=KT[off2:off2 + 64, ch, 128 * kt:128 * kt + 128],
                                    rhs=qbd[off2:off2 + 64, ch, :, qlo + cs:qlo + cs + n], start=True, stop=True,
                                    tile_position=(off2, 0)),
                                    reads=[("KT", ch, kt // NDIAG), ("qbd", ch)], writes=[sp_])
                                pt = pT.get()
                                bc = bias_col_fn(kt)
                                P.op("act", lambda e: e.activation(
                                    out=pt[:, 0:2 * n], in_=sp_[:, 0:2 * n], func=AF.Exp, bias=biasc[:, bc:bc + 1], scale=1.0),
                                    reads=[sp_, biasc], writes=[pt])
                                if m is not None:
                                    P.op("pool", lambda e: e.tensor_tensor(
                                        out=pt[:, 0:2 * n].rearrange("p (c n) -> p c n", c=2),
                                        in0=pt[:, 0:2 * n].rearrange("p (c n) -> p c n", c=2),
                                        in1=caus[:, 0:n].unsqueeze(1).broadcast_to([128, 2, n]), op=ALU.mult),
                                        reads=[pt, cstb], writes=[pt])
                                pend.append((pt, kt, cs, n))
                            if idx >= LA:
                                pt, kt, cs, n = pend[idx - LA]
                                first = (idx == LA)
                                for c in range(2):
                                    P.op("pe", lambda e: e.matmul(
                                        acc[c][0:65, o_lo + cs:o_lo + cs + n], lhsT=Vc[:, kt, h, :], rhs=pt[:, c * n:(c + 1) * n],
                                        start=first, stop=False), reads=[pt, ("Vc", kt)], writes=[acc[c]])

                    for h in range(4):
                        onrm = [F[4], F[5]]
                        if h == 0:
                            for qs in range(NDIAG):
                                gq = NDIAG * t + qs
                                kts = [(kt, None) for kt in range(max(0, gq - 4), gq)] + [(gq, 0)]
                                attn_block(h, 128 * qs, 128, kts, lambda kt, gq=gq: (gq - kt), 128 * qs)
                        else:
                            lo_kt = max(0, NDIAG * t - 16) if h == 1 else 0
                            kts = [(kt, None) for kt in range(lo_kt, NDIAG * t)] + [(NDIAG * t + m, m) for m in range(NDIAG)]
                            attn_block(h, 0, TT, kts,
                                       lambda kt, h=h, t=t: 5 + (h - 1) * ND + (NDIAG * t - kt) + (NDIAG - 1), 0)
                        for c in range(2):
                            o_ps = acc[c]
                            oa = F[2 + c]
                            P.op("act", lambda e, o_ps=o_ps, oa=oa: e.activation(out=oa[0:65, :], in_=o_ps[0:65, 0:TT],
                                                                               func=AF.Copy),
                                 reads=[o_ps], writes=[oa])
                            dps = pring.get()
                            P.op("pe", lambda e, dps=dps, oa=oa: e.matmul(dps[0:64, 0:TT], lhsT=Esel[0:65, :],
                                                                          rhs=oa[0:65, :], start=True, stop=True),
                                 reads=[Esel, oa], writes=[dps])
                            rd = F[6]
                            P.op("dve", lambda e, dps=dps: e.reciprocal(out=rd[0:64, :], in_=dps[0:64, 0:TT]),
                                 reads=[dps], writes=[rd])
                            P.op("dve", lambda e, oa=oa, c=c: e.tensor_tensor(
                                out=onrm[c][0:64, :], in0=oa[0:64, :], in1=rd[0:64, :], op=ALU.mult),
                                reads=[oa, rd], writes=[onrm[c]])
                        df = F[7]
                        P.op("dve", lambda e: e.scalar_tensor_tensor(
                            out=df[0:64, :], in0=onrm[1][0:64, :], scalar=neglam[0:64, :], in1=onrm[0][0:64, :],
                            op0=ALU.mult, op1=ALU.add), reads=[onrm[0], onrm[1], neglam], writes=[df])
                        sq = Bb[1]
                        P.op("act", lambda e: e.activation(out=sq[0:64, :], in_=df[0:64, :], func=AF.Square),
                             reads=[df], writes=[sq])
                        mps = pring.get()
                        P.op("pe", lambda e, mps=mps: e.matmul(mps[0:64, 0:TT], lhsT=ones_bf[0:64, 0:64], rhs=sq[0:64, :],
                                                               start=True, stop=True), reads=[ones_bf, sq], writes=[mps])
                        sd = F[8]
                        s1 = 1.0 - lam_init
                        P.op("act", lambda e, mps=mps, s1=s1: e.activation(
                            out=sd[0:64, :], in_=mps[0:64, 0:TT], func=AF.Sqrt, scale=1.0 / (64.0 * s1 * s1),
                            bias=RMS_EPS / (s1 * s1)), reads=[mps], writes=[sd])
                        P.op("dve", lambda e: e.reciprocal(out=sd[0:64, :], in_=sd[0:64, :]), reads=[sd], writes=[sd])
                        P.op("dve", lambda e, h=h: e.scalar_tensor_tensor(
                            out=mixA[0:64, h, :], in0=df[0:64, :], scalar=vcol(V_GA, 64), in1=sd[0:64, :],
                            op0=ALU.mult, op1=ALU.mult), reads=[df, sd, vecs], writes=[("mixA", h)])
                elif phase == "a":
                    for h in range(4):
                        P.op("pool", lambda e, h=h: e.memset(mixA[0:64, h, :], 0.0), writes=[("mixA", h)])

                def transpose_to(out_ap, in_ap, out_key, in_key, eng="act"):
                    tp = pring.get()
                    tpb = tp[:].bitcast(BF16)
                    P.op("pe", lambda e: e.transpose(tpb[:, 0:128], in_ap, ident), reads=[in_key, cstb], writes=[tp])
                    if eng == "act":
                        P.op("act", lambda e: e.activation(out=out_ap, in_=tpb[:, 0:128], func=AF.Copy),
                             reads=[tp], writes=[out_key])
                    else:
                        P.op("dve", lambda e: e.tensor_copy(out=out_ap, in_=tpb[:, 0:128]), reads=[tp], writes=[out_key])

                def lin_attn(kind):
                    if kind == "B":
                        RC, Kh, qcol, kcol, cbase, sc = 1, 32, 768, 896, 0, -1.0 / 16.0
                        grow = rows[:, R_GB:R_GB + 256]
                    else:
                        RC, Kh, qcol, kcol, cbase, sc = 2, 64, 1552, 1808, 2, 1.0
                        grow = rows[:, R_GC:R_GC + 256]
                    hpc = 128 // Kh
                    qt_l, kt_l = [], []
                    for sub in range(NDIAG):
                        if kind == "B":
                            psv = tm_proj(1024, 256, sub)
                            P.op("act", lambda e, psv=psv, sub=sub: e.activation(out=Vtm[:, sub, :], in_=psv[:, 0:256], func=AF.Copy),
                                 reads=[psv], writes=[("Vtm", sub)])
                            pso = tm_proj(1296, 256, sub)
                            P.op("act", lambda e, pso=pso, sub=sub: e.activation(out=sog[:, sub, :], in_=pso[:, 0:256], func=AF.Silu),
                                 reads=[pso], writes=[("sog", sub)])
                        else:
                            psv = tm_proj(2064, 512, sub)
                            P.op("act", lambda e, psv=psv, sub=sub: e.activation(out=Vtm[:, sub, :], in_=psv[:, 0:256], func=AF.Copy),
                                 reads=[psv], writes=[("Vtm", sub)])
                            P.op("act", lambda e, psv=psv, sub=sub: e.activation(out=sog[:, sub, :], in_=psv[:, 256:512], func=AF.Silu),
                                 reads=[psv], writes=[("sog", sub)])
                    for rc in range(RC):
                        qt, ktl, kh_ = Bb[2 + rc], Bb[4 + rc], Bb[6]
                        qt_l.append(qt); kt_l.append(ktl)
                        lf, Lc, eq, ek, dd = F[0], F[1], F[2], F[3], F[4]
                        if kind == "B":
                            psg = fm_proj(1280, 16)
                            gdb = Bb[7]
                            P.op("act", lambda e, psg=psg: e.activation(out=gdb[0:16, :], in_=psg[0:16, 0:TT], func=AF.Copy),
                                 reads=[psg], writes=[gdb])
                            pre = pring.get()
                            P.op("pe", lambda e, pre=pre: e.matmul(pre[:, 0:TT], lhsT=swb[0:16, SW_GU:SW_GU + 128], rhs=gdb[0:16, :],
                                                                   start=True, stop=True), reads=[swb, gdb], writes=[pre])
                            e1 = F[5]
                            P.op("act", lambda e, pre=pre: e.activation(out=e1[:], in_=pre[:, 0:TT], func=AF.Exp, scale=-1.0,
                                                                        bias=negb[:, 0:1]), reads=[pre, negb], writes=[e1])
                            P.op("act", lambda e: e.activation(out=lf[:], in_=e1[:], func=AF.Ln, bias=1.0, scale=1.0),
                                 reads=[e1], writes=[lf])
                            psq = fm_proj(qcol, 128)
                            psk = fm_proj(kcol, 128)
                            kv = None
                        else:
                            psf = fm_proj(kcol + 128 * rc, 128)
                            sg, fg, kvf = F[5], F[6], F[7]
                            ez = F[11]
                            P.op("act", lambda e, psf=psf: e.activation(out=ez[:], in_=psf[:, 0:TT], func=AF.Exp, scale=-1.0),
                                 reads=[psf], writes=[ez])
                            P.op("dve", lambda e: e.tensor_scalar(out=sg[:], in0=ez[:], scalar1=1.0, scalar2=None, op0=ALU.add),
                                 reads=[ez], writes=[sg])
                            P.op("dve", lambda e: e.reciprocal(out=sg[:], in_=sg[:]), reads=[sg], writes=[sg])
                            lcol = rc * L_total + lid
                            P.op("dve", lambda e, lcol=lcol: e.tensor_scalar(
                                out=fg[:], in0=sg[:], scalar1=omlT[:, lcol:lcol + 1], scalar2=lbT[:, lcol:lcol + 1],
                                op0=ALU.mult, op1=ALU.add), reads=[sg, omlT, lbT], writes=[fg])
                            P.op("dve", lambda e: e.tensor_scalar(out=fg[:], in0=fg[:], scalar1=1e-30, scalar2=None, op0=ALU.max),
                                 reads=[fg], writes=[fg])
                            P.op("act", lambda e: e.activation(out=lf[:], in_=fg[:], func=AF.Ln), reads=[fg], writes=[lf])
                            P.op("dve", lambda e, lcol=lcol: e.scalar_tensor_tensor(
                                out=kvf[:], in0=ez[:], scalar=omlT[:, lcol:lcol + 1], in1=sg[:], op0=ALU.mult, op1=ALU.mult),
                                reads=[ez, sg, omlT], writes=[kvf])
                            psq = fm_proj(qcol + 128 * rc, 128)
                            qsl = F[8]
                            P.op("act", lambda e, psq=psq: e.activation(out=qsl[:], in_=psq[:, 0:TT], func=AF.Silu),
                                 reads=[psq], writes=[qsl])
                            kv = kvf
                        P.op("dve", lambda e: e.tensor_tensor_scan(out=Lc[:], data0=cstb[:, C_RS32:C_RS32 + TT], data1=lf[:],
                                                                   initial=0.0, op0=ALU.mult, op1=ALU.add),
                             reads=[cstb, lf], writes=[Lc])
                        Lc3 = Lc[:].rearrange("p (c j) -> p c j", j=32)
                        if debug and kind == "B" and t == 0:
                            P.op("dve", lambda e: e.tensor_copy(out=F[14][0:64, :], in_=swb[:, 0:256]), reads=[swb], writes=[F[14]])
                            P.dma("sp", lambda e: e.dma_start(out=dbg[768:832, 0:256], in_=F[14][0:64, :]), reads=[F[14]])
                            P.op("dve", lambda e: e.tensor_copy(out=F[13][0:32, :], in_=gdb[0:32, :]), reads=[gdb], writes=[F[13]])
                            P.dma("sp", lambda e: e.dma_start(out=dbg[832:864, 0:256], in_=F[13][0:32, :]), reads=[F[13]])
                        if debug and kind == "B" and False:
                            P.dma("sp", lambda e, c0=c0: e.dma_start(out=dbg[768:896, c0:c0 + TT], in_=Lc[:]), reads=[Lc])
                            P.dma("sp", lambda e, c0=c0: e.dma_start(out=dbg[896:1024, c0:c0 + TT], in_=lf[:]), reads=[lf])
                        P.op("dve", lambda e: e.tensor_copy(out=ltot[:], in_=Lc3[:, :, 31]), reads=[Lc], writes=[ltot])
                        P.op("act", lambda e: e.activation(out=eq[:], in_=Lc[:], func=AF.Exp, scale=sc), reads=[Lc], writes=[eq])
                        P.op("act", lambda e: e.activation(out=ek[:], in_=Lc[:], func=AF.Exp, scale=-sc), reads=[Lc], writes=[ek])
                        P.op("dve", lambda e: e.tensor_tensor(
                            out=dd[:].rearrange("p (c j) -> p c j", j=32), in0=ltot[:].unsqueeze(2).broadcast_to([128, 8, 32]),
                            in1=Lc3, op=ALU.subtract), reads=[ltot, Lc], writes=[dd])
                        P.op("act", lambda e: e.activation(out=dd[:], in_=dd[:], func=AF.Exp, scale=sc), reads=[dd], writes=[dd])
                        g_ = gam[rc]
                        P.op("act", lambda e, g_=g_: e.activation(out=g_[:], in_=ltot[:], func=AF.Exp, scale=sc),
                             reads=[ltot], writes=[g_])
                        if kind == "B":
                            P.op("dve", lambda e, psq=psq, qt=qt: e.scalar_tensor_tensor(
                                out=qt[:], in0=psq[:, 0:TT], scalar=32.0 ** -0.5, in1=eq[:], op0=ALU.mult, op1=ALU.mult),
                                reads=[psq, eq], writes=[qt])
                            P.op("dve", lambda e, psk=psk, ktl=ktl: e.tensor_tensor(out=ktl[:], in0=psk[:, 0:TT], in1=ek[:], op=ALU.mult),
                                 reads=[psk, ek], writes=[ktl])
                            P.op("dve", lambda e, psk=psk: e.tensor_tensor(out=kh_[:], in0=psk[:, 0:TT], in1=dd[:], op=ALU.mult),
                                 reads=[psk, dd], writes=[kh_])
                        else:
                            P.op("dve", lambda e, qt=qt: e.tensor_tensor(out=qt[:], in0=qsl[:], in1=eq[:], op=ALU.mult),
                                 reads=[qsl, eq], writes=[qt])
                            P.op("dve", lambda e, ktl=ktl: e.tensor_tensor(out=ktl[:], in0=kv[:], in1=ek[:], op=ALU.mult),
                                 reads=[kv, ek], writes=[ktl])
                            P.op("dve", lambda e: e.tensor_tensor(out=kh_[:], in0=kv[:], in1=dd[:], op=ALU.mult),
                                 reads=[kv, dd], writes=[kh_])
                        for sub in range(NDIAG):
                            transpose_to(khat[:, sub, 128 * rc:128 * rc + 128], kh_[:, 128 * sub:128 * sub + 128],
                                         ("khat", sub, rc), kh_)
                        W = 64 * hpc
                        wb = W * rc
                        D3 = Dsb[:].rearrange("p (v c) -> p v c", c=8)
                        G3 = Grep[:].rearrange("p (v c) -> p v c", c=8)
                        for sub in range(NDIAG):
                            P.op("pool", lambda e, sub=sub: e.tensor_tensor(
                                out=Vm[:], in0=Vtm[:, sub, :].unsqueeze(1).broadcast_to([128, 4, 256]),
                                in1=cstb[:, C_HM32:C_HM32 + 4].unsqueeze(2).broadcast_to([128, 4, 256]), op=ALU.mult),
                                reads=[("Vtm", sub), cstb], writes=[("Vm", 0)])
                            nslot = 512 // W
                            for c0_ in range(0, 4, nslot):
                                dps = pring.get()
                                for sl_ in range(nslot):
                                    cc = c0_ + sl_
                                    P.op("pe", lambda e, dps=dps, sl_=sl_, cc=cc, sub=sub: e.matmul(
                                        dps[:, sl_ * W:(sl_ + 1) * W], lhsT=khat[:, sub, 128 * rc:128 * rc + 128],
                                        rhs=Vm[:, cc, wb:wb + W], start=True, stop=True),
                                        reads=[("khat", sub, rc), ("Vm", 0)], writes=[dps])
                                for hh in range(hpc):
                                    hoff = Kh * hh
                                    cg = 4 * sub + c0_
                                    P.op("dve", lambda e, dps=dps, hh=hh, hoff=hoff, cg=cg: e.tensor_copy(
                                        out=D3[hoff:hoff + Kh, :, cg:cg + nslot],
                                        in_=dps[hoff:hoff + Kh, 0:nslot * W].rearrange("p (s w) -> p w s", w=W)[:, 64 * hh:64 * hh + 64, :]),
                                        reads=[dps], writes=[Dsb])
                            if kind == "C" and rc == 0 and sub == 0:
                                pass
                        cr = carry[(0 if kind == "B" else 1) + rc]
                        P.op("act", lambda e, g_=g_: e.activation(out=G3, in_=g_[:].unsqueeze(1).broadcast_to([128, 64, 8]),
                                                                  func=AF.Copy), reads=[g_], writes=[Grep])
                        P.op("pool", lambda e: e.memset(G3[:, :, 0:1], 0.0), reads=[], writes=[Grep])
                        P.op("dve", lambda e, cr=cr, g_=g_: e.scalar_tensor_tensor(
                            out=D3[:, :, 0], in0=cr[:], scalar=g_[:, 0:1], in1=D3[:, :, 0], op0=ALU.mult, op1=ALU.add),
                            reads=[cr, g_, Dsb], writes=[Dsb])
                        P.op("dve", lambda e: e.tensor_tensor_scan(out=Hs[:], data0=Grep[:], data1=Dsb[:], initial=0.0,
                                                                   op0=ALU.mult, op1=ALU.add), reads=[Grep, Dsb], writes=[Hs])
                        hb = HbB if kind == "B" else HbC[rc]
                        Hs_cv = Hs[:].rearrange("p (v c) -> p c v", c=8)
                        for hh in range(hpc):
                            hoff = Kh * hh
                            P.op("act", lambda e, hb=hb, cr=cr, hh=hh, hoff=hoff: e.activation(
                                out=hb[hoff:hoff + Kh, 0, 64 * hh:64 * hh + 64], in_=cr[hoff:hoff + Kh, :], func=AF.Copy),
                                reads=[cr], writes=[hb])
                            P.op("act", lambda e, hb=hb, hh=hh, hoff=hoff: e.activation(
                                out=hb[hoff:hoff + Kh, 1:8, 64 * hh:64 * hh + 64], in_=Hs_cv[hoff:hoff + Kh, 0:7, :], func=AF.Copy),
                                reads=[Hs], writes=[hb])
                        P.op("dve", lambda e, cr=cr: e.tensor_copy(out=cr[:], in_=Hs[:].rearrange("p (v c) -> p v c", c=8)[:, :, 7]),
                             reads=[Hs], writes=[cr])
                    W = 64 * hpc
                    hmK = cstb[:, C_HM32:C_HM32 + 4] if kind == "B" else cstb[:, C_HM64:C_HM64 + 2]
                    for sub in range(NDIAG):
                        sps = pring.get()
                        tsl = slice(128 * sub, 128 * sub + 128)
                        for rc in range(RC):
                            P.op("pool", lambda e, rc=rc, tsl=tsl: e.tensor_tensor(
                                out=Qbd[rc][:, 0:hpc * 128].rearrange("p (h i) -> p h i", h=hpc),
                                in0=qt_l[rc][:, tsl].unsqueeze(1).broadcast_to([128, hpc, 128]),
                                in1=hmK.unsqueeze(2).broadcast_to([128, hpc, 128]), op=ALU.mult),
                                reads=[qt_l[rc], cstb], writes=[Qbd[rc]])
                            P.op("pe", lambda e, rc=rc, tsl=tsl, sps=sps: e.matmul(
                                sps[:, 128 * hpc * rc:128 * hpc * (rc + 1)], lhsT=kt_l[rc][:, tsl], rhs=Qbd[rc][:, 0:hpc * 128],
                                start=True, stop=True), reads=[kt_l[rc], Qbd[rc]], writes=[sps])
                            P.op("pool", lambda e, rc=rc, tsl=tsl: e.tensor_tensor(
                                out=Qm[rc][:], in0=qt_l[rc][:, tsl].unsqueeze(1).broadcast_to([128, 4, 128]),
                                in1=cstb[:, C_TM4:C_TM4 + 512].rearrange("p (c i) -> p c i", c=4), op=ALU.mult),
                                reads=[qt_l[rc], cstb], writes=[Qm[rc]])
                        P.op("dve", lambda e, sps=sps: e.tensor_tensor(
                            out=Smk[:], in0=sps[:].rearrange("p (h i) -> p h i", h=4),
                            in1=cstb[:, C_BD32:C_BD32 + 128].unsqueeze(1).broadcast_to([128, 4, 128]), op=ALU.mult),
                            reads=[sps, cstb], writes=[Smk])
                        P.op("pool", lambda e, sub=sub: e.tensor_tensor(
                            out=Vbd[:], in0=Vtm[:, sub, :].unsqueeze(1).broadcast_to([128, 4, 256]),
                            in1=cstb[:, C_CM:C_CM + 1024].rearrange("p (h c) -> p h c", h=4), op=ALU.mult),
                            reads=[("Vtm", sub), cstb], writes=[Vbd])
                        ops_ = pring.get()
                        for h in range(4):
                            P.op("pe", lambda e, h=h, ops_=ops_: e.matmul(
                                ops_[:, 0:256], lhsT=Smk[:, h, :], rhs=Vbd[:, h, :], start=(h == 0), stop=False),
                                reads=[Smk, Vbd], writes=[ops_])
                        for rc in range(RC):
                            hb = HbB if kind == "B" else HbC[rc]
                            for cc in range(4):
                                last = (rc == RC - 1 and cc == 3)
                                P.op("pe", lambda e, rc=rc, cc=cc, hb=hb, last=last, ops_=ops_: e.matmul(
                                    ops_[:, W * rc:W * (rc + 1)], lhsT=Qm[rc][:, cc, :], rhs=hb[:, 4 * sub + cc, :],
                                    start=False, stop=last), reads=[Qm[rc], hb], writes=[ops_])
                        sqo, on = F[9], F[10]
                        P.op("act", lambda e, ops_=ops_: e.activation(out=sqo[:], in_=ops_[:, 0:256], func=AF.Square),
                             reads=[ops_], writes=[sqo])
                        P.op("dve", lambda e: e.tensor_reduce(out=st4[:, 0:4], in_=sqo[:].rearrange("p (h v) -> p h v", h=4),
                                                              axis=AX.X, op=ALU.add), reads=[sqo], writes=[st4])
                        P.op("act", lambda e: e.activation(out=st4[:, 4:8], in_=st4[:, 0:4], func=AF.Sqrt, scale=1.0 / 64.0,
                                                           bias=RMS_EPS), reads=[st4], writes=[st4])
                        P.op("dve", lambda e: e.reciprocal(out=st4[:, 8:12], in_=st4[:, 4:8]), reads=[st4], writes=[st4])
                        P.op("dve", lambda e, ops_=ops_: e.tensor_tensor(
                            out=on[:].rearrange("p (h v) -> p h v", h=4), in0=ops_[:, 0:256].rearrange("p (h v) -> p h v", h=4),
                            in1=st4[:, 8:12].unsqueeze(2).broadcast_to([128, 4, 64]), op=ALU.mult),
                            reads=[ops_, st4], writes=[on])
                        P.op("dve", lambda e: e.tensor_tensor(out=on[:], in0=on[:], in1=grow, op=ALU.mult),
                             reads=[on, rows], writes=[on])
                        P.op("dve", lambda e, sub=sub: e.tensor_tensor(out=mtm[:], in0=on[:], in1=sog[:, sub, :], op=ALU.mult),
                             reads=[on, ("sog", sub)], writes=[mtm])
                        for j in range(2):
                            transpose_to(mixB[:, cbase + j, 128 * sub:128 * sub + 128], mtm[:, 128 * j:128 * j + 128],
                                         ("mixB", cbase + j), mtm, eng="dve")

                if phase == "b" and "B" in mixers:
                    lin_attn("B")
                if phase == "b":
                    prefetch()
                if phase == "b" and "C" in mixers:
                    lin_attn("C")
                def rwkv():
                    CD = 0.606531
                    hm64 = cstb[:, C_HM64:C_HM64 + 2]

                    def shifted(col0, nrows, mucol, pidx, out_ap, out_key):
                        ps = fm_proj(col0, nrows)
                        P.op("pool", lambda e: e.tensor_copy(out=pf[0:nrows, 0:1], in_=prevc[0:nrows, pidx:pidx + 1]),
                             reads=[prevc], writes=[pf])
                        P.op("act", lambda e: e.activation(out=pf[0:nrows, 1:TT + 1], in_=ps[0:nrows, 0:TT], func=AF.Copy),
                             reads=[ps], writes=[pf])
                        dtmp = F[15]
                        P.op("pool", lambda e: e.tensor_tensor(out=dtmp[0:nrows, :], in0=pf[0:nrows, 0:TT], in1=pf[0:nrows, 1:TT + 1],
                                                               op=ALU.subtract), reads=[pf], writes=[dtmp])
                        P.op("dve", lambda e: e.scalar_tensor_tensor(
                            out=out_ap, in0=dtmp[0:nrows, :], scalar=vcol(mucol, nrows), in1=pf[0:nrows, 1:TT + 1],
                            op0=ALU.mult, op1=ALU.add), reads=[dtmp, pf, vecs], writes=[out_key])
                        P.op("pool", lambda e: e.tensor_copy(out=prevc[0:nrows, pidx:pidx + 1], in_=pf[0:nrows, TT:TT + 1]),
                             reads=[pf], writes=[prevc])

                    tw, adb, sgd = Bb[0], Bb[1], Bb[2]
                    wdf = F[14]
                    shifted(3344, 32, V_MU + 6, 6, wdf[0:32, :], wdf)
                    P.op("act", lambda e: e.activation(out=tw[0:32, :], in_=wdf[0:32, :], func=AF.Tanh), reads=[wdf], writes=[tw])
                    shifted(3376, 32, V_MU + 7, 7, wdf[0:32, :], wdf)
                    P.op("act", lambda e: e.activation(out=adb[0:32, :], in_=wdf[0:32, :], func=AF.Copy), reads=[wdf], writes=[adb])
                    shifted(3408, 64, V_MU + 8, 8, wdf[0:64, :], wdf)
                    P.op("act", lambda e: e.activation(out=sgd[0:64, :], in_=wdf[0:64, :], func=AF.Sigmoid), reads=[wdf], writes=[sgd])

                    for rc in range(2):
                        rp, kp, vp, sgw, av, gT, kkn, kmod, bonus, Lw, Lx, eX, qa, dd = (F[i] for i in range(14))
                        shifted(2576 + 128 * rc, 128, V_MU + rc, rc, rp[:], rp)
                        shifted(2832 + 128 * rc, 128, V_MU + 2 + rc, 2 + rc, kp[:], kp)
                        shifted(3088 + 128 * rc, 128, V_MU + 4 + rc, 4 + rc, vp[:], vp)
                        P.op("act", lambda e: e.activation(out=vpb[:], in_=vp[:], func=AF.Copy), reads=[vp], writes=[vpb])
                        wps = pring.get()
                        P.op("pe", lambda e: e.matmul(wps[:, 0:TT], lhsT=swb[0:32, SW_WU + 128 * rc:SW_WU + 128 * rc + 128],
                                                      rhs=tw[0:32, :], start=True, stop=True), reads=[swb, tw], writes=[wps])
                        P.op("act", lambda e: e.activation(out=sgw[:], in_=wps[:, 0:TT], func=AF.Sigmoid, bias=vcol(V_W0 + rc), scale=1.0),
                             reads=[wps, vecs], writes=[sgw])
                        aps = pring.get()
                        P.op("pe", lambda e: e.matmul(aps[:, 0:TT], lhsT=swb[0:32, SW_AU + 128 * rc:SW_AU + 128 * rc + 128],
                                                      rhs=adb[0:32, :], start=True, stop=True), reads=[swb, adb], writes=[aps])
                        P.op("act", lambda e: e.activation(out=av[:], in_=aps[:, 0:TT], func=AF.Sigmoid, bias=vcol(V_A0 + rc), scale=1.0),
                             reads=[aps, vecs], writes=[av])
                        gps = pring.get()
                        P.op("pe", lambda e: e.matmul(gps[:, 0:TT], lhsT=swb[0:64, SW_GUP + 128 * rc:SW_GUP + 128 * rc + 128],
                                                      rhs=sgd[0:64, :], start=True, stop=True), reads=[swb, sgd], writes=[gps])
                        P.op("act", lambda e: e.activation(out=gT[:], in_=gps[:, 0:TT], func=AF.Copy), reads=[gps], writes=[gT])
                        P.op("dve", lambda e: e.tensor_scalar(out=kkn[:], in0=kp[:], scalar1=vcol(V_KK + rc), scalar2=None, op0=ALU.mult),
                             reads=[kp, vecs], writes=[kkn])
                        sqk = Bb[3]
                        P.op("act", lambda e: e.activation(out=sqk[:], in_=kkn[:], func=AF.Square), reads=[kkn], writes=[sqk])
                        sps_ = pring.get()
                        P.op("pe", lambda e: e.matmul(sps_[:, 0:TT], lhsT=bo64[:], rhs=sqk[:], start=True, stop=True),
                             reads=[bo64, sqk], writes=[sps_])
                        P.op("dve", lambda e: e.tensor_scalar(out=eX[:], in0=sps_[:, 0:TT], scalar1=1e-24, scalar2=None, op0=ALU.max),
                             reads=[sps_], writes=[eX])
                        P.op("act", lambda e: e.activation(out=eX[:], in_=eX[:], func=AF.Sqrt), reads=[eX], writes=[eX])
                        P.op("dve", lambda e: e.reciprocal(out=eX[:], in_=eX[:]), reads=[eX], writes=[eX])
                        P.op("dve", lambda e: e.tensor_tensor(out=kkn[:], in0=kkn[:], in1=eX[:], op=ALU.mult), reads=[kkn, eX], writes=[kkn])
                        P.op("dve", lambda e: e.tensor_scalar(out=kmod[:], in0=av[:], scalar1=-1.0, scalar2=vcol(V_KA + rc),
                                                              op0=ALU.add, op1=ALU.mult), reads=[av, vecs], writes=[kmod])
                        P.op("dve", lambda e: e.scalar_tensor_tensor(out=kmod[:], in0=kmod[:], scalar=1.0, in1=kp[:],
                                                                     op0=ALU.add, op1=ALU.mult), reads=[kmod, kp], writes=[kmod])
                        rkb = Bb[4]
                        P.op("dve", lambda e: e.scalar_tensor_tensor(out=rkb[:], in0=rp[:], scalar=vcol(V_RK + rc), in1=kmod[:],
                                                                     op0=ALU.mult, op1=ALU.mult), reads=[rp, kmod, vecs], writes=[rkb])
                        bps = pring.get()
                        P.op("pe", lambda e: e.matmul(bps[:, 0:TT], lhsT=bo64[:], rhs=rkb[:], start=True, stop=True),
                             reads=[bo64, rkb], writes=[bps])
                        P.op("dve", lambda e: e.tensor_tensor(out=bonus[:], in0=bps[:, 0:TT], in1=vp[:], op=ALU.mult),
                             reads=[bps, vp], writes=[bonus])
                        P.op("dve", lambda e: e.tensor_tensor_scan(out=Lw[:], data0=cstb[:, C_RS64:C_RS64 + TT], data1=sgw[:],
                                                                   initial=0.0, op0=ALU.mult, op1=ALU.add),
                             reads=[cstb, sgw], writes=[Lw])
                        P.op("pool", lambda e: e.tensor_tensor(out=Lx[:], in0=Lw[:], in1=sgw[:], op=ALU.subtract),
                             reads=[Lw, sgw], writes=[Lx])
                        Lw3 = Lw[:].rearrange("p (c j) -> p c j", j=64)
                        P.op("dve", lambda e: e.tensor_copy(out=ltot4[:], in_=Lw3[:, :, 63]), reads=[Lw], writes=[ltot4])
                        g4 = gam4[rc]
                        P.op("act", lambda e: e.activation(out=g4[:], in_=ltot4[:], func=AF.Exp, scale=-CD), reads=[ltot4], writes=[g4])
                        PR4 = PRb[:].rearrange("p s (w t) -> p s w t", w=2)
                        P.op("act", lambda e: e.activation(out=eX[:], in_=Lw[:], func=AF.Exp, scale=-CD), reads=[Lw], writes=[eX])
                        P.op("dve", lambda e: e.tensor_tensor(out=PR4[:, :, 1, :], in0=rp[:].rearrange("p (s t) -> p s t", s=2),
                                                              in1=eX[:].rearrange("p (s t) -> p s t", s=2), op=ALU.mult),
                             reads=[rp, eX], writes=[PRb])
                        P.op("act", lambda e: e.activation(out=eX[:], in_=Lw[:], func=AF.Exp, scale=CD), reads=[Lw], writes=[eX])
                        P.op("dve", lambda e: e.tensor_tensor(out=Ktb[:], in0=kmod[:], in1=eX[:], op=ALU.mult), reads=[kmod, eX], writes=[Ktb])
                        P.op("pool", lambda e: e.tensor_tensor(out=qa[:], in0=kkn[:], in1=av[:], op=ALU.mult), reads=[kkn, av], writes=[qa])
                        P.op("dve", lambda e: e.tensor_tensor(out=Qtb[:], in0=qa[:], in1=eX[:], op=ALU.mult), reads=[qa, eX], writes=[Qtb])
                        P.op("act", lambda e: e.activation(out=eX[:], in_=Lx[:], func=AF.Exp, scale=-CD), reads=[Lx], writes=[eX])
                        P.op("dve", lambda e: e.scalar_tensor_tensor(
                            out=PR4[:, :, 0, :], in0=kkn[:].rearrange("p (s t) -> p s t", s=2), scalar=-1.0,
                            in1=eX[:].rearrange("p (s t) -> p s t", s=2), op0=ALU.mult, op1=ALU.mult),
                            reads=[kkn, eX], writes=[PRb])
                        P.op("dve", lambda e: e.tensor_tensor(
                            out=dd[:].rearrange("p (c j) -> p c j", j=64), in0=ltot4[:].unsqueeze(2).broadcast_to([128, 4, 64]),
                            in1=Lw3, op=ALU.subtract), reads=[ltot4, Lw], writes=[dd])
                        P.op("act", lambda e: e.activation(out=dd[:], in_=dd[:], func=AF.Exp, scale=-CD), reads=[dd], writes=[dd])
                        P.op("dve", lambda e: e.tensor_tensor(out=Khb[:], in0=kmod[:], in1=dd[:], op=ALU.mult), reads=[kmod, dd], writes=[Khb])
                        P.op("pool", lambda e: e.tensor_tensor(out=Qhb[:], in0=qa[:], in1=dd[:], op=ALU.mult), reads=[qa, dd], writes=[Qhb])

                        hx = Hexp[rc]
                        hcur = [Hbf[0]]
                        hring = Ring(Hbf)
                        hring.i = 1
                        P.op("act", lambda e: e.activation(out=Hbf[0][:], in_=hx[:], func=AF.Copy), reads=[hx], writes=[Hbf[0]])
                        for sub in range(NDIAG):
                            tsl = slice(128 * sub, 128 * sub + 128)
                            transpose_to(Vdtm[:], vpb[:, tsl], Vdtm, vpb)
                            transpose_to(Ptm[:], PR4[:, sub, 0, :], Ptm, PRb)
                            transpose_to(Qhtm[:], Qhb[:, tsl], Qhtm, Qhb)
                            transpose_to(Khtm[:], Khb[:, tsl], Khtm, Khb)
                            P.op("pool", lambda e: e.tensor_tensor(
                                out=PRm[:], in0=PRb[:, sub, :].unsqueeze(1).broadcast_to([128, 2, 256]),
                                in1=hm64.unsqueeze(2).broadcast_to([128, 2, 256]), op=ALU.mult), reads=[PRb, cstb], writes=[PRm])
                            P.op("pool", lambda e: e.tensor_tensor(
                                out=Rm[:], in0=PR4[:, sub, 1, :].unsqueeze(1).broadcast_to([128, 2, 128]),
                                in1=cstb[:, C_TM2:C_TM2 + 256].rearrange("p (j t) -> p j t", j=2), op=ALU.mult),
                                reads=[PRb, cstb], writes=[Rm])
                            P.op("pool", lambda e: e.tensor_tensor(
                                out=Vmj[:], in0=Vdtm[:].unsqueeze(1).broadcast_to([128, 2, 128]),
                                in1=hm64.unsqueeze(2).broadcast_to([128, 2, 128]), op=ALU.mult), reads=[Vdtm, cstb], writes=[Vmj])
                            wcur = 0
                            Mcur = [None, None]
                            Ncur = [None, None]
                            W0 = Wb[0]
                            for hh in range(2):
                                AM = AMb[hh]
                                mps = pring.get()
                                P.op("pe", lambda e: e.matmul(mps[:, 0:256], lhsT=Qtb[:, tsl], rhs=PRm[:, hh, :], start=True, stop=True),
                                     reads=[Qtb, PRm], writes=[mps])
                                P.op("pe", lambda e: e.matmul(mps[:, 256:512], lhsT=Ktb[:, tsl], rhs=PRm[:, hh, :], start=True, stop=True),
                                     reads=[Ktb, PRm], writes=[mps])
                                P.op("dve", lambda e: e.tensor_tensor(
                                    out=AM[:], in0=mps[:].rearrange("p (a b) -> p a b", a=2),
                                    in1=cstb[:, C_ST64:C_ST64 + 256].unsqueeze(1).broadcast_to([128, 2, 256]), op=ALU.mult),
                                    reads=[mps, cstb], writes=[AM])
                                lps = pring.get()
                                P.op("pe", lambda e: e.matmul(lps[:, 0:128], lhsT=PRm[:, hh, 0:128], rhs=Qtb[:, tsl], start=True, stop=True),
                                     reads=[Qtb, PRm], writes=[lps])
                                N0 = Nb[hh][2]
                                P.op("dve", lambda e: e.tensor_tensor(out=N0[:], in0=lps[:, 0:128], in1=cstb[:, C_LO64:C_LO64 + 128],
                                                                      op=ALU.mult), reads=[lps, cstb], writes=[N0])
                                Ncur[hh] = N0
                                Mcur[hh] = (AM[:, 0, 0:128], AM)
                                avp = pring.get()
                                P.op("pe", lambda e: e.matmul(avp[:, 0:64], lhsT=AM[:, 1, 0:128], rhs=Vdtm[:, 64 * hh:64 * hh + 64],
                                                              start=True, stop=True), reads=[AM, Vdtm], writes=[avp])
                                P.op("act", lambda e: e.activation(out=W0[:, hh, 64:128], in_=avp[:, 0:64], func=AF.Copy),
                                     reads=[avp], writes=[(W0.name, hh)])
                                P.op("pool", lambda e: e.tensor_copy(out=W0[:, hh, 0:64], in_=Ptm[:, 64 * hh:64 * hh + 64]),
                                     reads=[Ptm], writes=[(W0.name, hh)])
                            wi = 0
                            for i in range(6):
                                Wc, Wn = Wb[wi], Wb[1 - wi]
                                for hh in range(2):
                                    Mcur_ap, Mcur_key = Mcur[hh]
                                    Nc = Ncur[hh]
                                    ups = pring.get()
                                    P.op("pe", lambda e: e.matmul(ups[:, 0:128], lhsT=Mcur_ap, rhs=Wc[:, hh, :], start=True, stop=True),
                                         reads=[Mcur_key, (Wc.name, hh)], writes=[ups])
                                    P.op("dve", lambda e: e.tensor_tensor(out=Wn[:, hh, :], in0=ups[:, 0:128], in1=Wc[:, hh, :], op=ALU.add),
                                         reads=[ups, (Wc.name, hh)], writes=[(Wn.name, hh)])
                                    if i < 5:
                                        Mn, Nn = Mb[hh][i % 2], Nb[hh][i % 2]
                                        m2 = pring.get()
                                        P.op("pe", lambda e: e.matmul(m2[:, 0:128], lhsT=Nc[:], rhs=Mcur_ap, start=True, stop=True),
                                             reads=[Nc, Mcur_key], writes=[m2])
                                        P.op("pe", lambda e: e.matmul(m2[:, 128:256], lhsT=Mcur_ap, rhs=Nc[:], start=True, stop=True),
                                             reads=[Nc, Mcur_key], writes=[m2])
                                        P.op("act", lambda e: e.activation(
                                            out=Mn[:], in_=m2[:, 0:128], func=AF.Copy), reads=[m2], writes=[Mn])
                                        P.op("act", lambda e: e.activation(
                                            out=Nn[:], in_=m2[:, 128:256], func=AF.Copy), reads=[m2], writes=[Nn])
                                        Mcur[hh] = (Mn[:], Mn)
                                        Ncur[hh] = Nn
                                wi = 1 - wi
                            wcur = wi
                            Wf = Wb[wcur]
                            P.op("pool", lambda e: e.tensor_copy(out=PH[:].rearrange("p (h k) -> p h k", h=2), in_=Wf[:, :, 0:64]),
                                 reads=[(Wf.name, 0), (Wf.name, 1)], writes=[PH])
                            transpose_to(PHT[:], PH[:], PHT, PH)
                            P.op("pool", lambda e: e.tensor_tensor(
                                out=Uhm[:].rearrange("p j (h v) -> p j h v", h=2),
                                in0=Wf[:, :, 64:128].unsqueeze(1).broadcast_to([128, 2, 2, 64]),
                                in1=hm64.unsqueeze(2).unsqueeze(3).broadcast_to([128, 2, 2, 64]), op=ALU.mult),
                                reads=[(Wf.name, 0), (Wf.name, 1), cstb], writes=[Uhm])
                            hstart = []
                            for j in range(2):
                                hb_c = hcur[0]
                                hstart.append(hb_c)
                                ups = pring.get()
                                P.op("pe", lambda e: e.matmul(ups[:, 0:128], lhsT=PHT[:], rhs=hb_c[:], start=True, stop=True),
                                     reads=[PHT, hb_c], writes=[ups])
                                P.op("dve", lambda e: e.scalar_tensor_tensor(
                                    out=Umb[:, j, :], in0=ups[:, 0:128], scalar=hm64[:, j:j + 1], in1=Uhm[:, j, :],
                                    op0=ALU.mult, op1=ALU.add), reads=[ups, Uhm, cstb], writes=[("Umb", j)])
                                hps = pring.get()
                                P.op("pe", lambda e: e.matmul(hps[:, 0:128], lhsT=Qhtm[:], rhs=Umb[:, j, :], start=True, stop=False),
                                     reads=[Qhtm, ("Umb", j)], writes=[hps])
                                P.op("pe", lambda e: e.matmul(hps[:, 0:128], lhsT=Khtm[:], rhs=Vmj[:, j, :], start=False, stop=True),
                                     reads=[Khtm, Vmj], writes=[hps])
                                htmp = F[14]
                                P.op("dve", lambda e: e.tensor_tensor(out=htmp[:, 0:128], in0=hps[:, 0:128],
                                                                      in1=cstb[:, C_HM64B:C_HM64B + 128], op=ALU.mult),
                                     reads=[hps, cstb], writes=[htmp])
                                ci = 2 * sub + j
                                P.op("dve", lambda e: e.scalar_tensor_tensor(
                                    out=hx[:], in0=hx[:], scalar=g4[:, ci:ci + 1], in1=htmp[:, 0:128], op0=ALU.mult, op1=ALU.add),
                                    reads=[hx, g4, htmp], writes=[hx])
                                hb_n = hring.get()
                                P.op("act", lambda e: e.activation(out=hb_n[:], in_=hx[:], func=AF.Copy), reads=[hx], writes=[hb_n])
                                hcur[0] = hb_n
                            P.op("pool", lambda e: e.tensor_tensor(out=Ucomb[:], in0=Umb[:, 0, :], in1=Umb[:, 1, :], op=ALU.add),
                                 reads=[("Umb", 0), ("Umb", 1)], writes=[Ucomb])
                            yps = pring.get()
                            for j in range(2):
                                hs_ = hstart[j]
                                P.op("pe", lambda e: e.matmul(yps[:, 0:128], lhsT=Rm[:, j, :], rhs=hs_[:], start=(j == 0), stop=False),
                                     reads=[Rm, hs_], writes=[yps])
                            for hh in range(2):
                                AM = AMb[hh]
                                P.op("pe", lambda e: e.matmul(yps[:, 64 * hh:64 * hh + 64], lhsT=AM[:, 0, 128:256],
                                                              rhs=Ucomb[:, 64 * hh:64 * hh + 64], start=False, stop=False),
                                     reads=[AM, Ucomb], writes=[yps])
                                P.op("pe", lambda e: e.matmul(yps[:, 64 * hh:64 * hh + 64], lhsT=AM[:, 1, 128:256],
                                                              rhs=Vdtm[:, 64 * hh:64 * hh + 64], start=False, stop=(hh == 1)),
                                     reads=[AM, Vdtm], writes=[yps])
                            y3 = ysb[:].rearrange("p (h v) -> p h v", h=2)
                            sqy = F[15]
                            P.op("act", lambda e: e.activation(out=ysb[:], in_=yps[:, 0:128], func=AF.Copy), reads=[yps], writes=[ysb])
                            P.op("act", lambda e: e.activation(out=sqy[:, 0:128], in_=yps[:, 0:128], func=AF.Square), reads=[yps], writes=[sqy])
                            P.op("dve", lambda e: e.tensor_reduce(out=lnst[:, 0:2], in_=y3, axis=AX.X, op=ALU.add), reads=[ysb], writes=[lnst])
                            P.op("dve", lambda e: e.tensor_reduce(out=lnst[:, 2:4], in_=sqy[:, 0:128].rearrange("p (h v) -> p h v", h=2),
                                                                  axis=AX.X, op=ALU.add), reads=[sqy], writes=[lnst])
                            P.op("dve", lambda e: e.tensor_scalar(out=lnst[:, 4:6], in0=lnst[:, 0:2], scalar1=1.0 / 64.0, scalar2=None,
                                                                  op0=ALU.mult), reads=[lnst], writes=[lnst])
                            P.op("dve", lambda e: e.tensor_tensor(out=lnst[:, 6:8], in0=lnst[:, 4:6], in1=lnst[:, 4:6], op=ALU.mult),
                                 reads=[lnst], writes=[lnst])
                            P.op("dve", lambda e: e.scalar_tensor_tensor(out=lnst[:, 8:10], in0=lnst[:, 2:4], scalar=1.0 / 64.0,
                                                                         in1=lnst[:, 6:8], op0=ALU.mult, op1=ALU.subtract),
                                 reads=[lnst], writes=[lnst])
                            P.op("act", lambda e: e.activation(out=lnst[:, 10:12], in_=lnst[:, 8:10], func=AF.Sqrt, bias=64e-5, scale=1.0),
                                 reads=[lnst], writes=[lnst])
                            P.op("dve", lambda e: e.reciprocal(out=lnst[:, 12:14], in_=lnst[:, 10:12]), reads=[lnst], writes=[lnst])
                            P.op("dve", lambda e: e.tensor_tensor(out=y3, in0=y3, in1=lnst[:, 4:6].unsqueeze(2).broadcast_to([128, 2, 64]),
                                                                  op=ALU.subtract), reads=[ysb, lnst], writes=[ysb])
                            P.op("dve", lambda e: e.tensor_tensor(out=ynb[:].rearrange("p (h v) -> p h v", h=2), in0=y3,
                                                                  in1=lnst[:, 12:14].unsqueeze(2).broadcast_to([128, 2, 64]), op=ALU.mult),
                                 reads=[ysb, lnst], writes=[ynb])
                            tp = pring.get()
                            tpb = tp[:].bitcast(BF16)
                            P.op("pe", lambda e: e.transpose(tpb[:, 0:128], ynb[:], ident), reads=[ynb, cstb], writes=[tp])
                            o1 = F[15]
                            P.op("dve", lambda e: e.tensor_scalar(out=o1[:, 0:128], in0=tpb[:, 0:128], scalar1=vcol(V_LNG + rc),
                                                                  scalar2=vcol(V_LNB + rc), op0=ALU.mult, op1=ALU.add),
                                 reads=[tp, vecs], writes=[o1])
                            P.op("pool", lambda e: e.tensor_tensor(out=o1[:, 0:128], in0=o1[:, 0:128], in1=bonus[:, tsl], op=ALU.add),
                                 reads=[o1, bonus], writes=[o1])
                            P.op("dve", lambda e: e.tensor_tensor(out=mixB[:, 4 + rc, tsl], in0=o1[:, 0:128], in1=gT[:, tsl], op=ALU.mult),
                                 reads=[o1, gT], writes=[("mixB", 4 + rc)])

                if phase == "b" and "D" in mixers:
                    rwkv()
                for c2 in range(6):
                    mixer = "BBCCDD"[c2]
                    if phase == "b" and mixer not in mixers:
                        P.op("pool", lambda e, c2=c2: e.memset(mixB[:, c2, :], 0.0), writes=[("mixB", c2)])

                if debug and phase == "a":
                    for h in range(4):
                        dt_ = F[9]
                        P.op("dve", lambda e, h=h: e.tensor_copy(out=dt_[0:64, :], in_=mixA[0:64, h, :]),
                             reads=[("mixA", h)], writes=[dt_])
                        P.dma("sp", lambda e, h=h, c0=c0: e.dma_start(out=dbg[64 * h:64 * h + 64, c0:c0 + TT],
                                                                       in_=dt_[0:64, :]), reads=[dt_])
                if debug and phase == "b":
                    for c2 in range(6 if "D" in mixers else 4):
                        dt_ = F[9]
                        P.op("dve", lambda e, c2=c2: e.tensor_copy(out=dt_[:], in_=mixB[:, c2, :]),
                             reads=[("mixB", c2)], writes=[dt_])
                        P.dma("sp", lambda e, c2=c2, c0=c0: e.dma_start(
                            out=dbg[256 + 128 * c2:256 + 128 * (c2 + 1), c0:c0 + TT], in_=dt_[:]), reads=[dt_])

                prefetch()
                for dc in range(8):
                    ps = pring.get()
                    if phase == "a":
                        for h in range(4):
                            P.op("pe", lambda e, ps=ps, h=h, dc=dc: e.matmul(
                                ps[:, 0:TT], lhsT=woA[0:64, h, 128 * dc:128 * (dc + 1)], rhs=mixA[0:64, h, :],
                                start=(h == 0), stop=(h == 3)), reads=["wo", ("mixA", h)], writes=[ps])
                        xm = F[12 + dc % 2]
                        P.dma("sp", lambda e: e.dma_start(out=xm[:], in_=x_src[128 * dc:128 * (dc + 1), c0:c0 + TT]), writes=[xm])
                        res_ap, res_key = xm[:], xm
                    else:
                        for c2 in range(6):
                            P.op("pe", lambda e, ps=ps, c2=c2, dc=dc: e.matmul(
                                ps[:, 0:TT], lhsT=woB[:, c2, 128 * dc:128 * (dc + 1)], rhs=mixB[:, c2, :],
                                start=(c2 == 0), stop=(c2 == 5)), reads=["wo", ("mixB", c2)], writes=[ps])
                        xm = F[12 + dc % 2]
                        P.dma("sp", lambda e, dc=dc, c0=c0, xm=xm, x_mid=x_mid: e.dma_start(
                            out=xm[:], in_=x_mid[128 * dc:128 * (dc + 1), c0:c0 + TT]), reads=[("xmid", t, dc)], writes=[xm])
                        res_ap, res_key = xm[:], xm
                    xo = F[10 + dc % 2]
                    P.op("dve", lambda e, ps=ps, res_ap=res_ap, xo=xo: e.tensor_tensor(
                        out=xo[:], in0=ps[:, 0:TT], in1=res_ap, op=ALU.add), reads=[ps, res_key], writes=[xo])
                    P.dma("sp", lambda e, dc=dc, c0=c0, xo=xo, x_mid=x_mid: e.dma_start(
                        out=x_mid[128 * dc:128 * (dc + 1), c0:c0 + TT], in_=xo[:]), reads=[xo], writes=[("xmid", t, dc)])

        barrier()
        if not do_mlp:
            for t in range(NT):
                c0 = t * TT
                for k in range(8):
                    P.dma("sp", lambda e, k=k, c0=c0: e.dma_start(out=xs[:, k, :], in_=x_mid[128 * k:128 * (k + 1), c0:c0 + TT]),
                          writes=[("xs", k)])
                    P.dma("sp", lambda e, k=k, c0=c0, x_dst=x_dst: e.dma_start(
                        out=x_dst[128 * k:128 * (k + 1), c0:c0 + TT], in_=xs[:, k, :]), reads=[("xs", k)])
            x_src = x_dst
            continue
        T2 = TT
        for cb in range(0, DFF, 512):
            P.dma("pool", lambda e, li=li, cb=cb: e.dma_start(
                out=wup[:, :, cb:cb + 512], in_=w_up_d[li, :, cb:cb + 512].rearrange("(k p) n -> p k n", p=128)),
                writes=["wup"])
        for kb in range(0, 32, 4):
            P.dma("pool", lambda e, li=li, kb=kb: e.dma_start(
                out=wdn[:, kb:kb + 4, :], in_=w_dn_d[li, 128 * kb:128 * (kb + 4), :].rearrange("(k p) n -> p k n", p=128)),
                writes=["wdn"])
        for t in range(S // T2):
            c0 = t * T2
            bi = t % 2
            if t == 0:
                rmsnorm_tile(x_mid, c0, T2, V_GMLP, FM[7], 0)
            hT = hT_l[bi]
            hT_keys = [("hT", bi, k) for k in range(8)]
            for fc in range(32):
                ps = pring.get()
                for k in range(8):
                    P.op("pe", lambda e, k=k, ps=ps, fc=fc: e.matmul(
                        ps[:, 0:T2], lhsT=wup[:, k, 128 * fc:128 * (fc + 1)], rhs=hT[:, k, 0:T2],
                        start=(k == 0), stop=(k == 7)), reads=["wup"] + hT_keys, writes=[ps])
                r_ = FM[fc % 2]
                P.op("act", lambda e, ps=ps, r_=r_: e.activation(out=r_[:, 0:T2], in_=ps[:, 0:T2], func=AF.Relu),
                     reads=[ps], writes=[r_])
                P.op("dve", lambda e, fc=fc, r_=r_: e.tensor_tensor(out=uT[:, fc, :], in0=r_[:, 0:T2], in1=r_[:, 0:T2],
                                                                  op=ALU.mult), reads=[r_], writes=[("uT", fc)])
            if t + 1 < S // T2:
                rmsnorm_tile(x_mid, (t + 1) * T2, T2, V_GMLP, FM[7], (t + 1) % 2)
            for dc in range(8):
                ps = pring.get()
                for fc in range(32):
                    P.op("pe", lambda e, ps=ps, fc=fc, dc=dc: e.matmul(
                        ps[:, 0:T2], lhsT=wdn[:, fc, 128 * dc:128 * (dc + 1)], rhs=uT[:, fc, :],
                        start=(fc == 0), stop=(fc == 31)), reads=["wdn", ("uT", fc)], writes=[ps])
                xo = FM[2 + dc % 2]
                xm = FM[4 + dc % 2]
                P.dma("sp", lambda e: e.dma_start(out=xm[:, 0:T2], in_=x_mid[128 * dc:128 * (dc + 1), c0:c0 + T2]), writes=[xm])
                P.op("dve", lambda e, ps=ps, dc=dc, xo=xo: e.tensor_tensor(
                    out=xo[:, 0:T2], in0=ps[:, 0:T2], in1=xm[:, 0:T2], op=ALU.add),
                    reads=[ps, xm], writes=[xo])
                P.dma("sp", lambda e, dc=dc, c0=c0, xo=xo, x_dst=x_dst: e.dma_start(
                    out=x_dst[128 * dc:128 * (dc + 1), c0:c0 + T2], in_=xo[:, 0:T2]), reads=[xo], writes=["xdst"])
        x_src = x_dst
    P.finalize()
    return nc, P


def make_inputs(inp, core, S, layer_ids):
    L = len(layer_ids)
    vs, rs, sws = [], [], []
    for l in layer_ids:
        v, r, sw = pack_layer_params(inp, l)
        vs.append(v); rs.append(r); sws.append(sw)
    lbl = np.ascontiguousarray(np.asarray(inp["hgrn_lb_logits"], np.float32).T.reshape(2, 128, -1).transpose(1, 0, 2).reshape(128, -1))
    m = {
        "xT": np.ascontiguousarray(np.asarray(inp["x"][core, :S], np.float32).T),
        "w_in": np.ascontiguousarray(inp["w_in"][layer_ids]),
        "w_out": np.ascontiguousarray(inp["w_out"][layer_ids]),
        "w_up": np.ascontiguousarray(inp["w_mlp_up"][layer_ids]),
        "w_dn": np.ascontiguousarray(inp["w_mlp_down"][layer_ids]),
        "vecs": np.stack(vs), "rows": np.stack(rs), "smallw": np.stack(sws),
        "lbl": lbl, "consts": make_consts(),
    }
    return m


FUSED = True
N_CORES = 8
SEQ = 8192
DEPTH = 4


def kernel(**inputs):
    inp = {k: np.asarray(v) for k, v in inputs.items()}
    L = DEPTH
    if FUSED:
        nc, _ = build_program(SEQ, L, L, list(range(L)))
        base = make_inputs(inp, 0, SEQ, list(range(L)))
        in_maps = []
        for c in range(N_CORES):
            m = dict(base)
            m["xT"] = np.ascontiguousarray(np.asarray(inp["x"][c], np.float32).T)
            in_maps.append(m)
        res = run_bass_kernel_spmd(nc, in_maps, core_ids=list(range(N_CORES)))
        outs = [res.results[c]["yT"] for c in range(N_CORES)]
    else:
        xT = [np.ascontiguousarray(np.asarray(inp["x"][c], np.float32).T) for c in range(N_CORES)]
        for l in range(L):
            nc, _ = build_program(SEQ, 1, L, [l])
            base = make_inputs(inp, 0, SEQ, [l])
            in_maps = []
            for c in range(N_CORES):
                m = dict(base)
                m["xT"] = xT[c]
                in_maps.append(m)
            res = run_bass_kernel_spmd(nc, in_maps, core_ids=list(range(N_CORES)))
            xT = [np.ascontiguousarray(res.results[c]["yT"]) for c in range(N_CORES)]
        outs = xT
    out = np.stack([np.asarray(o, np.float32).T for o in outs])
    return np.ascontiguousarray(out)
```

```python
import numpy as np
from contextlib import ExitStack
import concourse.bass as bass
import concourse.mybir as mybir

F32 = mybir.dt.float32
BF16 = mybir.dt.bfloat16
I32 = mybir.dt.int32
ALU = mybir.AluOpType
AF = mybir.ActivationFunctionType
AX = mybir.AxisListType

ENGS = ("pe", "act", "dve", "pool", "sp")
N_DMA_SEMS = 24


import types


def freeze(fn):
    if fn is None or fn.__closure__ is None:
        return fn
    cells = []
    for c in fn.__closure__:
        try:
            cells.append(types.CellType(c.cell_contents))
        except ValueError:
            cells.append(c)
    return types.FunctionType(fn.__code__, fn.__globals__, fn.__name__, fn.__defaults__, tuple(cells))


class Prog:
    def __init__(self, nc):
        self.nc = nc
        self.es = ExitStack()
        self.ops = {e: [] for e in ENGS}
        self.cnt = {e: 0 for e in ENGS}
        self.st = {}
        self.dma_tot = [0] * N_DMA_SEMS
        self.dma_rr = 0
        self.waited = {e: {} for e in ENGS}
        self.dma_events = []
        self.n_t = 0

    def sb(self, shape, dt, name=None):
        self.n_t += 1
        return self.es.enter_context(self.nc.sbuf_tensor(name or f"t{self.n_t}", list(shape), dt))

    def ps(self, shape, dt, name=None):
        self.n_t += 1
        return self.es.enter_context(self.nc.psum_tensor(name or f"p{self.n_t}", list(shape), dt))

    @staticmethod
    def K(k):
        if isinstance(k, (str, int)):
            return k
        if isinstance(k, tuple):
            return tuple(Prog.K(x) for x in k)
        return k.name

    def _deps(self, eng, reads, writes, is_dma):
        reads = [self.K(k) for k in reads]
        writes = [self.K(k) for k in writes]
        deps = []
        for k in reads:
            s = self.st.get(k)
            if s and s[0] is not None:
                deps.append((s[0], "raw"))
        for k in writes:
            s = self.st.get(k)
            if s:
                if s[0] is not None:
                    deps.append((s[0], "waw"))
                for r in s[1]:
                    deps.append((r, "war"))
        waits = []
        for ev, kind in deps:
            if ev[0] == "eng":
                _, e2, n = ev
                if e2 == eng and not is_dma:
                    if kind != "raw" or eng in ("pe", "sp"):
                        continue
                semkey = ("eng", e2)
                val = n
            else:
                _, si, tot = ev
                semkey = ("dma", si)
                val = tot
            if self.waited[eng].get(semkey, 0) >= val:
                continue
            self.waited[eng][semkey] = val
            waits.append((semkey, val))
        return waits

    def _commit(self, ev, reads, writes):
        reads = [self.K(k) for k in reads]
        writes = [self.K(k) for k in writes]
        for k in reads:
            s = self.st.setdefault(k, [None, []])
            s[1].append(ev)
        for k in writes:
            self.st[k] = [ev, []]

    def op(self, eng, fn, reads=(), writes=()):
        waits = self._deps(eng, reads, writes, False)
        self.cnt[eng] += 1
        ev = ("eng", eng, self.cnt[eng])
        self.ops[eng].append(dict(fn=freeze(fn), waits=waits, ev=ev))
        self._commit(ev, reads, writes)
        return ev

    def dma(self, eng, fn, reads=(), writes=()):
        si = self.dma_rr
        self.dma_rr = (self.dma_rr + 1) % N_DMA_SEMS
        waits = self._deps(eng, reads, writes, True)
        semkey = ("dma", si)
        prev = self.dma_tot[si]
        if prev > 0 and self.waited[eng].get(semkey, 0) < prev:
            self.waited[eng][semkey] = prev
            waits.append((semkey, prev))
        self.dma_tot[si] += 16
        ev = ("dma", si, self.dma_tot[si])
        self.ops[eng].append(dict(fn=freeze(fn), waits=waits, ev=ev))
        self._commit(ev, reads, writes)
        self.dma_events.append(ev)
        return ev

    def wait_events(self, eng, events):
        waits = []
        for ev in events:
            if ev[0] == "eng":
                semkey, val = ("eng", ev[1]), ev[2]
            else:
                semkey, val = ("dma", ev[1]), ev[2]
            if self.waited[eng].get(semkey, 0) >= val:
                continue
            self.waited[eng][semkey] = val
            waits.append((semkey, val))
        self.ops[eng].append(dict(fn=None, waits=waits, ev=None))

    def finalize(self):
        nc = self.nc
        fin = [("dma", si, self.dma_tot[si]) for si in range(N_DMA_SEMS) if self.dma_tot[si] > 0]
        self.wait_events("sp", fin)
        needed = {e: set() for e in ENGS}
        for e in ENGS:
            for o in self.ops[e]:
                for (semkey, val) in o["waits"]:
                    if semkey[0] == "eng":
                        needed[semkey[1]].add(val)
        rank = {}
        for e in ENGS:
            for i, v in enumerate(sorted(needed[e])):
                rank[(e, v)] = i + 1
        sems = {}
        for e in ENGS:
            sems[("eng", e)] = self.es.enter_context(nc.semaphore(f"s_{e}"))
        for si in range(N_DMA_SEMS):
            sems[("dma", si)] = self.es.enter_context(nc.semaphore(f"s_dma{si}"))
        handles = dict(pe="tensor", act="scalar", dve="vector", pool="gpsimd", sp="sync")
        self.n_inst = 0

        def emit_engine(e, eng):
            for o in self.ops[e]:
                for (semkey, val) in o["waits"]:
                    if semkey[0] == "eng":
                        val = rank[(semkey[1], val)]
                    eng.wait_ge(sems[semkey], val)
                    self.n_inst += 1
                if o["fn"] is None:
                    continue
                ins = o["fn"](eng)
                self.n_inst += 1
                ev = o["ev"]
                if ev[0] == "dma":
                    ins.then_inc(sems[("dma", ev[1])], 16)
                elif (e, ev[2]) in rank:
                    ins.then_inc(sems[("eng", e)], 1)

        with nc.Block() as block:
            @block.tensor
            def _(eng):
                emit_engine("pe", eng)

            @block.scalar
            def _(eng):
                emit_engine("act", eng)

            @block.vector
            def _(eng):
                emit_engine("dve", eng)

            @block.gpsimd
            def _(eng):
                emit_engine("pool", eng)

            @block.sync
            def _(eng):
                emit_engine("sp", eng)
        self.es.close()


import math
import numpy as np
import concourse.bass as bass
import concourse.mybir as mybir
from concourse.bass_utils import run_bass_kernel_spmd

D = 1024
PIN = 3472
DFF = 4096
TT = 256
NDIAG = TT // 128
RMS_EPS = 1e-6
SLOPES = [2.0 ** (-8.0 * (i + 1) / 4) for i in range(4)]
ND = 66

C_ID = 0
C_BD32 = 128
C_ST64 = 256
C_IN64 = 384
C_LO64 = 512
C_RS32 = 640
C_RS64 = 1152
C_CAUS = 1664
C_TM4 = 2176
C_CM = 2688
C_HM32 = 3712
C_HM64 = 3716
C_HM64B = 3718
C_TM2 = 3846
C_CM2 = 4102
C_B0 = 4104
C_BH = C_B0 + 5
NCONST = C_BH + 3 * ND


def make_consts():
    c = np.zeros((128, NCONST), np.float32)
    p = np.arange(128)
    c[:, C_ID:C_ID + 128] = np.eye(128)
    same32 = (p[:, None] // 32) == (p[None, :] // 32)
    same64 = (p[:, None] // 64) == (p[None, :] // 64)
    c[:, C_BD32:C_BD32 + 128] = same32 & (p[:, None] <= p[None, :])
    c[:, C_ST64:C_ST64 + 128] = same64 & (p[:, None] < p[None, :])
    c[:, C_IN64:C_IN64 + 128] = same64 & (p[:, None] <= p[None, :])
    c[:, C_LO64:C_LO64 + 128] = same64 & (p[:, None] > p[None, :])
    t = np.arange(512)
    c[:, C_RS32:C_RS32 + 512] = (t % 32 != 0)[None, :]
    c[:, C_RS64:C_RS64 + 512] = (t % 64 != 0)[None, :]
    c[:, C_CAUS:C_CAUS + 512] = t[None, :] >= p[:, None]
    tok = np.arange(128)
    for cc in range(4):
        c[:, C_TM4 + 128 * cc:C_TM4 + 128 * (cc + 1)] = (tok // 32 == cc)[None, :]
    col = np.arange(256)
    for h in range(4):
        c[:, C_CM + 256 * h:C_CM + 256 * (h + 1)] = (col // 64 == h)[None, :]
        c[:, C_HM32 + h] = (p // 32 == h)
    for h in range(2):
        c[:, C_HM64 + h] = (p // 64 == h)
    c[:, C_HM64B:C_HM64B + 128] = (p[:, None] // 64) == (tok[None, :] // 64)
    for j in range(2):
        c[:, C_TM2 + 128 * j:C_TM2 + 128 * (j + 1)] = (tok // 64 == j)[None, :]
    for j in range(2):
        c[:, C_CM2 + j] = ((p // 32) % 2 == j)
    for d in range(5):
        c[:, C_B0 + d] = SLOPES[0] * (p - 127 - 128 * d)
    for h in range(1, 4):
        for di in range(ND):
            delta = di - (NDIAG - 1)
            c[:, C_BH + (h - 1) * ND + di] = SLOPES[h] * (p - (TT - 1) - 128 * delta)
    return c


V_GMIX = 0
V_GMLP = 8
V_GQ = 16
V_GK = 17
V_GA = 18
V_GLAB = 19
V_MU = 20
V_W0 = 29
V_A0 = 31
V_KK = 33
V_KA = 35
V_RK = 37
V_LNG = 39
V_LNB = 41
NV = 43
R_LAM = 0
R_GB = 128
R_GC = 384
NR = 640
SW_GU = 0
SW_WU = 128
SW_AU = 384
SW_GUP = 640
NSW = 896


def pack_layer_params(inp, l):
    g = lambda n: np.asarray(inp[n][l], np.float32)
    v = np.zeros((128, NV), np.float32)
    v[:, V_GMIX:V_GMIX + 8] = g("norm_mix_g").reshape(8, 128).T
    v[:, V_GMLP:V_GMLP + 8] = g("norm_mlp_g").reshape(8, 128).T
    v[:, V_GQ] = np.tile(g("da_q_norm_g"), 4)
    v[:, V_GK] = np.tile(g("da_k_norm_g"), 4)
    v[:, V_GA] = np.tile(g("da_out_norm_g"), 2)
    v[:, V_GLAB] = g("gla_gate_b")
    mu = g("rw_shift_mu")
    for i in range(6):
        v[:, V_MU + i] = mu[128 * i:128 * (i + 1)]
    v[:32, V_MU + 6] = mu[768:800]
    v[:32, V_MU + 7] = mu[800:832]
    v[:64, V_MU + 8] = mu[832:896]
    for name, col in (("rw_w0", V_W0), ("rw_a0", V_A0), ("rw_k_k", V_KK), ("rw_k_a", V_KA),
                      ("rw_ln_g", V_LNG), ("rw_ln_b", V_LNB)):
        v[:, col:col + 2] = g(name).reshape(2, 128).T
    v[:, V_RK:V_RK + 2] = g("rw_r_k").reshape(2, 128).T
    r = np.zeros((128, NR), np.float32)
    lam = np.concatenate([g("da_lambda_q1"), g("da_lambda_k1"), g("da_lambda_q2"), g("da_lambda_k2")])
    r[:, R_LAM:R_LAM + 128] = lam[None, :]
    r[:, R_GB:R_GB + 256] = np.tile(g("gla_out_norm_g"), 4)[None, :]
    r[:, R_GC:R_GC + 256] = np.tile(g("hgrn_out_norm_g"), 4)[None, :]
    sw = np.zeros((64, NSW), np.float32)
    sw[:16, SW_GU:SW_GU + 128] = g("gla_gate_up")
    sw[:32, SW_WU:SW_WU + 256] = g("rw_w_up")
    sw[:32, SW_AU:SW_AU + 256] = g("rw_a_up")
    sw[:64, SW_GUP:SW_GUP + 256] = g("rw_g_up")
    return v, r, sw


class Ring:
    def __init__(self, items):
        self.items = items
        self.i = 0

    def get(self):
        x = self.items[self.i]
        self.i = (self.i + 1) % len(self.items)
        return x


class View:
    def __init__(self, ap, name):
        self.ap = ap
        self.name = name

    def __getitem__(self, k):
        return self.ap[k]


def build_program(S, n_layers, L_total, layer_ids, debug=False, mixers="ABCD", do_mlp=True):
    NT = S // TT
    nc = bass.Bass("TRN2", target_bir_lowering=False)
    dt_in = lambda name, shape: nc.dram_tensor(name, list(shape), F32, kind="ExternalInput").ap()
    xT_in = dt_in("xT", [D, S])
    w_in_d = dt_in("w_in", [n_layers, D, PIN])
    w_out_d = dt_in("w_out", [n_layers, D, D])
    w_up_d = dt_in("w_up", [n_layers, D, DFF])
    w_dn_d = dt_in("w_dn", [n_layers, DFF, D])
    vecs_d = dt_in("vecs", [n_layers, 128, NV])
    rows_d = dt_in("rows", [n_layers, 128, NR])
    sw_d = dt_in("smallw", [n_layers, 64, NSW])
    lbl_d = dt_in("lbl", [128, 2 * L_total])
    consts_d = dt_in("consts", [128, NCONST])
    yT = nc.dram_tensor("yT", [D, S], F32, kind="ExternalOutput").ap()
    xa = nc.dram_tensor("xa", [D, S], F32, kind="Internal").ap()
    xb = nc.dram_tensor("xb", [D, S], F32, kind="Internal").ap()
    dbg = nc.dram_tensor("dbg", [D, S], F32, kind="ExternalOutput").ap() if debug else None

    P = Prog(nc)
    cstb = P.sb([128, C_B0], BF16, "cstb")
    for cb in range(0, C_B0, 1024):
        ce = min(C_B0, cb + 1024)
        P.dma("pool", lambda e, cb=cb, ce=ce: e.dma_start(out=cstb[:, cb:ce], in_=consts_d[:, cb:ce]), writes=[cstb])
    biasc = P.sb([128, NCONST - C_B0], F32, "biasc")
    P.dma("sp", lambda e: e.dma_start(out=biasc[:], in_=consts_d[:, C_B0:NCONST]), writes=[biasc])
    ident = cstb[:, C_ID:C_ID + 128]
    caus = cstb[:, C_CAUS:C_CAUS + 512]
    ones_bf = P.sb([128, 128], BF16, "ones_bf")
    P.op("pool", lambda e: e.memset(ones_bf[:], 1.0), writes=[ones_bf])
    bo32 = P.sb([128, 128], BF16, "bo32")
    bo64 = P.sb([128, 128], BF16, "bo64")
    P.op("pool", lambda e: e.memset(bo32[:], 0.0), writes=[bo32])
    P.op("pool", lambda e: e.memset(bo64[:], 0.0), writes=[bo64])
    for b in range(4):
        P.op("pool", lambda e, b=b: e.memset(bo32[32 * b:32 * b + 32, 32 * b:32 * b + 32], 1.0), writes=[bo32])
    for b in range(2):
        P.op("pool", lambda e, b=b: e.memset(bo64[64 * b:64 * b + 64, 64 * b:64 * b + 64], 1.0), writes=[bo64])
    Esel = P.sb([128, 64], F32, "Esel")
    P.op("pool", lambda e: e.memset(Esel[:], 0.0), writes=[Esel])
    P.op("pool", lambda e: e.memset(Esel[64:65, :], 1.0), writes=[Esel])

    NKT = S // 128
    R1_EL = 65536
    R1 = P.sb([128, R1_EL], BF16, "R1")
    winA = R1[:, 0:6144].rearrange("p (k n) -> p k n", k=8)
    woA = R1[:, 6144:10240].rearrange("p (k n) -> p k n", k=4)
    o = 10240
    KT = R1[:, o:o + 2 * S].rearrange("p (c s) -> p c s", c=2); o += 2 * S
    Vc = R1[:, o:o + NKT * 260].rearrange("p (t h v) -> p t h v", h=4, v=65); o += NKT * 260
    assert o <= R1_EL
    winB = R1[:, 0:8 * PIN].rearrange("p (k n) -> p k n", k=8)
    woB = R1[:, 8 * PIN:8 * PIN + 6 * D].rearrange("p (k n) -> p k n", k=6)
    carve_o = [8 * PIN + 6 * D]

    def carve(shape, dt, name):
        n = int(np.prod(shape[1:]))
        nb = n if dt == BF16 else 2 * n
        a = carve_o[0]
        carve_o[0] += nb + (nb % 2)
        assert carve_o[0] <= R1_EL, (name, carve_o[0])
        ap = R1[:, a:a + nb]
        if dt != BF16:
            ap = ap.bitcast(dt)
        if len(shape) == 3:
            ap = ap.rearrange("p (a b) -> p a b", a=shape[1])
        return View(ap[0:shape[0]], name)
    wup = R1[:, 0:32768].rearrange("p (k n) -> p k n", k=8)
    wdn = R1[:, 32768:65536].rearrange("p (k n) -> p k n", k=32)

    NF = 16
    R2 = P.sb([128, 6144], F32, "R2")
    F = [View(R2[:, TT * i:TT * (i + 1)], f"F{i}") for i in range(NF)]
    R2b = R2[:, 4096:6144].bitcast(BF16)
    pT = Ring([View(R2b[:, 512 * i:512 * (i + 1)], f"pT{i}") for i in range(3)]
              + [P.sb([128, 512], BF16, f"pTx{i}") for i in range(2)])
    mixA = R2b[:, 1536:2560].rearrange("p (c n) -> p c n", c=4)
    mixB = R2b[:, 2560:4096].rearrange("p (c n) -> p c n", c=6)
    uT = R2[:, 0:4096].bitcast(BF16).rearrange("p (k n) -> p k n", k=32)
    FM = [View(R2[:, 4096 + TT * i:4096 + TT * (i + 1)], f"FM{i}") for i in range(8)]

    banks = [P.ps([128, 512], F32, f"bank{i}") for i in range(8)]
    pring = Ring(banks[0:4])
    acc = banks[4:8]

    xs = P.sb([128, 8, TT], F32, "xs")
    qbd = P.sb([128, 2, 2, TT], BF16, "qbd")
    hT_l = [P.sb([128, 8, TT], BF16, f"hT{i}") for i in range(2)]
    Bb = [P.sb([128, TT], BF16, f"B{i}") for i in range(12)]
    bring = Ring(Bb[8:12])
    vecs = P.sb([128, NV], F32, "vecs_s")
    rows = P.sb([128, NR], F32, "rows_s")
    neglam = P.sb([128, 1], F32, "neglam")
    ltmp = P.sb([128, 64], F32, "ltmp")
    lsum = P.sb([128, 2], F32, "lsum")


    Dsb = carve([128, 512], F32, "Dsb")
    Grep = carve([128, 512], F32, "Grep")
    Hs = carve([128, 512], F32, "Hs")
    HbB = carve([128, 8, 256], BF16, "HbB")
    HbC = [carve([128, 8, 128], BF16, f"HbC{i}") for i in range(2)]
    Vm = carve([128, 4, 256], BF16, "Vm")
    Vbd = carve([128, 4, 256], BF16, "Vbd")
    Qbd = [carve([128, 512], BF16, f"Qbd{i}") for i in range(2)]
    Qm = [carve([128, 4, 128], BF16, f"Qm{i}") for i in range(2)]
    khat = carve([128, 2, 256], BF16, "khat")
    Vtm = carve([128, 2, 256], BF16, "Vtm")
    sog = carve([128, 2, 256], F32, "sog")
    mtm = carve([128, 256], BF16, "mtm")
    Smk = carve([128, 4, 128], BF16, "Smk")
    pf = carve([128, TT + 2], F32, "pf")
    PRb = carve([128, 2, 256], BF16, "PRb")
    Ktb = carve([128, TT], BF16, "Ktb")
    Qtb = carve([128, TT], BF16, "Qtb")
    Khb = carve([128, TT], BF16, "Khb")
    Qhb = carve([128, TT], BF16, "Qhb")
    vpb = carve([128, TT], BF16, "vpb")
    Vdtm = carve([128, 128], BF16, "Vdtm")
    Ptm = carve([128, 128], BF16, "Ptm")
    Qhtm = carve([128, 128], BF16, "Qhtm")
    Khtm = carve([128, 128], BF16, "Khtm")
    AMb = [carve([128, 2, 256], BF16, f"AM{i}") for i in range(2)]
    Nb = [[carve([128, 128], BF16, f"Nb{h}_{i}") for i in range(3)] for h in range(2)]
    Mb = [[carve([128, 128], BF16, f"Mb{h}_{i}") for i in range(2)] for h in range(2)]
    Wb = [carve([128, 2, 128], BF16, f"Wb{i}") for i in range(2)]
    PRm = carve([128, 2, 256], BF16, "PRm")
    Rm = carve([128, 2, 128], BF16, "Rm")
    PH = carve([128, 128], BF16, "PH")
    PHT = carve([128, 128], BF16, "PHT")
    Umb = carve([128, 2, 128], BF16, "Umb")
    Ucomb = carve([128, 128], BF16, "Ucomb")
    Uhm = carve([128, 2, 128], BF16, "Uhm")
    Vmj = carve([128, 2, 128], BF16, "Vmj")
    Hexp = [carve([128, 128], F32, f"Hexp{i}") for i in range(2)]
    Hbf = [carve([128, 128], BF16, f"Hbf{i}") for i in range(3)]
    ysb = carve([128, 128], F32, "ysb")
    ynb = carve([128, 128], BF16, "ynb")
    prevc = P.sb([128, 9], F32, "prevc")
    lnst = P.sb([128, 16], F32, "lnst")
    ltot4 = P.sb([128, 4], F32, "ltot4")
    gam4 = [P.sb([128, 4], F32, f"gam4_{i}") for i in range(2)]
    carry = [P.sb([128, 64], F32, f"carry{i}") for i in range(3)]
    gam = [P.sb([128, 8], F32, f"gam{i}") for i in range(2)]
    ltot = P.sb([128, 8], F32, "ltot")
    st4 = P.sb([128, 16], F32, "st4")
    swb = P.sb([64, NSW], BF16, "swb")
    negb = P.sb([128, 1], F32, "negb")
    lbl = P.sb([128, 2 * L_total], F32, "lbl_s")
    lbT = P.sb([128, 2 * L_total], F32, "lbT")
    omlT = P.sb([128, 2 * L_total], F32, "omlT")
    lb_t = P.sb([128, 4], F32, "lb_t")
    ones4 = P.sb([128, L_total], F32, "ones4")
    P.dma("sp", lambda e: e.dma_start(out=lbl[:], in_=lbl_d[:, :]), writes=[lbl])
    P.op("pool", lambda e: e.memset(ones4[:], 1.0), writes=[ones4])
    Lt = L_total
    for rc in range(2):
        sl = slice(rc * Lt, (rc + 1) * Lt)
        P.op("dve", lambda e, sl=sl: e.tensor_reduce(out=lb_t[:, 0:1], in_=lbl[:, sl], axis=AX.X, op=ALU.max, negate=True),
             reads=[lbl], writes=[lb_t])
        P.op("act", lambda e, sl=sl: e.activation(out=lbT[:, sl], in_=lbl[:, sl], func=AF.Exp, bias=lb_t[:, 0:1], scale=1.0),
             reads=[lbl, lb_t], writes=[lbT])
        P.op("dve", lambda e, sl=sl: e.tensor_reduce(out=lb_t[:, 1:2], in_=lbT[:, sl], axis=AX.X, op=ALU.add),
             reads=[lbT], writes=[lb_t])
        P.op("dve", lambda e: e.reciprocal(out=lb_t[:, 2:3], in_=lb_t[:, 1:2]), reads=[lb_t], writes=[lb_t])
        P.op("dve", lambda e, sl=sl: e.tensor_scalar(out=lbT[:, sl], in0=lbT[:, sl], scalar1=lb_t[:, 2:3], scalar2=None,
                                                      op0=ALU.mult), reads=[lbT, lb_t], writes=[lbT])
        P.op("dve", lambda e, sl=sl: e.tensor_copy(out=lb_t[:, 3:4], in_=lbT[:, rc * Lt:rc * Lt + 1]), reads=[lbT], writes=[lb_t])
        P.op("dve", lambda e, sl=sl: e.tensor_tensor_scan(out=omlT[:, sl], data0=ones4[:], data1=lbT[:, sl], initial=0.0,
                                                           op0=ALU.mult, op1=ALU.add), reads=[lbT, ones4], writes=[omlT])
        P.op("dve", lambda e, sl=sl: e.tensor_scalar(out=lbT[:, sl], in0=omlT[:, sl], scalar1=lb_t[:, 3:4], scalar2=None,
                                                      op0=ALU.subtract), reads=[omlT, lb_t], writes=[lbT])
        P.op("dve", lambda e, sl=sl: e.tensor_scalar(out=omlT[:, sl], in0=lbT[:, sl], scalar1=-1.0, scalar2=1.0,
                                                      op0=ALU.mult, op1=ALU.add), reads=[lbT], writes=[omlT])

    def vcol(c, n=128):
        return vecs[0:n, c:c + 1]

    def barrier():
        evs = []
        for e in ENGS:
            if P.cnt[e] > 0:
                evs.append(("eng", e, P.cnt[e]))
        for si in range(N_DMA_SEMS):
            if P.dma_tot[si] > 0:
                evs.append(("dma", si, P.dma_tot[si]))
        for e in ENGS:
            P.wait_events(e, [ev for ev in evs if not (ev[0] == "eng" and ev[1] == e)])
        P.st.clear()

    def rmsnorm_tile(x_src, c0, n, gcol0, Ft, bi):
        hT = hT_l[bi]
        for k in range(8):
            P.dma("sp", lambda e, k=k: e.dma_start(
                out=xs[:, k, 0:n], in_=x_src[128 * k:128 * (k + 1), c0:c0 + n]), writes=[("xs", k)])
        ssp = pring.get()
        for k in range(8):
            sq = bring.get()
            P.op("act", lambda e, k=k, sq=sq: e.activation(out=sq[:, 0:n], in_=xs[:, k, 0:n], func=AF.Square),
                 reads=[("xs", k)], writes=[sq])
            P.op("pe", lambda e, k=k, sq=sq: e.matmul(ssp[:, 0:n], lhsT=ones_bf[:], rhs=sq[:, 0:n],
                                                      start=(k == 0), stop=(k == 7)),
                 reads=[ones_bf, sq], writes=[ssp])
        rstd = Ft
        P.op("act", lambda e: e.activation(out=rstd[:, 0:n], in_=ssp[:, 0:n], func=AF.Sqrt,
                                           scale=1.0 / D, bias=RMS_EPS), reads=[ssp], writes=[rstd])
        P.op("dve", lambda e: e.reciprocal(out=rstd[:, 0:n], in_=rstd[:, 0:n]), reads=[rstd], writes=[rstd])
        for k in range(8):
            P.op("dve", lambda e, k=k: e.scalar_tensor_tensor(
                out=hT[:, k, 0:n], in0=xs[:, k, 0:n], scalar=vcol(gcol0 + k), in1=rstd[:, 0:n],
                op0=ALU.mult, op1=ALU.mult), reads=[("xs", k), vecs, rstd], writes=[("hT", bi, k)])

    x_src = xT_in
    for li in range(n_layers):
        lid = layer_ids[li]
        lam_init = 0.8 - 0.6 * math.exp(-0.3 * lid)
        x_mid = xa
        x_dst = yT if li == n_layers - 1 else xb
        barrier()
        P.dma("sp", lambda e, li=li: e.dma_start(out=vecs[:], in_=vecs_d[li, :, :]), writes=[vecs])
        P.dma("sp", lambda e, li=li: e.dma_start(out=rows[:], in_=rows_d[li, :, :]), writes=[rows])
        P.dma("pool", lambda e, li=li: e.dma_start(out=swb[:], in_=sw_d[li, :, :]), writes=[swb])
        P.op("dve", lambda e: e.tensor_scalar(out=negb[:], in0=vcol(V_GLAB), scalar1=-1.0, scalar2=None, op0=ALU.mult),
             reads=[vecs], writes=[negb])
        lam4 = rows[:, R_LAM:R_LAM + 128].rearrange("p (a t b) -> p a t b", a=2, t=2)
        P.op("dve", lambda e: e.tensor_tensor(
            out=ltmp[:].rearrange("p (a b) -> p a b", a=2), in0=lam4[:, :, 0, :], in1=lam4[:, :, 1, :],
            op=ALU.mult), reads=[rows], writes=[ltmp])
        P.op("dve", lambda e: e.tensor_reduce(out=lsum[:], in_=ltmp[:].rearrange("p (a b) -> p a b", a=2),
                                               axis=AX.X, op=ALU.add), reads=[ltmp], writes=[lsum])
        P.op("act", lambda e: e.activation(out=lsum[:], in_=lsum[:], func=AF.Exp), reads=[lsum], writes=[lsum])
        P.op("dve", lambda e, lam_init=lam_init: e.scalar_tensor_tensor(
            out=neglam[:], in0=lsum[:, 1:2], scalar=-lam_init, in1=lsum[:, 0:1], op0=ALU.add, op1=ALU.subtract),
            reads=[lsum], writes=[neglam])

        for phase in ("a", "b"):
            barrier()
            if phase == "a":
                win = winA
                P.dma("pool", lambda e, li=li: e.dma_start(
                    out=winA[:, :, :], in_=w_in_d[li, :, 0:768].rearrange("(k p) n -> p k n", p=128)), writes=[("win", 0), ("win", 1)])
                P.dma("pool", lambda e, li=li: e.dma_start(
                    out=woA[0:64, :, :], in_=w_out_d[li, 0:256, :].rearrange("(h v) n -> v h n", v=64)), writes=["wo"])
                P.op("pool", lambda e: e.memset(Vc[:, :, :, 64:65], 1.0), writes=["Vones"])
            else:
                win = winB
                for cb in range(0, PIN, 512):
                    ce = min(PIN, cb + 512)
                    P.dma("pool", lambda e, li=li, cb=cb, ce=ce: e.dma_start(
                        out=winB[:, :, cb:ce], in_=w_in_d[li, :, cb:ce].rearrange("(k p) n -> p k n", p=128)),
                        writes=[("win", cb // 512)])
                for c2 in range(6):
                    P.dma("pool", lambda e, li=li, c2=c2: e.dma_start(
                        out=woB[:, c2, :], in_=w_out_d[li, 256 + 128 * c2:256 + 128 * (c2 + 1), :]), writes=["wo"])
                for cr in carry:
                    P.op("pool", lambda e, cr=cr: e.memset(cr[:], 0.0), writes=[cr])
                P.op("pool", lambda e: e.memset(prevc[:], 0.0), writes=[prevc])
                for hx in Hexp:
                    P.op("pool", lambda e, hx=hx: e.memset(hx[:], 0.0), writes=[hx])
                P.op("pool", lambda e: e.memset(HbB[:], 0.0), writes=[HbB])
                for hb_ in HbC:
                    P.op("pool", lambda e, hb_=hb_: e.memset(hb_[:], 0.0), writes=[hb_])
            for t in range(NT):
                c0 = t * TT
                bi = t % 2
                if t == 0:
                    rmsnorm_tile(x_src, c0, TT, V_GMIX, F[15], 0)
                hT = hT_l[bi]
                hT_keys = [("hT", bi, k) for k in range(8)]
                pf_done = [False]

                def prefetch():
                    if not pf_done[0] and t + 1 < NT:
                        rmsnorm_tile(x_src, (t + 1) * TT, TT, V_GMIX, F[15], (t + 1) % 2)
                    pf_done[0] = True

                def fm_proj(col0, nrows):
                    ps = pring.get()
                    for k in range(8):
                        P.op("pe", lambda e, k=k, ps=ps: e.matmul(
                            ps[0:nrows, 0:TT], lhsT=win[:, k, col0:col0 + nrows], rhs=hT[:, k, :],
                            start=(k == 0), stop=(k == 7)),
                            reads=[("win", b) for b in range(col0 // 512, (col0 + nrows - 1) // 512 + 1)] + hT_keys, writes=[ps])
                    return ps

                def tm_proj(col0, ncols, sub):
                    ps = pring.get()
                    for k in range(8):
                        P.op("pe", lambda e, k=k, ps=ps: e.matmul(
                            ps[:, 0:ncols], lhsT=hT[:, k, 128 * sub:128 * (sub + 1)], rhs=win[:, k, col0:col0 + ncols],
                            start=(k == 0), stop=(k == 7)),
                            reads=[("win", b) for b in range(col0 // 512, (col0 + ncols - 1) // 512 + 1)] + hT_keys, writes=[ps])
                    return ps

                if phase == "a" and "A" in mixers:
                    for which in range(4):
                        isq = which < 2
                        ch = which % 2
                        ps = fm_proj((0 if isq else 256) + 128 * ch, 128)
                        qf, sq = F[0], Bb[0]
                        P.op("act", lambda e, ps=ps: e.activation(out=qf[:], in_=ps[:, 0:TT], func=AF.Copy),
                             reads=[ps], writes=[qf])
                        P.op("act", lambda e, ps=ps: e.activation(out=sq[:], in_=ps[:, 0:TT], func=AF.Square),
                             reads=[ps], writes=[sq])
                        gs = pring.get()
                        P.op("pe", lambda e, gs=gs: e.matmul(gs[:, 0:TT], lhsT=bo32[:], rhs=sq[:], start=True, stop=True),
                             reads=[bo32, sq], writes=[gs])
                        sd = F[1]
                        if isq:
                            P.op("act", lambda e, gs=gs: e.activation(out=sd[:], in_=gs[:, 0:TT], func=AF.Sqrt,
                                                                      scale=1.0, bias=32.0 * RMS_EPS),
                                 reads=[gs], writes=[sd])
                        else:
                            P.op("act", lambda e, gs=gs: e.activation(out=sd[:], in_=gs[:, 0:TT], func=AF.Sqrt,
                                                                      scale=1.0 / 32.0, bias=RMS_EPS),
                                 reads=[gs], writes=[sd])
                        P.op("dve", lambda e: e.reciprocal(out=sd[:], in_=sd[:]), reads=[sd], writes=[sd])
                        if isq:
                            qtmp = Bb[2]
                            P.op("dve", lambda e, ch=ch: e.scalar_tensor_tensor(
                                out=qtmp[:], in0=qf[:], scalar=vcol(V_GQ), in1=sd[:], op0=ALU.mult, op1=ALU.mult),
                                reads=[qf, sd, vecs], writes=[qtmp])
                            P.op("pool", lambda e, ch=ch: e.tensor_tensor(
                                out=qbd[:, ch, :, :], in0=qtmp[:].unsqueeze(1).broadcast_to([128, 2, TT]),
                                in1=cstb[:, C_CM2:C_CM2 + 2].unsqueeze(2).broadcast_to([128, 2, TT]), op=ALU.mult),
                                reads=[qtmp, cstb], writes=[("qbd", ch)])
                        else:
                            P.op("dve", lambda e, ch=ch, c0=c0: e.scalar_tensor_tensor(
                                out=KT[:, ch, c0:c0 + TT], in0=qf[:], scalar=vcol(V_GK), in1=sd[:],
                                op0=ALU.mult, op1=ALU.mult),
                                reads=[qf, sd, vecs], writes=[("KT", ch, t)])
                    for sub in range(NDIAG):
                        ps = tm_proj(512, 256, sub)
                        P.op("act", lambda e, ps=ps, sub=sub, t=t: e.activation(
                            out=Vc[:, NDIAG * t + sub, :, 0:64], in_=ps[:, 0:256].rearrange("p (h v) -> p h v", h=4),
                            func=AF.Copy), reads=[ps, "Vones"], writes=[("Vc", NDIAG * t + sub)])

                    prefetch()

                    def attn_block(h, qlo, qn_cols, ktiles, bias_col_fn, o_lo):
                        ch = h // 2
                        off2 = 64 * (h % 2)
                        LA = 3
                        pend = []
                        nk = len(ktiles)
                        for idx in range(nk + LA):
                            if idx < nk:
                                kt, m = ktiles[idx]
                                cs = 0 if m is None else 128 * m
                                n = qn_cols - cs
                                sp_ = pring.get()
                                P.op("pe", lambda e: e.matmul(
                                    sp_[:, 0:2 * n], lhsT=KT[off2:off2 + 64, ch, 128 * kt:128 * kt + 128],
                                    rhs=qbd[off2:off2 + 64, ch, :, qlo + cs:qlo + cs + n], start=True, stop=True,
                                    tile_position=(off2, 0)),
                                    reads=[("KT", ch, kt // NDIAG), ("qbd", ch)], writes=[sp_])
                                pt = pT.get()
                                bc = bias_col_fn(kt)
                                P.op("act", lambda e: e.activation(
                                    out=pt[:, 0:2 * n], in_=sp_[:, 0:2 * n], func=AF.Exp, bias=biasc[:, bc:bc + 1], scale=1.0),
                                    reads=[sp_, biasc], writes=[pt])
                                if m is not None:
                                    P.op("pool", lambda e: e.tensor_tensor(
                                        out=pt[:, 0:2 * n].rearrange("p (c n) -> p c n", c=2),
                                        in0=pt[:, 0:2 * n].rearrange("p (c n) -> p c n", c=2),
                                        in1=caus[:, 0:n].unsqueeze(1).broadcast_to([128, 2, n]), op=ALU.mult),
                                        reads=[pt, cstb], writes=[pt])
                                pend.append((pt, kt, cs, n))
                            if idx >= LA:
                                pt, kt, cs, n = pend[idx - LA]
                                first = (idx == LA)
                                for c in range(2):
                                    P.op("pe", lambda e: e.matmul(
                                        acc[c][0:65, o_lo + cs:o_lo + cs + n], lhsT=Vc[:, kt, h, :], rhs=pt[:, c * n:(c + 1) * n],
                                        start=first, stop=False), reads=[pt, ("Vc", kt)], writes=[acc[c]])

                    for h in range(4):
                        onrm = [F[4], F[5]]
                        if h == 0:
                            for qs in range(NDIAG):
                                gq = NDIAG * t + qs
                                kts = [(kt, None) for kt in range(max(0, gq - 4), gq)] + [(gq, 0)]
                                attn_block(h, 128 * qs, 128, kts, lambda kt, gq=gq: (gq - kt), 128 * qs)
                        else:
                            lo_kt = max(0, NDIAG * t - 16) if h == 1 else 0
                            kts = [(kt, None) for kt in range(lo_kt, NDIAG * t)] + [(NDIAG * t + m, m) for m in range(NDIAG)]
                            attn_block(h, 0, TT, kts,
                                       lambda kt, h=h, t=t: 5 + (h - 1) * ND + (NDIAG * t - kt) + (NDIAG - 1), 0)
                        for c in range(2):
                            o_ps = acc[c]
                            oa = F[2 + c]
                            P.op("act", lambda e, o_ps=o_ps, oa=oa: e.activation(out=oa[0:65, :], in_=o_ps[0:65, 0:TT],
                                                                               func=AF.Copy),
                                 reads=[o_ps], writes=[oa])
                            dps = pring.get()
                            P.op("pe", lambda e, dps=dps, oa=oa: e.matmul(dps[0:64, 0:TT], lhsT=Esel[0:65, :],
                                                                          rhs=oa[0:65, :], start=True, stop=True),
                                 reads=[Esel, oa], writes=[dps])
                            rd = F[6]
                            P.op("dve", lambda e, dps=dps: e.reciprocal(out=rd[0:64, :], in_=dps[0:64, 0:TT]),
                                 reads=[dps], writes=[rd])
                            P.op("dve", lambda e, oa=oa, c=c: e.tensor_tensor(
                                out=onrm[c][0:64, :], in0=oa[0:64, :], in1=rd[0:64, :], op=ALU.mult),
                                reads=[oa, rd], writes=[onrm[c]])
                        df = F[7]
                        P.op("dve", lambda e: e.scalar_tensor_tensor(
                            out=df[0:64, :], in0=onrm[1][0:64, :], scalar=neglam[0:64, :], in1=onrm[0][0:64, :],
                            op0=ALU.mult, op1=ALU.add), reads=[onrm[0], onrm[1], neglam], writes=[df])
                        sq = Bb[1]
                        P.op("act", lambda e: e.activation(out=sq[0:64, :], in_=df[0:64, :], func=AF.Square),
                             reads=[df], writes=[sq])
                        mps = pring.get()
                        P.op("pe", lambda e, mps=mps: e.matmul(mps[0:64, 0:TT], lhsT=ones_bf[0:64, 0:64], rhs=sq[0:64, :],
                                                               start=True, stop=True), reads=[ones_bf, sq], writes=[mps])
                        sd = F[8]
                        s1 = 1.0 - lam_init
                        P.op("act", lambda e, mps=mps, s1=s1: e.activation(
                            out=sd[0:64, :], in_=mps[0:64, 0:TT], func=AF.Sqrt, scale=1.0 / (64.0 * s1 * s1),
                            bias=RMS_EPS / (s1 * s1)), reads=[mps], writes=[sd])
                        P.op("dve", lambda e: e.reciprocal(out=sd[0:64, :], in_=sd[0:64, :]), reads=[sd], writes=[sd])
                        P.op("dve", lambda e, h=h: e.scalar_tensor_tensor(
                            out=mixA[0:64, h, :], in0=df[0:64, :], scalar=vcol(V_GA, 64), in1=sd[0:64, :],
                            op0=ALU.mult, op1=ALU.mult), reads=[df, sd, vecs], writes=[("mixA", h)])
                elif phase == "a":
                    for h in range(4):
                        P.op("pool", lambda e, h=h: e.memset(mixA[0:64, h, :], 0.0), writes=[("mixA", h)])

                def transpose_to(out_ap, in_ap, out_key, in_key, eng="act"):
                    tp = pring.get()
                    tpb = tp[:].bitcast(BF16)
                    P.op("pe", lambda e: e.transpose(tpb[:, 0:128], in_ap, ident), reads=[in_key, cstb], writes=[tp])
                    if eng == "act":
                        P.op("act", lambda e: e.activation(out=out_ap, in_=tpb[:, 0:128], func=AF.Copy),
                             reads=[tp], writes=[out_key])
                    else:
                        P.op("dve", lambda e: e.tensor_copy(out=out_ap, in_=tpb[:, 0:128]), reads=[tp], writes=[out_key])

                def lin_attn(kind):
                    if kind == "B":
                        RC, Kh, qcol, kcol, cbase, sc = 1, 32, 768, 896, 0, -1.0 / 16.0
                        grow = rows[:, R_GB:R_GB + 256]
                    else:
                        RC, Kh, qcol, kcol, cbase, sc = 2, 64, 1552, 1808, 2, 1.0
                        grow = rows[:, R_GC:R_GC + 256]
                    hpc = 128 // Kh
                    qt_l, kt_l = [], []
                    for sub in range(NDIAG):
                        if kind == "B":
                            psv = tm_proj(1024, 256, sub)
                            P.op("act", lambda e, psv=psv, sub=sub: e.activation(out=Vtm[:, sub, :], in_=psv[:, 0:256], func=AF.Copy),
                                 reads=[psv], writes=[("Vtm", sub)])
                            pso = tm_proj(1296, 256, sub)
                            P.op("act", lambda e, pso=pso, sub=sub: e.activation(out=sog[:, sub, :], in_=pso[:, 0:256], func=AF.Silu),
                                 reads=[pso], writes=[("sog", sub)])
                        else:
                            psv = tm_proj(2064, 512, sub)
                            P.op("act", lambda e, psv=psv, sub=sub: e.activation(out=Vtm[:, sub, :], in_=psv[:, 0:256], func=AF.Copy),
                                 reads=[psv], writes=[("Vtm", sub)])
                            P.op("act", lambda e, psv=psv, sub=sub: e.activation(out=sog[:, sub, :], in_=psv[:, 256:512], func=AF.Silu),
                                 reads=[psv], writes=[("sog", sub)])
                    for rc in range(RC):
                        qt, ktl, kh_ = Bb[2 + rc], Bb[4 + rc], Bb[6]
                        qt_l.append(qt); kt_l.append(ktl)
                        lf, Lc, eq, ek, dd = F[0], F[1], F[2], F[3], F[4]
                        if kind == "B":
                            psg = fm_proj(1280, 16)
                            gdb = Bb[7]
                            P.op("act", lambda e, psg=psg: e.activation(out=gdb[0:16, :], in_=psg[0:16, 0:TT], func=AF.Copy),
                                 reads=[psg], writes=[gdb])
                            pre = pring.get()
                            P.op("pe", lambda e, pre=pre: e.matmul(pre[:, 0:TT], lhsT=swb[0:16, SW_GU:SW_GU + 128], rhs=gdb[0:16, :],
                                                                   start=True, stop=True), reads=[swb, gdb], writes=[pre])
                            e1 = F[5]
                            P.op("act", lambda e, pre=pre: e.activation(out=e1[:], in_=pre[:, 0:TT], func=AF.Exp, scale=-1.0,
                                                                        bias=negb[:, 0:1]), reads=[pre, negb], writes=[e1])
                            P.op("act", lambda e: e.activation(out=lf[:], in_=e1[:], func=AF.Ln, bias=1.0, scale=1.0),
                                 reads=[e1], writes=[lf])
                            psq = fm_proj(qcol, 128)
                            psk = fm_proj(kcol, 128)
                            kv = None
                        else:
                            psf = fm_proj(kcol + 128 * rc, 128)
                            sg, fg, kvf = F[5], F[6], F[7]
                            ez = F[11]
                            P.op("act", lambda e, psf=psf: e.activation(out=ez[:], in_=psf[:, 0:TT], func=AF.Exp, scale=-1.0),
                                 reads=[psf], writes=[ez])
                            P.op("dve", lambda e: e.tensor_scalar(out=sg[:], in0=ez[:], scalar1=1.0, scalar2=None, op0=ALU.add),
                                 reads=[ez], writes=[sg])
                            P.op("dve", lambda e: e.reciprocal(out=sg[:], in_=sg[:]), reads=[sg], writes=[sg])
                            lcol = rc * L_total + lid
                            P.op("dve", lambda e, lcol=lcol: e.tensor_scalar(
                                out=fg[:], in0=sg[:], scalar1=omlT[:, lcol:lcol + 1], scalar2=lbT[:, lcol:lcol + 1],
                                op0=ALU.mult, op1=ALU.add), reads=[sg, omlT, lbT], writes=[fg])
                            P.op("dve", lambda e: e.tensor_scalar(out=fg[:], in0=fg[:], scalar1=1e-30, scalar2=None, op0=ALU.max),
                                 reads=[fg], writes=[fg])
                            P.op("act", lambda e: e.activation(out=lf[:], in_=fg[:], func=AF.Ln), reads=[fg], writes=[lf])
                            P.op("dve", lambda e, lcol=lcol: e.scalar_tensor_tensor(
                                out=kvf[:], in0=ez[:], scalar=omlT[:, lcol:lcol + 1], in1=sg[:], op0=ALU.mult, op1=ALU.mult),
                                reads=[ez, sg, omlT], writes=[kvf])
                            psq = fm_proj(qcol + 128 * rc, 128)
                            qsl = F[8]
                            P.op("act", lambda e, psq=psq: e.activation(out=qsl[:], in_=psq[:, 0:TT], func=AF.Silu),
                                 reads=[psq], writes=[qsl])
                            kv = kvf
                        P.op("dve", lambda e: e.tensor_tensor_scan(out=Lc[:], data0=cstb[:, C_RS32:C_RS32 + TT], data1=lf[:],
                                                                   initial=0.0, op0=ALU.mult, op1=ALU.add),
                             reads=[cstb, lf], writes=[Lc])
                        Lc3 = Lc[:].rearrange("p (c j) -> p c j", j=32)
                        if debug and kind == "B" and t == 0:
                            P.op("dve", lambda e: e.tensor_copy(out=F[14][0:64, :], in_=swb[:, 0:256]), reads=[swb], writes=[F[14]])
                            P.dma("sp", lambda e: e.dma_start(out=dbg[768:832, 0:256], in_=F[14][0:64, :]), reads=[F[14]])
                            P.op("dve", lambda e: e.tensor_copy(out=F[13][0:32, :], in_=gdb[0:32, :]), reads=[gdb], writes=[F[13]])
                            P.dma("sp", lambda e: e.dma_start(out=dbg[832:864, 0:256], in_=F[13][0:32, :]), reads=[F[13]])
                        if debug and kind == "B" and False:
                            P.dma("sp", lambda e, c0=c0: e.dma_start(out=dbg[768:896, c0:c0 + TT], in_=Lc[:]), reads=[Lc])
                            P.dma("sp", lambda e, c0=c0: e.dma_start(out=dbg[896:1024, c0:c0 + TT], in_=lf[:]), reads=[lf])
                        P.op("dve", lambda e: e.tensor_copy(out=ltot[:], in_=Lc3[:, :, 31]), reads=[Lc], writes=[ltot])
                        P.op("act", lambda e: e.activation(out=eq[:], in_=Lc[:], func=AF.Exp, scale=sc), reads=[Lc], writes=[eq])
                        P.op("act", lambda e: e.activation(out=ek[:], in_=Lc[:], func=AF.Exp, scale=-sc), reads=[Lc], writes=[ek])
                        P.op("dve", lambda e: e.tensor_tensor(
                            out=dd[:].rearrange("p (c j) -> p c j", j=32), in0=ltot[:].unsqueeze(2).broadcast_to([128, 8, 32]),
                            in1=Lc3, op=ALU.subtract), reads=[ltot, Lc], writes=[dd])
                        P.op("act", lambda e: e.activation(out=dd[:], in_=dd[:], func=AF.Exp, scale=sc), reads=[dd], writes=[dd])
                        g_ = gam[rc]
                        P.op("act", lambda e, g_=g_: e.activation(out=g_[:], in_=ltot[:], func=AF.Exp, scale=sc),
                             reads=[ltot], writes=[g_])
                        if kind == "B":
                            P.op("dve", lambda e, psq=psq, qt=qt: e.scalar_tensor_tensor(
                                out=qt[:], in0=psq[:, 0:TT], scalar=32.0 ** -0.5, in1=eq[:], op0=ALU.mult, op1=ALU.mult),
                                reads=[psq, eq], writes=[qt])
                            P.op("dve", lambda e, psk=psk, ktl=ktl: e.tensor_tensor(out=ktl[:], in0=psk[:, 0:TT], in1=ek[:], op=ALU.mult),
                                 reads=[psk, ek], writes=[ktl])
                            P.op("dve", lambda e, psk=psk: e.tensor_tensor(out=kh_[:], in0=psk[:, 0:TT], in1=dd[:], op=ALU.mult),
                                 reads=[psk, dd], writes=[kh_])
                        else:
                            P.op("dve", lambda e, qt=qt: e.tensor_tensor(out=qt[:], in0=qsl[:], in1=eq[:], op=ALU.mult),
                                 reads=[qsl, eq], writes=[qt])
                            P.op("dve", lambda e, ktl=ktl: e.tensor_tensor(out=ktl[:], in0=kv[:], in1=ek[:], op=ALU.mult),
                                 reads=[kv, ek], writes=[ktl])
                            P.op("dve", lambda e: e.tensor_tensor(out=kh_[:], in0=kv[:], in1=dd[:], op=ALU.mult),
                                 reads=[kv, dd], writes=[kh_])
                        for sub in range(NDIAG):
                            transpose_to(khat[:, sub, 128 * rc:128 * rc + 128], kh_[:, 128 * sub:128 * sub + 128],
                                         ("khat", sub, rc), kh_)
                        W = 64 * hpc
                        wb = W * rc
                        D3 = Dsb[:].rearrange("p (v c) -> p v c", c=8)
                        G3 = Grep[:].rearrange("p (v c) -> p v c", c=8)
                        for sub in range(NDIAG):
                            P.op("pool", lambda e, sub=sub: e.tensor_tensor(
                                out=Vm[:], in0=Vtm[:, sub, :].unsqueeze(1).broadcast_to([128, 4, 256]),
                                in1=cstb[:, C_HM32:C_HM32 + 4].unsqueeze(2).broadcast_to([128, 4, 256]), op=ALU.mult),
                                reads=[("Vtm", sub), cstb], writes=[("Vm", 0)])
                            nslot = 512 // W
                            for c0_ in range(0, 4, nslot):
                                dps = pring.get()
                                for sl_ in range(nslot):
                                    cc = c0_ + sl_
                                    P.op("pe", lambda e, dps=dps, sl_=sl_, cc=cc, sub=sub: e.matmul(
                                        dps[:, sl_ * W:(sl_ + 1) * W], lhsT=khat[:, sub, 128 * rc:128 * rc + 128],
                                        rhs=Vm[:, cc, wb:wb + W], start=True, stop=True),
                                        reads=[("khat", sub, rc), ("Vm", 0)], writes=[dps])
                                for hh in range(hpc):
                                    hoff = Kh * hh
                                    cg = 4 * sub + c0_
                                    P.op("dve", lambda e, dps=dps, hh=hh, hoff=hoff, cg=cg: e.tensor_copy(
                                        out=D3[hoff:hoff + Kh, :, cg:cg + nslot],
                                        in_=dps[hoff:hoff + Kh, 0:nslot * W].rearrange("p (s w) -> p w s", w=W)[:, 64 * hh:64 * hh + 64, :]),
                                        reads=[dps], writes=[Dsb])
                            if kind == "C" and rc == 0 and sub == 0:
                                pass
                        cr = carry[(0 if kind == "B" else 1) + rc]
                        P.op("act", lambda e, g_=g_: e.activation(out=G3, in_=g_[:].unsqueeze(1).broadcast_to([128, 64, 8]),
                                                                  func=AF.Copy), reads=[g_], writes=[Grep])
                        P.op("pool", lambda e: e.memset(G3[:, :, 0:1], 0.0), reads=[], writes=[Grep])
                        P.op("dve", lambda e, cr=cr, g_=g_: e.scalar_tensor_tensor(
                            out=D3[:, :, 0], in0=cr[:], scalar=g_[:, 0:1], in1=D3[:, :, 0], op0=ALU.mult, op1=ALU.add),
                            reads=[cr, g_, Dsb], writes=[Dsb])
                        P.op("dve", lambda e: e.tensor_tensor_scan(out=Hs[:], data0=Grep[:], data1=Dsb[:], initial=0.0,
                                                                   op0=ALU.mult, op1=ALU.add), reads=[Grep, Dsb], writes=[Hs])
                        hb = HbB if kind == "B" else HbC[rc]
                        Hs_cv = Hs[:].rearrange("p (v c) -> p c v", c=8)
                        for hh in range(hpc):
                            hoff = Kh * hh
                            P.op("act", lambda e, hb=hb, cr=cr, hh=hh, hoff=hoff: e.activation(
                                out=hb[hoff:hoff + Kh, 0, 64 * hh:64 * hh + 64], in_=cr[hoff:hoff + Kh, :], func=AF.Copy),
                                reads=[cr], writes=[hb])
                            P.op("act", lambda e, hb=hb, hh=hh, hoff=hoff: e.activation(
                                out=hb[hoff:hoff + Kh, 1:8, 64 * hh:64 * hh + 64], in_=Hs_cv[hoff:hoff + Kh, 0:7, :], func=AF.Copy),
                                reads=[Hs], writes=[hb])
                        P.op("dve", lambda e, cr=cr: e.tensor_copy(out=cr[:], in_=Hs[:].rearrange("p (v c) -> p v c", c=8)[:, :, 7]),
                             reads=[Hs], writes=[cr])
                    W = 64 * hpc
                    hmK = cstb[:, C_HM32:C_HM32 + 4] if kind == "B" else cstb[:, C_HM64:C_HM64 + 2]
                    for sub in range(NDIAG):
                        sps = pring.get()
                        tsl = slice(128 * sub, 128 * sub + 128)
                        for rc in range(RC):
                            P.op("pool", lambda e, rc=rc, tsl=tsl: e.tensor_tensor(
                                out=Qbd[rc][:, 0:hpc * 128].rearrange("p (h i) -> p h i", h=hpc),
                                in0=qt_l[rc][:, tsl].unsqueeze(1).broadcast_to([128, hpc, 128]),
                                in1=hmK.unsqueeze(2).broadcast_to([128, hpc, 128]), op=ALU.mult),
                                reads=[qt_l[rc], cstb], writes=[Qbd[rc]])
                            P.op("pe", lambda e, rc=rc, tsl=tsl, sps=sps: e.matmul(
                                sps[:, 128 * hpc * rc:128 * hpc * (rc + 1)], lhsT=kt_l[rc][:, tsl], rhs=Qbd[rc][:, 0:hpc * 128],
                                start=True, stop=True), reads=[kt_l[rc], Qbd[rc]], writes=[sps])
                            P.op("pool", lambda e, rc=rc, tsl=tsl: e.tensor_tensor(
                                out=Qm[rc][:], in0=qt_l[rc][:, tsl].unsqueeze(1).broadcast_to([128, 4, 128]),
                                in1=cstb[:, C_TM4:C_TM4 + 512].rearrange("p (c i) -> p c i", c=4), op=ALU.mult),
                                reads=[qt_l[rc], cstb], writes=[Qm[rc]])
                        P.op("dve", lambda e, sps=sps: e.tensor_tensor(
                            out=Smk[:], in0=sps[:].rearrange("p (h i) -> p h i", h=4),
                            in1=cstb[:, C_BD32:C_BD32 + 128].unsqueeze(1).broadcast_to([128, 4, 128]), op=ALU.mult),
                            reads=[sps, cstb], writes=[Smk])
                        P.op("pool", lambda e, sub=sub: e.tensor_tensor(
                            out=Vbd[:], in0=Vtm[:, sub, :].unsqueeze(1).broadcast_to([128, 4, 256]),
                            in1=cstb[:, C_CM:C_CM + 1024].rearrange("p (h c) -> p h c", h=4), op=ALU.mult),
                            reads=[("Vtm", sub), cstb], writes=[Vbd])
                        ops_ = pring.get()
                        for h in range(4):
                            P.op("pe", lambda e, h=h, ops_=ops_: e.matmul(
                                ops_[:, 0:256], lhsT=Smk[:, h, :], rhs=Vbd[:, h, :], start=(h == 0), stop=False),
                                reads=[Smk, Vbd], writes=[ops_])
                        for rc in range(RC):
                            hb = HbB if kind == "B" else HbC[rc]
                            for cc in range(4):
                                last = (rc == RC - 1 and cc == 3)
                                P.op("pe", lambda e, rc=rc, cc=cc, hb=hb, last=last, ops_=ops_: e.matmul(
                                    ops_[:, W * rc:W * (rc + 1)], lhsT=Qm[rc][:, cc, :], rhs=hb[:, 4 * sub + cc, :],
                                    start=False, stop=last), reads=[Qm[rc], hb], writes=[ops_])
                        sqo, on = F[9], F[10]
                        P.op("act", lambda e, ops_=ops_: e.activation(out=sqo[:], in_=ops_[:, 0:256], func=AF.Square),
                             reads=[ops_], writes=[sqo])
                        P.op("dve", lambda e: e.tensor_reduce(out=st4[:, 0:4], in_=sqo[:].rearrange("p (h v) -> p h v", h=4),
                                                              axis=AX.X, op=ALU.add), reads=[sqo], writes=[st4])
                        P.op("act", lambda e: e.activation(out=st4[:, 4:8], in_=st4[:, 0:4], func=AF.Sqrt, scale=1.0 / 64.0,
                                                           bias=RMS_EPS), reads=[st4], writes=[st4])
                        P.op("dve", lambda e: e.reciprocal(out=st4[:, 8:12], in_=st4[:, 4:8]), reads=[st4], writes=[st4])
                        P.op("dve", lambda e, ops_=ops_: e.tensor_tensor(
                            out=on[:].rearrange("p (h v) -> p h v", h=4), in0=ops_[:, 0:256].rearrange("p (h v) -> p h v", h=4),
                            in1=st4[:, 8:12].unsqueeze(2).broadcast_to([128, 4, 64]), op=ALU.mult),
                            reads=[ops_, st4], writes=[on])
                        P.op("dve", lambda e: e.tensor_tensor(out=on[:], in0=on[:], in1=grow, op=ALU.mult),
                             reads=[on, rows], writes=[on])
                        P.op("dve", lambda e, sub=sub: e.tensor_tensor(out=mtm[:], in0=on[:], in1=sog[:, sub, :], op=ALU.mult),
                             reads=[on, ("sog", sub)], writes=[mtm])
                        for j in range(2):
                            transpose_to(mixB[:, cbase + j, 128 * sub:128 * sub + 128], mtm[:, 128 * j:128 * j + 128],
                                         ("mixB", cbase + j), mtm, eng="dve")

                if phase == "b" and "B" in mixers:
                    lin_attn("B")
                if phase == "b":
                    prefetch()
                if phase == "b" and "C" in mixers:
                    lin_attn("C")
                def rwkv():
                    CD = 0.606531
                    hm64 = cstb[:, C_HM64:C_HM64 + 2]

                    def shifted(col0, nrows, mucol, pidx, out_ap, out_key):
                        ps = fm_proj(col0, nrows)
                        P.op("pool", lambda e: e.tensor_copy(out=pf[0:nrows, 0:1], in_=prevc[0:nrows, pidx:pidx + 1]),
                             reads=[prevc], writes=[pf])
                        P.op("act", lambda e: e.activation(out=pf[0:nrows, 1:TT + 1], in_=ps[0:nrows, 0:TT], func=AF.Copy),
                             reads=[ps], writes=[pf])
                        dtmp = F[15]
                        P.op("pool", lambda e: e.tensor_tensor(out=dtmp[0:nrows, :], in0=pf[0:nrows, 0:TT], in1=pf[0:nrows, 1:TT + 1],
                                                               op=ALU.subtract), reads=[pf], writes=[dtmp])
                        P.op("dve", lambda e: e.scalar_tensor_tensor(
                            out=out_ap, in0=dtmp[0:nrows, :], scalar=vcol(mucol, nrows), in1=pf[0:nrows, 1:TT + 1],
                            op0=ALU.mult, op1=ALU.add), reads=[dtmp, pf, vecs], writes=[out_key])
                        P.op("pool", lambda e: e.tensor_copy(out=prevc[0:nrows, pidx:pidx + 1], in_=pf[0:nrows, TT:TT + 1]),
                             reads=[pf], writes=[prevc])

                    tw, adb, sgd = Bb[0], Bb[1], Bb[2]
                    wdf = F[14]
                    shifted(3344, 32, V_MU + 6, 6, wdf[0:32, :], wdf)
                    P.op("act", lambda e: e.activation(out=tw[0:32, :], in_=wdf[0:32, :], func=AF.Tanh), reads=[wdf], writes=[tw])
                    shifted(3376, 32, V_MU + 7, 7, wdf[0:32, :], wdf)
                    P.op("act", lambda e: e.activation(out=adb[0:32, :], in_=wdf[0:32, :], func=AF.Copy), reads=[wdf], writes=[adb])
                    shifted(3408, 64, V_MU + 8, 8, wdf[0:64, :], wdf)
                    P.op("act", lambda e: e.activation(out=sgd[0:64, :], in_=wdf[0:64, :], func=AF.Sigmoid), reads=[wdf], writes=[sgd])

                    for rc in range(2):
                        rp, kp, vp, sgw, av, gT, kkn, kmod, bonus, Lw, Lx, eX, qa, dd = (F[i] for i in range(14))
                        shifted(2576 + 128 * rc, 128, V_MU + rc, rc, rp[:], rp)
                        shifted(2832 + 128 * rc, 128, V_MU + 2 + rc, 2 + rc, kp[:], kp)
                        shifted(3088 + 128 * rc, 128, V_MU + 4 + rc, 4 + rc, vp[:], vp)
                        P.op("act", lambda e: e.activation(out=vpb[:], in_=vp[:], func=AF.Copy), reads=[vp], writes=[vpb])
                        wps = pring.get()
                        P.op("pe", lambda e: e.matmul(wps[:, 0:TT], lhsT=swb[0:32, SW_WU + 128 * rc:SW_WU + 128 * rc + 128],
                                                      rhs=tw[0:32, :], start=True, stop=True), reads=[swb, tw], writes=[wps])
                        P.op("act", lambda e: e.activation(out=sgw[:], in_=wps[:, 0:TT], func=AF.Sigmoid, bias=vcol(V_W0 + rc), scale=1.0),
                             reads=[wps, vecs], writes=[sgw])
                        aps = pring.get()
                        P.op("pe", lambda e: e.matmul(aps[:, 0:TT], lhsT=swb[0:32, SW_AU + 128 * rc:SW_AU + 128 * rc + 128],
                                                      rhs=adb[0:32, :], start=True, stop=True), reads=[swb, adb], writes=[aps])
                        P.op("act", lambda e: e.activation(out=av[:], in_=aps[:, 0:TT], func=AF.Sigmoid, bias=vcol(V_A0 + rc), scale=1.0),
                             reads=[aps, vecs], writes=[av])
                        gps = pring.get()
                        P.op("pe", lambda e: e.matmul(gps[:, 0:TT], lhsT=swb[0:64, SW_GUP + 128 * rc:SW_GUP + 128 * rc + 128],
                                                      rhs=sgd[0:64, :], start=True, stop=True), reads=[swb, sgd], writes=[gps])
                        P.op("act", lambda e: e.activation(out=gT[:], in_=gps[:, 0:TT], func=AF.Copy), reads=[gps], writes=[gT])
                        P.op("dve", lambda e: e.tensor_scalar(out=kkn[:], in0=kp[:], scalar1=vcol(V_KK + rc), scalar2=None, op0=ALU.mult),
                             reads=[kp, vecs], writes=[kkn])
                        sqk = Bb[3]
                        P.op("act", lambda e: e.activation(out=sqk[:], in_=kkn[:], func=AF.Square), reads=[kkn], writes=[sqk])
                        sps_ = pring.get()
                        P.op("pe", lambda e: e.matmul(sps_[:, 0:TT], lhsT=bo64[:], rhs=sqk[:], start=True, stop=True),
                             reads=[bo64, sqk], writes=[sps_])
                        P.op("dve", lambda e: e.tensor_scalar(out=eX[:], in0=sps_[:, 0:TT], scalar1=1e-24, scalar2=None, op0=ALU.max),
                             reads=[sps_], writes=[eX])
                        P.op("act", lambda e: e.activation(out=eX[:], in_=eX[:], func=AF.Sqrt), reads=[eX], writes=[eX])
                        P.op("dve", lambda e: e.reciprocal(out=eX[:], in_=eX[:]), reads=[eX], writes=[eX])
                        P.op("dve", lambda e: e.tensor_tensor(out=kkn[:], in0=kkn[:], in1=eX[:], op=ALU.mult), reads=[kkn, eX], writes=[kkn])
                        P.op("dve", lambda e: e.tensor_scalar(out=kmod[:], in0=av[:], scalar1=-1.0, scalar2=vcol(V_KA + rc),
                                                              op0=ALU.add, op1=ALU.mult), reads=[av, vecs], writes=[kmod])
                        P.op("dve", lambda e: e.scalar_tensor_tensor(out=kmod[:], in0=kmod[:], scalar=1.0, in1=kp[:],
                                                                     op0=ALU.add, op1=ALU.mult), reads=[kmod, kp], writes=[kmod])
                        rkb = Bb[4]
                        P.op("dve", lambda e: e.scalar_tensor_tensor(out=rkb[:], in0=rp[:], scalar=vcol(V_RK + rc), in1=kmod[:],
                                                                     op0=ALU.mult, op1=ALU.mult), reads=[rp, kmod, vecs], writes=[rkb])
                        bps = pring.get()
                        P.op("pe", lambda e: e.matmul(bps[:, 0:TT], lhsT=bo64[:], rhs=rkb[:], start=True, stop=True),
                             reads=[bo64, rkb], writes=[bps])
                        P.op("dve", lambda e: e.tensor_tensor(out=bonus[:], in0=bps[:, 0:TT], in1=vp[:], op=ALU.mult),
                             reads=[bps, vp], writes=[bonus])
                        P.op("dve", lambda e: e.tensor_tensor_scan(out=Lw[:], data0=cstb[:, C_RS64:C_RS64 + TT], data1=sgw[:],
                                                                   initial=0.0, op0=ALU.mult, op1=ALU.add),
                             reads=[cstb, sgw], writes=[Lw])
                        P.op("pool", lambda e: e.tensor_tensor(out=Lx[:], in0=Lw[:], in1=sgw[:], op=ALU.subtract),
                             reads=[Lw, sgw], writes=[Lx])
                        Lw3 = Lw[:].rearrange("p (c j) -> p c j", j=64)
                        P.op("dve", lambda e: e.tensor_copy(out=ltot4[:], in_=Lw3[:, :, 63]), reads=[Lw], writes=[ltot4])
                        g4 = gam4[rc]
                        P.op("act", lambda e: e.activation(out=g4[:], in_=ltot4[:], func=AF.Exp, scale=-CD), reads=[ltot4], writes=[g4])
                        PR4 = PRb[:].rearrange("p s (w t) -> p s w t", w=2)
                        P.op("act", lambda e: e.activation(out=eX[:], in_=Lw[:], func=AF.Exp, scale=-CD), reads=[Lw], writes=[eX])
                        P.op("dve", lambda e: e.tensor_tensor(out=PR4[:, :, 1, :], in0=rp[:].rearrange("p (s t) -> p s t", s=2),
                                                              in1=eX[:].rearrange("p (s t) -> p s t", s=2), op=ALU.mult),
                             reads=[rp, eX], writes=[PRb])
                        P.op("act", lambda e: e.activation(out=eX[:], in_=Lw[:], func=AF.Exp, scale=CD), reads=[Lw], writes=[eX])
                        P.op("dve", lambda e: e.tensor_tensor(out=Ktb[:], in0=kmod[:], in1=eX[:], op=ALU.mult), reads=[kmod, eX], writes=[Ktb])
                        P.op("pool", lambda e: e.tensor_tensor(out=qa[:], in0=kkn[:], in1=av[:], op=ALU.mult), reads=[kkn, av], writes=[qa])
                        P.op("dve", lambda e: e.tensor_tensor(out=Qtb[:], in0=qa[:], in1=eX[:], op=ALU.mult), reads=[qa, eX], writes=[Qtb])
                        P.op("act", lambda e: e.activation(out=eX[:], in_=Lx[:], func=AF.Exp, scale=-CD), reads=[Lx], writes=[eX])
                        P.op("dve", lambda e: e.scalar_tensor_tensor(
                            out=PR4[:, :, 0, :], in0=kkn[:].rearrange("p (s t) -> p s t", s=2), scalar=-1.0,
                            in1=eX[:].rearrange("p (s t) -> p s t", s=2), op0=ALU.mult, op1=ALU.mult),
                            reads=[kkn, eX], writes=[PRb])
                        P.op("dve", lambda e: e.tensor_tensor(
                            out=dd[:].rearrange("p (c j) -> p c j", j=64), in0=ltot4[:].unsqueeze(2).broadcast_to([128, 4, 64]),
                            in1=Lw3, op=ALU.subtract), reads=[ltot4, Lw], writes=[dd])
                        P.op("act", lambda e: e.activation(out=dd[:], in_=dd[:], func=AF.Exp, scale=-CD), reads=[dd], writes=[dd])
                        P.op("dve", lambda e: e.tensor_tensor(out=Khb[:], in0=kmod[:], in1=dd[:], op=ALU.mult), reads=[kmod, dd], writes=[Khb])
                        P.op("pool", lambda e: e.tensor_tensor(out=Qhb[:], in0=qa[:], in1=dd[:], op=ALU.mult), reads=[qa, dd], writes=[Qhb])

                        hx = Hexp[rc]
                        hcur = [Hbf[0]]
                        hring = Ring(Hbf)
                        hring.i = 1
                        P.op("act", lambda e: e.activation(out=Hbf[0][:], in_=hx[:], func=AF.Copy), reads=[hx], writes=[Hbf[0]])
                        for sub in range(NDIAG):
                            tsl = slice(128 * sub, 128 * sub + 128)
                            transpose_to(Vdtm[:], vpb[:, tsl], Vdtm, vpb)
                            transpose_to(Ptm[:], PR4[:, sub, 0, :], Ptm, PRb)
                            transpose_to(Qhtm[:], Qhb[:, tsl], Qhtm, Qhb)
                            transpose_to(Khtm[:], Khb[:, tsl], Khtm, Khb)
                            P.op("pool", lambda e: e.tensor_tensor(
                                out=PRm[:], in0=PRb[:, sub, :].unsqueeze(1).broadcast_to([128, 2, 256]),
                                in1=hm64.unsqueeze(2).broadcast_to([128, 2, 256]), op=ALU.mult), reads=[PRb, cstb], writes=[PRm])
                            P.op("pool", lambda e: e.tensor_tensor(
                                out=Rm[:], in0=PR4[:, sub, 1, :].unsqueeze(1).broadcast_to([128, 2, 128]),
                                in1=cstb[:, C_TM2:C_TM2 + 256].rearrange("p (j t) -> p j t", j=2), op=ALU.mult),
                                reads=[PRb, cstb], writes=[Rm])
                            P.op("pool", lambda e: e.tensor_tensor(
                                out=Vmj[:], in0=Vdtm[:].unsqueeze(1).broadcast_to([128, 2, 128]),
                                in1=hm64.unsqueeze(2).broadcast_to([128, 2, 128]), op=ALU.mult), reads=[Vdtm, cstb], writes=[Vmj])
                            wcur = 0
                            Mcur = [None, None]
                            Ncur = [None, None]
                            W0 = Wb[0]
                            for hh in range(2):
                                AM = AMb[hh]
                                mps = pring.get()
                                P.op("pe", lambda e: e.matmul(mps[:, 0:256], lhsT=Qtb[:, tsl], rhs=PRm[:, hh, :], start=True, stop=True),
                                     reads=[Qtb, PRm], writes=[mps])
                                P.op("pe", lambda e: e.matmul(mps[:, 256:512], lhsT=Ktb[:, tsl], rhs=PRm[:, hh, :], start=True, stop=True),
                                     reads=[Ktb, PRm], writes=[mps])
                                P.op("dve", lambda e: e.tensor_tensor(
                                    out=AM[:], in0=mps[:].rearrange("p (a b) -> p a b", a=2),
                                    in1=cstb[:, C_ST64:C_ST64 + 256].unsqueeze(1).broadcast_to([128, 2, 256]), op=ALU.mult),
                                    reads=[mps, cstb], writes=[AM])
                                lps = pring.get()
                                P.op("pe", lambda e: e.matmul(lps[:, 0:128], lhsT=PRm[:, hh, 0:128], rhs=Qtb[:, tsl], start=True, stop=True),
                                     reads=[Qtb, PRm], writes=[lps])
                                N0 = Nb[hh][2]
                                P.op("dve", lambda e: e.tensor_tensor(out=N0[:], in0=lps[:, 0:128], in1=cstb[:, C_LO64:C_LO64 + 128],
                                                                      op=ALU.mult), reads=[lps, cstb], writes=[N0])
                                Ncur[hh] = N0
                                Mcur[hh] = (AM[:, 0, 0:128], AM)
                                avp = pring.get()
                                P.op("pe", lambda e: e.matmul(avp[:, 0:64], lhsT=AM[:, 1, 0:128], rhs=Vdtm[:, 64 * hh:64 * hh + 64],
                                                              start=True, stop=True), reads=[AM, Vdtm], writes=[avp])
                                P.op("act", lambda e: e.activation(out=W0[:, hh, 64:128], in_=avp[:, 0:64], func=AF.Copy),
                                     reads=[avp], writes=[(W0.name, hh)])
                                P.op("pool", lambda e: e.tensor_copy(out=W0[:, hh, 0:64], in_=Ptm[:, 64 * hh:64 * hh + 64]),
                                     reads=[Ptm], writes=[(W0.name, hh)])
                            wi = 0
                            for i in range(6):
                                Wc, Wn = Wb[wi], Wb[1 - wi]
                                for hh in range(2):
                                    Mcur_ap, Mcur_key = Mcur[hh]
                                    Nc = Ncur[hh]
                                    ups = pring.get()
                                    P.op("pe", lambda e: e.matmul(ups[:, 0:128], lhsT=Mcur_ap, rhs=Wc[:, hh, :], start=True, stop=True),
                                         reads=[Mcur_key, (Wc.name, hh)], writes=[ups])
                                    P.op("dve", lambda e: e.tensor_tensor(out=Wn[:, hh, :], in0=ups[:, 0:128], in1=Wc[:, hh, :], op=ALU.add),
                                         reads=[ups, (Wc.name, hh)], writes=[(Wn.name, hh)])
                                    if i < 5:
                                        Mn, Nn = Mb[hh][i % 2], Nb[hh][i % 2]
                                        m2 = pring.get()
                                        P.op("pe", lambda e: e.matmul(m2[:, 0:128], lhsT=Nc[:], rhs=Mcur_ap, start=True, stop=True),
                                             reads=[Nc, Mcur_key], writes=[m2])
                                        P.op("pe", lambda e: e.matmul(m2[:, 128:256], lhsT=Mcur_ap, rhs=Nc[:], start=True, stop=True),
                                             reads=[Nc, Mcur_key], writes=[m2])
                                        P.op("act", lambda e: e.activation(
                                            out=Mn[:], in_=m2[:, 0:128], func=AF.Copy), reads=[m2], writes=[Mn])
                                        P.op("act", lambda e: e.activation(
                                            out=Nn[:], in_=m2[:, 128:256], func=AF.Copy), reads=[m2], writes=[Nn])
                                        Mcur[hh] = (Mn[:], Mn)
                                        Ncur[hh] = Nn
                                wi = 1 - wi
                            wcur = wi
                            Wf = Wb[wcur]
                            P.op("pool", lambda e: e.tensor_copy(out=PH[:].rearrange("p (h k) -> p h k", h=2), in_=Wf[:, :, 0:64]),
                                 reads=[(Wf.name, 0), (Wf.name, 1)], writes=[PH])
                            transpose_to(PHT[:], PH[:], PHT, PH)
                            P.op("pool", lambda e: e.tensor_tensor(
                                out=Uhm[:].rearrange("p j (h v) -> p j h v", h=2),
                                in0=Wf[:, :, 64:128].unsqueeze(1).broadcast_to([128, 2, 2, 64]),
                                in1=hm64.unsqueeze(2).unsqueeze(3).broadcast_to([128, 2, 2, 64]), op=ALU.mult),
                                reads=[(Wf.name, 0), (Wf.name, 1), cstb], writes=[Uhm])
                            hstart = []
                            for j in range(2):
                                hb_c = hcur[0]
                                hstart.append(hb_c)
                                ups = pring.get()
                                P.op("pe", lambda e: e.matmul(ups[:, 0:128], lhsT=PHT[:], rhs=hb_c[:], start=True, stop=True),
                                     reads=[PHT, hb_c], writes=[ups])
                                P.op("dve", lambda e: e.scalar_tensor_tensor(
                                    out=Umb[:, j, :], in0=ups[:, 0:128], scalar=hm64[:, j:j + 1], in1=Uhm[:, j, :],
                                    op0=ALU.mult, op1=ALU.add), reads=[ups, Uhm, cstb], writes=[("Umb", j)])
                                hps = pring.get()
                                P.op("pe", lambda e: e.matmul(hps[:, 0:128], lhsT=Qhtm[:], rhs=Umb[:, j, :], start=True, stop=False),
                                     reads=[Qhtm, ("Umb", j)], writes=[hps])
                                P.op("pe", lambda e: e.matmul(hps[:, 0:128], lhsT=Khtm[:], rhs=Vmj[:, j, :], start=False, stop=True),
                                     reads=[Khtm, Vmj], writes=[hps])
                                htmp = F[14]
                                P.op("dve", lambda e: e.tensor_tensor(out=htmp[:, 0:128], in0=hps[:, 0:128],
                                                                      in1=cstb[:, C_HM64B:C_HM64B + 128], op=ALU.mult),
                                     reads=[hps, cstb], writes=[htmp])
                                ci = 2 * sub + j
                                P.op("dve", lambda e: e.scalar_tensor_tensor(
                                    out=hx[:], in0=hx[:], scalar=g4[:, ci:ci + 1], in1=htmp[:, 0:128], op0=ALU.mult, op1=ALU.add),
                                    reads=[hx, g4, htmp], writes=[hx])
                                hb_n = hring.get()
                                P.op("act", lambda e: e.activation(out=hb_n[:], in_=hx[:], func=AF.Copy), reads=[hx], writes=[hb_n])
                                hcur[0] = hb_n
                            P.op("pool", lambda e: e.tensor_tensor(out=Ucomb[:], in0=Umb[:, 0, :], in1=Umb[:, 1, :], op=ALU.add),
                                 reads=[("Umb", 0), ("Umb", 1)], writes=[Ucomb])
                            yps = pring.get()
                            for j in range(2):
                                hs_ = hstart[j]
                                P.op("pe", lambda e: e.matmul(yps[:, 0:128], lhsT=Rm[:, j, :], rhs=hs_[:], start=(j == 0), stop=False),
                                     reads=[Rm, hs_], writes=[yps])
                            for hh in range(2):
                                AM = AMb[hh]
                                P.op("pe", lambda e: e.matmul(yps[:, 64 * hh:64 * hh + 64], lhsT=AM[:, 0, 128:256],
                                                              rhs=Ucomb[:, 64 * hh:64 * hh + 64], start=False, stop=False),
                                     reads=[AM, Ucomb], writes=[yps])
                                P.op("pe", lambda e: e.matmul(yps[:, 64 * hh:64 * hh + 64], lhsT=AM[:, 1, 128:256],
                                                              rhs=Vdtm[:, 64 * hh:64 * hh + 64], start=False, stop=(hh == 1)),
                                     reads=[AM, Vdtm], writes=[yps])
                            y3 = ysb[:].rearrange("p (h v) -> p h v", h=2)
                            sqy = F[15]
                            P.op("act", lambda e: e.activation(out=ysb[:], in_=yps[:, 0:128], func=AF.Copy), reads=[yps], writes=[ysb])
                            P.op("act", lambda e: e.activation(out=sqy[:, 0:128], in_=yps[:, 0:128], func=AF.Square), reads=[yps], writes=[sqy])
                            P.op("dve", lambda e: e.tensor_reduce(out=lnst[:, 0:2], in_=y3, axis=AX.X, op=ALU.add), reads=[ysb], writes=[lnst])
                            P.op("dve", lambda e: e.tensor_reduce(out=lnst[:, 2:4], in_=sqy[:, 0:128].rearrange("p (h v) -> p h v", h=2),
                                                                  axis=AX.X, op=ALU.add), reads=[sqy], writes=[lnst])
                            P.op("dve", lambda e: e.tensor_scalar(out=lnst[:, 4:6], in0=lnst[:, 0:2], scalar1=1.0 / 64.0, scalar2=None,
                                                                  op0=ALU.mult), reads=[lnst], writes=[lnst])
                            P.op("dve", lambda e: e.tensor_tensor(out=lnst[:, 6:8], in0=lnst[:, 4:6], in1=lnst[:, 4:6], op=ALU.mult),
                                 reads=[lnst], writes=[lnst])
                            P.op("dve", lambda e: e.scalar_tensor_tensor(out=lnst[:, 8:10], in0=lnst[:, 2:4], scalar=1.0 / 64.0,
                                                                         in1=lnst[:, 6:8], op0=ALU.mult, op1=ALU.subtract),
                                 reads=[lnst], writes=[lnst])
                            P.op("act", lambda e: e.activation(out=lnst[:, 10:12], in_=lnst[:, 8:10], func=AF.Sqrt, bias=64e-5, scale=1.0),
                                 reads=[lnst], writes=[lnst])
                            P.op("dve", lambda e: e.reciprocal(out=lnst[:, 12:14], in_=lnst[:, 10:12]), reads=[lnst], writes=[lnst])
                            P.op("dve", lambda e: e.tensor_tensor(out=y3, in0=y3, in1=lnst[:, 4:6].unsqueeze(2).broadcast_to([128, 2, 64]),
                                                                  op=ALU.subtract), reads=[ysb, lnst], writes=[ysb])
                            P.op("dve", lambda e: e.tensor_tensor(out=ynb[:].rearrange("p (h v) -> p h v", h=2), in0=y3,
                                                                  in1=lnst[:, 12:14].unsqueeze(2).broadcast_to([128, 2, 64]), op=ALU.mult),
                                 reads=[ysb, lnst], writes=[ynb])
                            tp = pring.get()
                            tpb = tp[:].bitcast(BF16)
                            P.op("pe", lambda e: e.transpose(tpb[:, 0:128], ynb[:], ident), reads=[ynb, cstb], writes=[tp])
                            o1 = F[15]
                            P.op("dve", lambda e: e.tensor_scalar(out=o1[:, 0:128], in0=tpb[:, 0:128], scalar1=vcol(V_LNG + rc),
                                                                  scalar2=vcol(V_LNB + rc), op0=ALU.mult, op1=ALU.add),
                                 reads=[tp, vecs], writes=[o1])
                            P.op("pool", lambda e: e.tensor_tensor(out=o1[:, 0:128], in0=o1[:, 0:128], in1=bonus[:, tsl], op=ALU.add),
                                 reads=[o1, bonus], writes=[o1])
                            P.op("dve", lambda e: e.tensor_tensor(out=mixB[:, 4 + rc, tsl], in0=o1[:, 0:128], in1=gT[:, tsl], op=ALU.mult),
                                 reads=[o1, gT], writes=[("mixB", 4 + rc)])

                if phase == "b" and "D" in mixers:
                    rwkv()
                for c2 in range(6):
                    mixer = "BBCCDD"[c2]
                    if phase == "b" and mixer not in mixers:
                        P.op("pool", lambda e, c2=c2: e.memset(mixB[:, c2, :], 0.0), writes=[("mixB", c2)])

                if debug and phase == "a":
                    for h in range(4):
                        dt_ = F[9]
                        P.op("dve", lambda e, h=h: e.tensor_copy(out=dt_[0:64, :], in_=mixA[0:64, h, :]),
                             reads=[("mixA", h)], writes=[dt_])
                        P.dma("sp", lambda e, h=h, c0=c0: e.dma_start(out=dbg[64 * h:64 * h + 64, c0:c0 + TT],
                                                                       in_=dt_[0:64, :]), reads=[dt_])
                if debug and phase == "b":
                    for c2 in range(6 if "D" in mixers else 4):
                        dt_ = F[9]
                        P.op("dve", lambda e, c2=c2: e.tensor_copy(out=dt_[:], in_=mixB[:, c2, :]),
                             reads=[("mixB", c2)], writes=[dt_])
                        P.dma("sp", lambda e, c2=c2, c0=c0: e.dma_start(
                            out=dbg[256 + 128 * c2:256 + 128 * (c2 + 1), c0:c0 + TT], in_=dt_[:]), reads=[dt_])

                prefetch()
                for dc in range(8):
                    ps = pring.get()
                    if phase == "a":
                        for h in range(4):
                            P.op("pe", lambda e, ps=ps, h=h, dc=dc: e.matmul(
                                ps[:, 0:TT], lhsT=woA[0:64, h, 128 * dc:128 * (dc + 1)], rhs=mixA[0:64, h, :],
                                start=(h == 0), stop=(h == 3)), reads=["wo", ("mixA", h)], writes=[ps])
                        xm = F[12 + dc % 2]
                        P.dma("sp", lambda e: e.dma_start(out=xm[:], in_=x_src[128 * dc:128 * (dc + 1), c0:c0 + TT]), writes=[xm])
                        res_ap, res_key = xm[:], xm
                    else:
                        for c2 in range(6):
                            P.op("pe", lambda e, ps=ps, c2=c2, dc=dc: e.matmul(
                                ps[:, 0:TT], lhsT=woB[:, c2, 128 * dc:128 * (dc + 1)], rhs=mixB[:, c2, :],
                                start=(c2 == 0), stop=(c2 == 5)), reads=["wo", ("mixB", c2)], writes=[ps])
                        xm = F[12 + dc % 2]
                        P.dma("sp", lambda e, dc=dc, c0=c0, xm=xm, x_mid=x_mid: e.dma_start(
                            out=xm[:], in_=x_mid[128 * dc:128 * (dc + 1), c0:c0 + TT]), reads=[("xmid", t, dc)], writes=[xm])
                        res_ap, res_key = xm[:], xm
                    xo = F[10 + dc % 2]
                    P.op("dve", lambda e, ps=ps, res_ap=res_ap, xo=xo: e.tensor_tensor(
                        out=xo[:], in0=ps[:, 0:TT], in1=res_ap, op=ALU.add), reads=[ps, res_key], writes=[xo])
                    P.dma("sp", lambda e, dc=dc, c0=c0, xo=xo, x_mid=x_mid: e.dma_start(
                        out=x_mid[128 * dc:128 * (dc + 1), c0:c0 + TT], in_=xo[:]), reads=[xo], writes=[("xmid", t, dc)])

        barrier()
        if not do_mlp:
            for t in range(NT):
                c0 = t * TT
                for k in range(8):
                    P.dma("sp", lambda e, k=k, c0=c0: e.dma_start(out=xs[:, k, :], in_=x_mid[128 * k:128 * (k + 1), c0:c0 + TT]),
                          writes=[("xs", k)])
                    P.dma("sp", lambda e, k=k, c0=c0, x_dst=x_dst: e.dma_start(
                        out=x_dst[128 * k:128 * (k + 1), c0:c0 + TT], in_=xs[:, k, :]), reads=[("xs", k)])
            x_src = x_dst
            continue
        T2 = TT
        for cb in range(0, DFF, 512):
            P.dma("pool", lambda e, li=li, cb=cb: e.dma_start(
                out=wup[:, :, cb:cb + 512], in_=w_up_d[li, :, cb:cb + 512].rearrange("(k p) n -> p k n", p=128)),
                writes=[("wup", cb // 512)])
        for kb in range(0, 32, 4):
            P.dma("pool", lambda e, li=li, kb=kb: e.dma_start(
                out=wdn[:, kb:kb + 4, :], in_=w_dn_d[li, 128 * kb:128 * (kb + 4), :].rearrange("(k p) n -> p k n", p=128)),
                writes=[("wdn", kb // 4)])
        for t in range(S // T2):
            c0 = t * T2
            bi = t % 2
            if t == 0:
                rmsnorm_tile(x_mid, c0, T2, V_GMLP, FM[7], 0)
            hT = hT_l[bi]
            hT_keys = [("hT", bi, k) for k in range(8)]
            for fc in range(32):
                ps = pring.get()
                for k in range(8):
                    P.op("pe", lambda e, k=k, ps=ps, fc=fc: e.matmul(
                        ps[:, 0:T2], lhsT=wup[:, k, 128 * fc:128 * (fc + 1)], rhs=hT[:, k, 0:T2],
                        start=(k == 0), stop=(k == 7)), reads=[("wup", fc // 4)] + hT_keys, writes=[ps])
                r_ = FM[fc % 2]
                P.op("act", lambda e, ps=ps, r_=r_: e.activation(out=r_[:, 0:T2], in_=ps[:, 0:T2], func=AF.Relu),
                     reads=[ps], writes=[r_])
                P.op("dve", lambda e, fc=fc, r_=r_: e.tensor_tensor(out=uT[:, fc, :], in0=r_[:, 0:T2], in1=r_[:, 0:T2],
                                                                  op=ALU.mult), reads=[r_], writes=[("uT", fc)])
            if t + 1 < S // T2:
                rmsnorm_tile(x_mid, (t + 1) * T2, T2, V_GMLP, FM[7], (t + 1) % 2)
            for dc in range(8):
                ps = pring.get()
                for fc in range(32):
                    P.op("pe", lambda e, ps=ps, fc=fc, dc=dc: e.matmul(
                        ps[:, 0:T2], lhsT=wdn[:, fc, 128 * dc:128 * (dc + 1)], rhs=uT[:, fc, :],
                        start=(fc == 0), stop=(fc == 31)), reads=[("wdn", fc // 4), ("uT", fc)], writes=[ps])
                xo = FM[2 + dc % 2]
                xm = FM[4 + dc % 2]
                P.dma("sp", lambda e: e.dma_start(out=xm[:, 0:T2], in_=x_mid[128 * dc:128 * (dc + 1), c0:c0 + T2]), writes=[xm])
                P.op("dve", lambda e, ps=ps, dc=dc, xo=xo: e.tensor_tensor(
                    out=xo[:, 0:T2], in0=ps[:, 0:T2], in1=xm[:, 0:T2], op=ALU.add),
                    reads=[ps, xm], writes=[xo])
                P.dma("sp", lambda e, dc=dc, c0=c0, xo=xo, x_dst=x_dst: e.dma_start(
                    out=x_dst[128 * dc:128 * (dc + 1), c0:c0 + T2], in_=xo[:, 0:T2]), reads=[xo], writes=["xdst"])
        x_src = x_dst
    P.finalize()
    return nc, P


def make_inputs(inp, core, S, layer_ids):
    L = len(layer_ids)
    vs, rs, sws = [], [], []
    for l in layer_ids:
        v, r, sw = pack_layer_params(inp, l)
        vs.append(v); rs.append(r); sws.append(sw)
    lbl = np.ascontiguousarray(np.asarray(inp["hgrn_lb_logits"], np.float32).T.reshape(2, 128, -1).transpose(1, 0, 2).reshape(128, -1))
    m = {
        "xT": np.ascontiguousarray(np.asarray(inp["x"][core, :S], np.float32).T),
        "w_in": np.ascontiguousarray(inp["w_in"][layer_ids]),
        "w_out": np.ascontiguousarray(inp["w_out"][layer_ids]),
        "w_up": np.ascontiguousarray(inp["w_mlp_up"][layer_ids]),
        "w_dn": np.ascontiguousarray(inp["w_mlp_down"][layer_ids]),
        "vecs": np.stack(vs), "rows": np.stack(rs), "smallw": np.stack(sws),
        "lbl": lbl, "consts": make_consts(),
    }
    return m


FUSED = True
N_CORES = 8
SEQ = 8192
DEPTH = 4


def kernel(**inputs):
    inp = {k: np.asarray(v) for k, v in inputs.items()}
    L = DEPTH
    if FUSED:
        nc, _ = build_program(SEQ, L, L, list(range(L)))
        base = make_inputs(inp, 0, SEQ, list(range(L)))
        in_maps = []
        for c in range(N_CORES):
            m = dict(base)
            m["xT"] = np.ascontiguousarray(np.asarray(inp["x"][c], np.float32).T)
            in_maps.append(m)
        res = run_bass_kernel_spmd(nc, in_maps, core_ids=list(range(N_CORES)))
        outs = [res.results[c]["yT"] for c in range(N_CORES)]
    else:
        xT = [np.ascontiguousarray(np.asarray(inp["x"][c], np.float32).T) for c in range(N_CORES)]
        for l in range(L):
            nc, _ = build_program(SEQ, 1, L, [l])
            base = make_inputs(inp, 0, SEQ, [l])
            in_maps = []
            for c in range(N_CORES):
                m = dict(base)
                m["xT"] = xT[c]
                in_maps.append(m)
            res = run_bass_kernel_spmd(nc, in_maps, core_ids=list(range(N_CORES)))
            xT = [np.ascontiguousarray(res.results[c]["yT"]) for c in range(N_CORES)]
        outs = xT
    out = np.stack([np.asarray(o, np.float32).T for o in outs])
    return np.ascontiguousarray(out)
```
